# Optimizing a Trainium2 kernel written in Bass

```python
import jax, jax.numpy as jnp
from jax import lax
import numpy as np

D_MODEL = 2048
BATCH = 16
SEQ = 2048
DEPTH = 2

CHUNK = 64
Q_BLOCK = 128
EPS = 1e-6
PLE_DIM = 256

A_HEADS = 8
A_LATENT = 128
A_TOPK_MAX = 256
IDX_HEADS = 16
IDX_DIM = 64
B_HEADS = 4
B_DK = 128
B_DV = 256
B_GATE_RANK = 16
B_GATE_NORM = 16.0
C_HEADS = 16
C_HEAD_DIM = D_MODEL // C_HEADS
C_WIDTH = C_HEADS * C_HEAD_DIM
FFN_HIDDEN = -(-8 * D_MODEL // (3 * 256)) * 256

EVEN_WIDTHS = (A_HEADS * A_LATENT, A_LATENT, IDX_HEADS * IDX_DIM, IDX_DIM, IDX_HEADS,
               B_HEADS * B_DK, B_HEADS * B_DK, B_HEADS * B_DV, B_HEADS * B_DV, B_GATE_RANK)
EVEN_IN = sum(EVEN_WIDTHS)
EVEN_SPLITS = [sum(EVEN_WIDTHS[:i + 1]) for i in range(len(EVEN_WIDTHS) - 1)]
EVEN_OUT = A_HEADS * A_LATENT + B_HEADS * B_DV

kernel_name = 'hybrid_dsa_gla_stickbreaking'


def _rmsnorm(x, g):
    xf = x.astype(jnp.float32)
    y = xf * lax.rsqrt(jnp.mean(xf * xf, axis=-1, keepdims=True) + EPS)
    return (y * g.astype(jnp.float32)).astype(x.dtype)


def _dsa(q_a, c, q_i, k_i, w_i):
    b, s_len = q_a.shape[0], q_a.shape[1]
    topk = min(A_TOPK_MAX, s_len // 4)
    nb = s_len // Q_BLOCK
    key_pos = jnp.arange(s_len, dtype=jnp.int32)
    starts = jnp.arange(nb, dtype=jnp.int32) * Q_BLOCK

    def to_blocks(t):
        return jnp.moveaxis(t.reshape((b, nb, Q_BLOCK) + t.shape[2:]), 1, 0)

    def one_block(args):
        qa, qi, wi, start = args
        limit = ((start + jnp.arange(Q_BLOCK, dtype=jnp.int32)) // CHUNK + 1) * CHUNK
        admissible = key_pos[None, :] < limit[:, None]
        logits = jnp.einsum('bqhd,bsd->bqsh', qi, k_i).astype(jnp.float32) * IDX_DIM ** -0.5
        score = jnp.einsum('bqsh,bqh->bqs', jax.nn.relu(logits),
                           wi.astype(jnp.float32)) * IDX_HEADS ** -0.5
        score = jnp.where(admissible[None], score, -jnp.inf)
        _, idx = lax.top_k(score, topk)
        c_sel = jax.vmap(lambda cb, ib: cb[ib])(c, idx)
        valid = idx < limit[None, :, None]
        att = jnp.einsum('bqhd,bqkd->bqhk', qa, c_sel).astype(jnp.float32) * A_LATENT ** -0.5
        att = jnp.where(valid[:, :, None, :], att, -jnp.inf)
        prob = jax.nn.softmax(att, axis=-1).astype(c.dtype)
        return jnp.einsum('bqhk,bqkd->bqhd', prob, c_sel)

    out = lax.map(one_block, (to_blocks(q_a), to_blocks(q_i), to_blocks(w_i), starts))
    return jnp.moveaxis(out, 0, 1).reshape(b, s_len, A_HEADS * A_LATENT)


def _gla(q, k, v, gk):
    b, s_len, h, dk = q.shape
    dv = v.shape[-1]
    nc = s_len // CHUNK

    def to_chunks(t):
        return t.reshape(b, nc, CHUNK, h, t.shape[-1]).transpose(0, 3, 1, 2, 4)

    f32 = jnp.float32
    q = to_chunks(q).astype(f32) * dk ** -0.5
    k = to_chunks(k).astype(f32)
    v = to_chunks(v).astype(f32)
    G = jnp.cumsum(to_chunks(gk.astype(f32)), axis=3)
    G_last = G[:, :, :, -1:, :]
    q_e = q * jnp.exp(G)
    k_e = k * jnp.exp(-G)
    k_d = k * jnp.exp(G_last - G)
    causal = jnp.tril(jnp.ones((CHUNK, CHUNK), dtype=bool))
    att = jnp.where(causal, jnp.einsum('bhncd,bhnsd->bhncs', q_e, k_e), 0.0)
    o = jnp.einsum('bhncs,bhnsv->bhncv', att, v)
    kv = jnp.einsum('bhnsd,bhnsv->bhndv', k_d, v)
    decay = jnp.exp(G_last[:, :, :, 0, :])

    def step(state, inp):
        dec, inc = inp
        return dec[..., None] * state + inc, state

    s0 = jnp.zeros((b, h, dk, dv), f32)
    _, s_prev = lax.scan(step, s0, (jnp.moveaxis(decay, 2, 0), jnp.moveaxis(kv, 2, 0)))
    s_prev = jnp.moveaxis(s_prev, 0, 2)
    o = o + jnp.einsum('bhncd,bhndv->bhncv', q_e, s_prev)
    return o.transpose(0, 2, 3, 1, 4).reshape(b, s_len, h, dv)


def _stick_breaking(q, k, v):
    b, s_len, h, d = q.shape
    q, k, v = (t.transpose(0, 2, 1, 3) for t in (q, k, v))
    outs = []
    for n in range(s_len // Q_BLOCK):
        start, end = n * Q_BLOCK, (n + 1) * Q_BLOCK
        z = jnp.einsum('bhqd,bhsd->bhqs', q[:, :, start:end], k[:, :, :end]).astype(jnp.float32) * d ** -0.5
        qpos = start + jnp.arange(Q_BLOCK)
        kpos = jnp.arange(end)
        mask = kpos[None, :] < qpos[:, None]
        log_1mb = jnp.where(mask, -jax.nn.softplus(z), 0.0)
        suffix = lax.cumsum(log_1mb, axis=3, reverse=True) - log_1mb
        weight = jnp.where(mask, jnp.exp(suffix - jax.nn.softplus(-z)), 0.0)
        outs.append(jnp.einsum('bhqs,bhsd->bhqd', weight.astype(v.dtype), v[:, :, :end]))
    o = jnp.concatenate(outs, axis=2)
    return o.transpose(0, 2, 1, 3).reshape(b, s_len, h * d)


def _even_mixer(h, norm_g, w_in, w_gk, b_gk, gla_norm, w_out):
    b, s_len, _ = h.shape
    u = _rmsnorm(h, norm_g) @ w_in
    q_a, c_a, q_i, k_i, w_i, q_b, k_b, v_b, g_b, gk_lr = jnp.split(u, EVEN_SPLITS, axis=-1)
    o_a = _dsa(q_a.reshape(b, s_len, A_HEADS, A_LATENT), c_a,
               q_i.reshape(b, s_len, IDX_HEADS, IDX_DIM), k_i, w_i)
    gk = jax.nn.log_sigmoid((gk_lr @ w_gk + b_gk).astype(jnp.float32)) / B_GATE_NORM
    o_b = _gla(q_b.reshape(b, s_len, B_HEADS, B_DK), k_b.reshape(b, s_len, B_HEADS, B_DK),
               v_b.reshape(b, s_len, B_HEADS, B_DV), gk.reshape(b, s_len, B_HEADS, B_DK))
    o_b = _rmsnorm(o_b, gla_norm) * jax.nn.silu(g_b.reshape(b, s_len, B_HEADS, B_DV).astype(jnp.float32))
    o = jnp.concatenate([o_a.astype(h.dtype), o_b.reshape(b, s_len, B_HEADS * B_DV).astype(h.dtype)], axis=-1)
    return o @ w_out


def _odd_mixer(h, norm_g, w_in, w_out):
    b, s_len, _ = h.shape
    q, k, v = jnp.split(_rmsnorm(h, norm_g) @ w_in, 3, axis=-1)
    shp = (b, s_len, C_HEADS, C_HEAD_DIM)
    o = _stick_breaking(q.reshape(shp), k.reshape(shp), v.reshape(shp))
    return o.astype(h.dtype) @ w_out


def _swiglu(h, norm_g, w_gate, w_up, w_down):
    xn = _rmsnorm(h, norm_g)
    return (jax.nn.silu(xn @ w_gate) * (xn @ w_up)) @ w_down


def setup_inputs(seed: int = 0) -> dict:
    key = jax.random.key(seed)
    ks = jax.random.split(key, 20)
    f32 = jnp.float32
    ne = (DEPTH + 1) // 2
    no = DEPTH // 2

    def dense(k, shape):
        return jax.random.normal(k, shape, f32) * shape[-2] ** -0.5

    def gain(k, shape):
        return 1.0 + 0.02 * jax.random.normal(k, shape, f32)

    return {
        'x': jax.random.normal(ks[0], (BATCH, SEQ, D_MODEL), f32),
        'p': jax.random.normal(ks[1], (DEPTH, BATCH, SEQ, PLE_DIM), f32),
        'even_norm': gain(ks[2], (ne, D_MODEL)),
        'even_w_in': dense(ks[3], (ne, D_MODEL, EVEN_IN)),
        'even_w_gk': dense(ks[4], (ne, B_GATE_RANK, B_HEADS * B_DK)),
        'even_b_gk': 0.01 * jax.random.normal(ks[5], (ne, B_HEADS * B_DK), f32),
        'even_gla_norm': gain(ks[6], (ne, B_DV)),
        'even_w_out': dense(ks[7], (ne, EVEN_OUT, D_MODEL)),
        'odd_norm': gain(ks[8], (no, D_MODEL)),
        'odd_w_in': dense(ks[9], (no, D_MODEL, 3 * C_WIDTH)),
        'odd_w_out': dense(ks[10], (no, C_WIDTH, D_MODEL)),
        'ffn_norm': gain(ks[11], (DEPTH, D_MODEL)),
        'ffn_w_gate': dense(ks[12], (DEPTH, D_MODEL, FFN_HIDDEN)),
        'ffn_w_up': dense(ks[13], (DEPTH, D_MODEL, FFN_HIDDEN)),
        'ffn_w_down': dense(ks[14], (DEPTH, FFN_HIDDEN, D_MODEL)),
        'ple_norm': gain(ks[15], (DEPTH, D_MODEL)),
        'ple_w_gate': dense(ks[16], (DEPTH, D_MODEL, D_MODEL)),
        'ple_w_proj': dense(ks[17], (DEPTH, PLE_DIM, D_MODEL)),
        'final_norm': gain(ks[18], (D_MODEL,)),
    }


def reference(x, p, even_norm, even_w_in, even_w_gk, even_b_gk, even_gla_norm, even_w_out,
              odd_norm, odd_w_in, odd_w_out, ffn_norm, ffn_w_gate, ffn_w_up, ffn_w_down,
              ple_norm, ple_w_gate, ple_w_proj, final_norm):
    h = x
    for i in range(DEPTH):
        j = i // 2
        if i % 2 == 0:
            h = h + _even_mixer(h, even_norm[j], even_w_in[j], even_w_gk[j], even_b_gk[j],
                                even_gla_norm[j], even_w_out[j])
        else:
            h = h + _odd_mixer(h, odd_norm[j], odd_w_in[j], odd_w_out[j])
        h = h + _swiglu(h, ffn_norm[i], ffn_w_gate[i], ffn_w_up[i], ffn_w_down[i])
        gate = jax.nn.sigmoid(_rmsnorm(h, ple_norm[i]) @ ple_w_gate[i])
        h = h + (p[i] @ ple_w_proj[i]) * gate
    return _rmsnorm(h, final_norm)
```

```python
import contextlib
import numpy as np
import concourse.bass as bass
import concourse.mybir as mybir
from concourse.bass_utils import run_bass_kernel_spmd

F32 = mybir.dt.float32
BF16 = mybir.dt.bfloat16
AF = mybir.ActivationFunctionType
ALU = mybir.AluOpType

D = 2048
FFH = 5632
PLE = 256
EPS = 1e-6
NEG = -30000.0
BIG = 1.0e30


class Ev:
    __slots__ = ("sem", "val", "key")

    def __init__(self, sem, val, key):
        self.sem, self.val, self.key = sem, val, key


class Res:
    __slots__ = ("w", "r", "name", "multi")

    def __init__(self, name="", multi=False):
        self.w = {}
        self.r = {}
        self.name = name
        self.multi = multi


class Eng:
    def __init__(self, fw, name, obj, is_pe=False):
        self.fw, self.name, self.obj, self.is_pe = fw, name, obj, is_pe
        self.seen = {}
        self.ownkeys = set()
        self._newsem()

    def _newsem(self):
        self.sem = self.fw.newsem(self.name)
        self.ownkeys.add(id(self.sem))
        self.cnt = 0

    def signal(self, inst):
        if self.cnt >= 30000:
            self._newsem()
        self.cnt += 1
        inst.then_inc(self.sem, 1)
        return Ev(self.sem, self.cnt, id(self.sem))


class FW:
    SAME_ENGINE_SYNC = True

    def __init__(self, nc, es):
        self.nc, self.es = nc, es
        self.nsem = 0
        self.pe = Eng(self, "pe", nc.tensor, True)
        self.act = Eng(self, "act", nc.scalar)
        self.dve = Eng(self, "dve", nc.vector)
        self.pool = Eng(self, "pool", nc.gpsimd)
        self.sp = Eng(self, "sp", nc.sync)
        self.engs = [self.pe, self.act, self.dve, self.pool, self.sp]
        self.slots = {}
        for q in ("sp", "pool"):
            self.slots[q] = [[self.newsem("dq%s" % q), 0] for _ in range(12)]
        self.slot_i = {"sp": 0, "pool": 0}
        self.banks = []
        self.bank_i = 0
        self.bank_ids = list(range(8))
        self.ninst = 0
        self.rec = None

    def newsem(self, name):
        self.nsem += 1
        return self.es.enter_context(self.nc.semaphore("s%s%d" % (name, self.nsem)))

    def wait(self, eng, ev):
        if eng.seen.get(ev.key, 0) >= ev.val:
            return
        eng.obj.wait_ge(ev.sem, ev.val)
        eng.seen[ev.key] = ev.val
        self.ninst += 1

    def _w(self, eng, ev, raw, is_dma):
        if ev.key in eng.ownkeys and not is_dma:
            if eng.is_pe or not raw or not self.SAME_ENGINE_SYNC:
                return
        self.wait(eng, ev)

    def deps(self, eng, reads, writes, is_dma=False):
        for r in reads:
            for ev in r.w.values():
                self._w(eng, ev, True, is_dma)
        for w in writes:
            if not w.multi:
                for ev in w.w.values():
                    self._w(eng, ev, True, is_dma)
            for ev in w.r.values():
                self._w(eng, ev, False, is_dma)

    def done(self, ev, reads, writes):
        for r in reads:
            old = r.r.get(ev.key)
            if old is None or old.val < ev.val:
                r.r[ev.key] = ev
        for w in writes:
            if w.multi:
                w.w[ev.key] = ev
            else:
                w.w = {ev.key: ev}
            w.r = {}

    def op(self, eng, fn, reads=(), writes=(), **kw):
        if self.rec is not None:
            self.rec.append((self._op, (eng, fn, reads, writes), kw))
            return None
        return self._op(eng, fn, reads, writes, **kw)

    def pe_group(self, mms, reads=(), writes=(), transpose=False):
        if self.rec is not None:
            self.rec.append((self._pe_group, (mms, reads, writes, transpose), {}))
            return None
        return self._pe_group(mms, reads, writes, transpose)

    def dma(self, q, out, in_, reads=(), writes=(), sem=None):
        if self.rec is not None:
            self.rec.append((self._dma, (q, out, in_, reads, writes, sem), {}))
            return None
        return self._dma(q, out, in_, reads, writes, sem)

    def _op(self, eng, fn, reads=(), writes=(), **kw):
        self.deps(eng, reads, writes)
        inst = getattr(eng.obj, fn)(**kw)
        ev = eng.signal(inst)
        self.done(ev, reads, writes)
        self.ninst += 1
        return ev

    def _pe_group(self, mms, reads=(), writes=(), transpose=False):
        eng = self.pe
        self.deps(eng, reads, writes)
        inst = None
        for m in mms:
            if transpose:
                inst = self.nc.tensor.transpose(out=m[0], in_=m[1], identity=m[2])
            else:
                inst = self.nc.tensor.matmul(m[0], lhsT=m[1], rhs=m[2], start=m[3], stop=m[4])
            self.ninst += 1
        ev = eng.signal(inst)
        self.done(ev, reads, writes)
        return ev

    def _dma(self, q, out, in_, reads=(), writes=(), sem=None):
        eng = self.sp if q == "sp" else self.pool
        if sem is None:
            sl = self.slots[q][self.slot_i[q] % len(self.slots[q])]
            self.slot_i[q] += 1
            if sl[1] > 0:
                self.wait(eng, Ev(sl[0], sl[1], id(sl[0])))
        else:
            sl = sem
        self.deps(eng, reads, writes, is_dma=True)
        eng.obj.dma_start(out=out, in_=in_).then_inc(sl[0], 16)
        sl[1] += 16
        ev = Ev(sl[0], sl[1], id(sl[0]))
        self.done(ev, reads, writes)
        self.ninst += 1
        return ev

    def barrier(self):
        evs = []
        for e in self.engs:
            if e.cnt > 0:
                evs.append(Ev(e.sem, e.cnt, id(e.sem)))
        for q in self.slots:
            for sl in self.slots[q]:
                if sl[1] > 0:
                    evs.append(Ev(sl[0], sl[1], id(sl[0])))
        for e in self.engs:
            for ev in evs:
                if ev.key in e.ownkeys:
                    continue
                self.wait(e, ev)

    def bank(self):
        i = self.bank_ids[self.bank_i % len(self.bank_ids)]
        self.bank_i += 1
        return self.banks[i]


class Scope:
    def __init__(self, fw):
        self.fw = fw

    def __enter__(self):
        self.fw.barrier()
        self.es = contextlib.ExitStack()
        self.es.__enter__()
        self.n = 0
        return self

    def sb(self, shape, dtype, name="t"):
        self.n += 1
        self.fw.nsem += 1
        t = self.es.enter_context(self.fw.nc.sbuf_tensor("%s_%d_%d" % (name, self.fw.nsem, self.n), list(shape), dtype))
        return t, Res(name)

    def __exit__(self, *a):
        self.fw.barrier()
        self.es.__exit__(*a)
        return False


class Ring:
    def __init__(self, items):
        self.items, self.i = items, 0

    def next(self):
        it = self.items[self.i % len(self.items)]
        self.i += 1
        return it


def make_consts():
    cols = {}
    parts = []
    off = 0

    def add(name, arr):
        nonlocal off
        a = np.zeros((128, arr.shape[1]), np.float32)
        a[: arr.shape[0]] = arr
        cols[name] = (off, arr.shape[1])
        parts.append(a)
        off += arr.shape[1]

    add("ident", np.eye(128, dtype=np.float32))
    s = np.arange(64)[:, None]
    t = np.arange(64)[None, :]
    add("triM", np.where(s <= t, -1.0 / 16, 0.0).astype(np.float32))
    add("triM2", np.where(s > t, -1.0 / 16, 0.0).astype(np.float32))
    add("chunkind", np.full((64, 2), -1.0 / 16, np.float32))
    add("maskG", np.where(s <= t, 1.0, 0.0).astype(np.float32))
    j = np.arange(128)[:, None]
    s2 = np.arange(128)[None, :]
    add("triNeg", np.where(j >= s2, -1.0, 0.0).astype(np.float32))
    add("onesNeg", np.full((128, 128), -1.0, np.float32))
    tl = np.arange(512)[None, :]
    for k in range(4):
        valid = tl > (j + 128 * k)
        add("m01_%d" % k, valid.astype(np.float32))
    for k in range(4):
        valid = tl > (j + 128 * k)
        add("mneg_%d" % k, np.where(valid, 0.0, NEG).astype(np.float32))
    add("pow2", np.tile((0.5 ** np.arange(1, 33))[None, :], (128, 1)).astype(np.float32))
    return np.concatenate(parts, axis=1), cols


CONSTS, CCOL = make_consts()
NCC = CONSTS.shape[1]

OFF_QA, OFF_CA, OFF_QI, OFF_KI, OFF_WI, OFF_QB, OFF_KB, OFF_VB, OFF_GB, OFF_GK = 0, 1024, 1152, 2176, 2240, 2256, 2768, 3280, 4304, 5328
NFM0 = 19
TMW0 = 3216


def build(S, NSEQ, debug=False, stop=None):
    TOPK = min(256, S // 4)
    NT = S // 128
    G = 512
    NG = S // G
    nc = bass.Bass("TRN2", target_bir_lowering=False)
    es = contextlib.ExitStack()
    fw = FW(nc, es)
    dbgkind = "ExternalOutput" if debug else "Internal"

    def din(name, shape, dt=F32):
        return nc.dram_tensor(name, list(shape), dt, kind="ExternalInput").ap()

    def dscr(name, shape, dt):
        return nc.dram_tensor(name, list(shape), dt, kind=dbgkind).ap()

    x = din("x", [NSEQ, S, D])
    p = din("p", [2, NSEQ, S, PLE])
    gains = din("gains", [7, 128, D])
    smalls = din("smalls", [64, 256])
    cst = din("cst", [128, NCC])
    wgk = din("wgk", [17, 512])
    wsrc = {
        "fm0": din("wfm0", [D, NFM0 * 128]), "tm0": din("wtm0", [D, TMW0]), "out0": din("wout0", [D, D]),
        "in1": din("win1", [D, 3 * D]), "out1": din("wout1", [D, D]),
    }
    for i in range(2):
        wsrc["g%d" % i] = din("wg%d" % i, [D, FFH])
        wsrc["u%d" % i] = din("wu%d" % i, [D, FFH])
        wsrc["d%d" % i] = din("wd%d" % i, [FFH, D])
        wsrc["pg%d" % i] = din("wpg%d" % i, [D, D])
        wsrc["pe%d" % i] = din("wpe%d" % i, [PLE, D])
    out = nc.dram_tensor("out", [NSEQ, S, D], F32, kind="ExternalOutput").ap()

    wb = {}
    wres = {}
    for k, a in wsrc.items():
        wb[k] = nc.dram_tensor("wb_" + k, list(a.shape), BF16, kind="Internal").ap()
        wres[k] = Res("w" + k, multi=True)
    h = dscr("h", [NSEQ, S, D], F32)
    fm0 = dscr("fm0", [NSEQ, NFM0, 128, S], BF16)
    tm0 = dscr("tm0", [NSEQ, S, TMW0], F32)
    o0 = dscr("o0", [NSEQ, S, D], BF16)
    fm1 = dscr("fm1", [NSEQ, 32, 128, S], BF16)
    tm1 = dscr("tm1", [NSEQ, S, D], BF16)
    oT1 = dscr("oT1", [NSEQ, 16, 128, S], BF16)
    R = {k: [Res(k + str(i), multi=True) for i in range(NSEQ)] for k in ("h", "fm0", "tm0", "o0", "fm1", "tm1", "oT1", "out")}

    if debug:
        dbg_score = dscr("dbg_score", [NT, 128, S], F32)
        dbg_mask = dscr("dbg_mask", [NT, 128, S], BF16)
        dbg_r = Res("dbg", multi=True)
    for i in range(8):
        t = es.enter_context(nc.psum_tensor("bank%d" % i, [128, 512], F32))
        fw.banks.append((t, Res("bank%d" % i)))

    cs = es.enter_context(nc.sbuf_tensor("cs", [128, NCC], BF16))
    cs_res = Res("cs")
    ident = None

    def C(name, rows=128):
        o, n = CCOL[name]
        return cs[0:rows, o:o + n]

    fw.dma("pool", out=cs[:], in_=cst[:, :], writes=[cs_res])
    conv_done = set()
    conv_sems = {}

    def conv_pieces(keys):
        out_ = []
        for k in keys:
            if k in conv_done:
                continue
            conv_done.add(k)
            a = wsrc[k]
            conv_sems[k] = [fw.newsem("cv" + k), 0]
            rows = a.shape[0]
            rb = 256
            for r0 in range(0, rows, rb):
                r1 = min(rows, r0 + rb)
                out_.append((k, r0, r1))
        return out_

    def conv_issue(piece):
        k, r0, r1 = piece
        fw.dma("pool", out=wb[k][r0:r1, :], in_=wsrc[k][r0:r1, :], writes=[wres[k]], sem=conv_sems[k])

    def convert(keys):
        for pc in conv_pieces(keys):
            conv_issue(pc)

    convert(["fm0", "tm0"])

    def load_gain(sc_t, sc_res, idx):
        fw.dma("sp", out=sc_t[:], in_=gains[idx, :, :], writes=[sc_res])

    class Dense:
        def __init__(self, sc, evs=True, small=False):
            self.hres, self.hres_r = [], []
            for i in range(4):
                t, r = sc.sb([128, D], F32, "hres")
                self.hres.append(t)
                self.hres_r.append(r)
            self.gain, self.gain_r = sc.sb([128, D], F32, "gain")
            if small:
                self.xn = Ring([sc.sb([128, D], BF16, "xn") for _ in range(1)])
                self.junk = Ring(list(self.xn.items))
            else:
                self.xn = Ring([sc.sb([128, D], BF16, "xn") for _ in range(2)])
                self.junk = Ring([sc.sb([128, D], BF16, "junk") for _ in range(1)])
            self.ss = Ring([sc.sb([128, 2], F32, "ss") for _ in range(4)])
            self.xnT, self.xnT_r = sc.sb([128, 16, G], BF16, "xnT")
            self.wbuf = Ring([sc.sb([128, 16, 512], BF16, "wbuf") for _ in range(2 if small else 3)])
            if evs:
                self.ev32 = Ring([sc.sb([128, 512], F32, "ev32") for _ in range(3)])
                self.ev16 = Ring([sc.sb([128, 512], BF16, "ev16") for _ in range(3)])

    def rms_rstd(dn, src, src_r, nparts, width, ss, ss_r, junk, junk_r):
        fw.op(fw.dve, "memset", writes=[ss_r], ap=ss[0:nparts, 0:1], constant=0.0)
        fw.op(fw.act, "activation", reads=[src_r], writes=[junk_r, ss_r], out=junk, in_=src, func=AF.Square,
              accum_out=ss[0:nparts, 0:1])
        fw.op(fw.dve, "tensor_scalar", reads=[], writes=[ss_r], out=ss[0:nparts, 0:1], in0=ss[0:nparts, 0:1],
              scalar1=1.0 / width, scalar2=EPS, op0=ALU.mult, op1=ALU.add)
        fw.op(fw.act, "sqrt", reads=[], writes=[ss_r], out=ss[0:nparts, 0:1], in_=ss[0:nparts, 0:1])
        fw.op(fw.dve, "reciprocal", reads=[], writes=[ss_r], out=ss[0:nparts, 0:1], in_=ss[0:nparts, 0:1])

    tog = [0]

    def evac_copy(out_ap, in_ap, reads, writes):
        tog[0] ^= 1
        if tog[0]:
            fw.op(fw.act, "copy", reads=reads, writes=writes, out=out_ap, in_=in_ap)
        else:
            fw.op(fw.dve, "tensor_copy", reads=reads, writes=writes, out=out_ap, in_=in_ap)

    def transpose_to(dn, src_bf, src_r, dstT, dst_r, col0):
        for half in range(2):
            bt, br = fw.bank()
            pt = bt[:].bitcast(BF16)
            mms = []
            for j in range(8):
                kc = half * 8 + j
                mms.append((pt[:, j * 128:(j + 1) * 128], src_bf[:, kc * 128:(kc + 1) * 128], C("ident")))
            fw.pe_group(mms, reads=[src_r, cs_res], writes=[br], transpose=True)
            evac_copy(dstT[:, half * 8:half * 8 + 8, col0:col0 + 128],
                      pt[:, 0:1024].rearrange("p (j c) -> p j c", j=8), [br], [dst_r])

    def norm_tile(dn, src, src_r, dstT, dst_r, col0):
        ss, ss_r = dn.ss.next()
        junk, junk_r = dn.junk.next()
        xn, xn_r = dn.xn.next()
        rms_rstd(dn, src[:], src_r, 128, D, ss, ss_r, junk[:], junk_r)
        fw.op(fw.dve, "scalar_tensor_tensor", reads=[src_r, ss_r, dn.gain_r], writes=[xn_r], out=xn[:], in0=src[:],
              scalar=ss[:, 0:1], in1=dn.gain[:], op0=ALU.mult, op1=ALU.mult)
        transpose_to(dn, xn, xn_r, dstT, dst_r, col0)

    def load_w(dn, key, r0, nkc, c0, cw):
        wt, wr = dn.wbuf.next()
        fw.dma("sp", out=wt[:, 0:nkc, 0:cw],
               in_=wb[key][r0:r0 + nkc * 128, c0:c0 + cw].rearrange("(kc p) c -> p kc c", p=128),
               reads=[wres[key]], writes=[wr])
        return wt, wr

    def proj_tm(dn, aT, aT_r, KC, KT, key, N, ntt, epilogue, c_start=0, r_off=0):
        nkt = KC // KT
        for c0 in range(c_start, N, 512):
            cw = min(512, N - c0)
            if nkt == 1:
                wt, wr = load_w(dn, key, r_off, KT, c0, cw)
                for tt in range(ntt):
                    bt, br = fw.bank()
                    mms = [(bt[:, 0:cw], aT[:, kc, tt * 128:(tt + 1) * 128], wt[:, kc, 0:cw], kc == 0, kc == KT - 1) for kc in range(KT)]
                    fw.pe_group(mms, reads=[aT_r, wr], writes=[br])
                    epilogue(c0, cw, tt, bt, br)
                continue
            bks = [fw.bank() for _ in range(ntt)]
            for kt in range(nkt):
                wt, wr = load_w(dn, key, r_off + kt * KT * 128, KT, c0, cw)
                for tt in range(ntt):
                    bt, br = bks[tt]
                    mms = [(bt[:, 0:cw], aT[:, kt * KT + kc, tt * 128:(tt + 1) * 128], wt[:, kc, 0:cw],
                            kt == 0 and kc == 0, kt == nkt - 1 and kc == KT - 1) for kc in range(KT)]
                    fw.pe_group(mms, reads=[aT_r, wr], writes=[br])
            for tt in range(ntt):
                epilogue(c0, cw, tt, bks[tt][0], bks[tt][1])

    def proj_fm(dn, aT, aT_r, KC, key, nchunks, T, epilogue, chunk0=0):
        for cb in range(chunk0, nchunks, 4):
            ncb = min(4, nchunks - cb)
            wt, wr = load_w(dn, key, 0, KC, cb * 128, ncb * 128)
            for cc in range(ncb):
                bt, br = fw.bank()
                mms = [(bt[:, 0:T], wt[:, kc, cc * 128:(cc + 1) * 128], aT[:, kc, 0:T], kc == 0, kc == KC - 1)
                       for kc in range(KC)]
                fw.pe_group(mms, reads=[aT_r, wr], writes=[br])
                epilogue(cb + cc, bt, br)

    def phase_inproj0(sc, b):
        if True:
            dn = Dense(sc, small=(len(fw.cur_banks) < 8))
            load_gain(dn.gain, dn.gain_r, 0)
            for g in range(NG):
                for tt in range(4):
                    fw.dma("sp", out=dn.hres[tt][:], in_=x[b, g * G + tt * 128:g * G + (tt + 1) * 128, :],
                           writes=[dn.hres_r[tt]])
                    norm_tile(dn, dn.hres[tt], dn.hres_r[tt], dn.xnT, dn.xnT_r, tt * 128)

                def ep_fm(chunk, bt, br):
                    st, sr = dn.ev16.next()
                    evac_copy(st[:, 0:G], bt[:, 0:G], [br], [sr])
                    fw.dma("pool", out=fm0[b, chunk, :, g * G:(g + 1) * G], in_=st[:, 0:G], reads=[sr], writes=[R["fm0"][b]])
                proj_fm(dn, dn.xnT, dn.xnT_r, 16, "fm0", NFM0, G, ep_fm)

                def ep_tm(c0, cw, tt, bt, br):
                    st, sr = dn.ev32.next()
                    evac_copy(st[:, 0:cw], bt[:, 0:cw], [br], [sr])
                    fw.dma("pool", out=tm0[b, g * G + tt * 128:g * G + (tt + 1) * 128, c0:c0 + cw], in_=st[:, 0:cw],
                           reads=[sr], writes=[R["tm0"][b]])
                proj_tm(dn, dn.xnT, dn.xnT_r, 16, 16, "tm0", TMW0, 4, ep_tm)

    def phase_dsa(sc, b):
        if True:
            qiT, qiT_r = sc.sb([128, 8, S], BF16, "qiT")
            kiT, kiT_r = sc.sb([128, S], BF16, "kiT")
            qaT, qaT_r = sc.sb([128, 8, S], BF16, "qaT")
            cT, cT_r = sc.sb([128, S], BF16, "cT")
            ctm, ctm_r = sc.sb([128, NT, 130], BF16, "ctm")
            wi, wi_r = sc.sb([128, NT, 16], F32, "wi")
            id32, id32_r = sc.sb([128, 128], F32, "id32")
            scores = Ring([sc.sb([128, S], F32, "score") for _ in range(4)])
            NIT = 26
            bis = Ring([(sc.sb([128, NIT + 2], F32, "thr"), sc.sb([128, NIT + 2], F32, "Dst"), sc.sb([128, NIT + 2], F32, "sums"),
                         sc.sb([128, 4], F32, "bmisc")) for _ in range(2)])
            sjunk = Ring([sc.sb([128, S], BF16, "sjunk") for _ in range(2)])
            dgs = Ring([sc.sb([128, 16, 128], BF16, "dg") for _ in range(2)])
            mx = Ring([sc.sb([128, 8], F32, "mx") for _ in range(2)])
            relu = Ring([sc.sb([128, 512], BF16, "relu") for _ in range(4)])
            masks = Ring([sc.sb([128, S], BF16, "mask") for _ in range(2)])
            maskT, maskT_r = sc.sb([128, NT, 128], BF16, "maskT")
            Eb = Ring([sc.sb([128, 4, 128], BF16, "E") for _ in range(4)])
            Pb = Ring([sc.sb([128, 4, 128], BF16, "P") for _ in range(4)])
            att_i = [0]
            rden, rden_r = sc.sb([128, 8], F32, "rden")
            oa = Ring([sc.sb([128, 1024], BF16, "oa") for _ in range(3)])
            rd = [R["fm0"][b]]
            fw.dma("sp", out=qiT[:], in_=fm0[b, 9:17].rearrange("c p s -> p c s"), reads=rd, writes=[qiT_r])
            fw.dma("sp", out=kiT[:], in_=fm0[b, 17], reads=rd, writes=[kiT_r])
            fw.dma("sp", out=wi[:], in_=tm0[b, :, 128:144].rearrange("(j p) c -> p j c", p=128),
                   reads=[R["tm0"][b]], writes=[wi_r])
            o_id, n_id = CCOL["ident"]
            fw.dma("sp", out=id32[:], in_=cst[:, o_id:o_id + n_id], writes=[id32_r])
            fw.dma("sp", out=qaT[:], in_=fm0[b, 0:8].rearrange("c p s -> p c s"), reads=rd, writes=[qaT_r])
            fw.dma("sp", out=cT[:], in_=fm0[b, 8], reads=rd, writes=[cT_r])
            fw.op(fw.dve, "memset", writes=[ctm_r], ap=ctm[:, :, 128:130], constant=1.0)
            fw.dma("pool", out=ctm[:, :, 0:128], in_=tm0[b, :, 0:128].rearrange("(j p) c -> p j c", p=128),
                   reads=[R["tm0"][b]], writes=[ctm_r])
            pieces = conv_pieces(["out0", "g0", "u0", "d0", "pg0", "pe0", "in1", "out1", "g1", "u1", "d1", "pg1", "pe1"])
            per_blk = -(-len(pieces) // max(1, NT - 1))
            fw.bank_ids = [6, 7]

            def indexer(n):
                t0 = n * 128
                L = t0 + 128
                score, score_r = scores.next()
                dg, dg_r = dgs.next()
                fw.op(fw.dve, "tensor_tensor", reads=[id32_r, wi_r], writes=[dg_r], out=dg[:],
                      in0=id32[:, :].unsqueeze(1).to_broadcast([128, 16, 128]),
                      in1=wi[:, n, :].unsqueeze(2).to_broadcast([128, 16, 128]), op=ALU.mult)
                sb_, sb_r = fw.banks[5]
                for s0 in range(0, L, 512):
                    wn = min(512, L - s0)
                    pend = None
                    for hh in range(17):
                        cur = None
                        if hh < 16:
                            m, e = hh // 2, hh % 2
                            bt, br = fw.bank()
                            fw.pe_group([(bt[:, 0:wn], qiT[64 * e:64 * e + 64, m, t0:t0 + 128], kiT[64 * e:64 * e + 64, s0:s0 + wn],
                                          True, True)], reads=[qiT_r, kiT_r], writes=[br])
                            rl, rl_r = relu.next()
                            if hh % 3 != 2:
                                fw.op(fw.act, "activation", reads=[br], writes=[rl_r], out=rl[:, 0:wn], in_=bt[:, 0:wn], func=AF.Relu)
                            else:
                                fw.op(fw.dve, "tensor_scalar", reads=[br], writes=[rl_r], out=rl[:, 0:wn], in0=bt[:, 0:wn],
                                      scalar1=0.0, scalar2=None, op0=ALU.max)
                            cur = (hh, rl, rl_r)
                        if pend is not None:
                            ph, prl, prl_r = pend
                            fw.pe_group([(sb_[:, 0:wn], dg[:, ph, :], prl[:, 0:wn], ph == 0, ph == 15)], reads=[dg_r, prl_r], writes=[sb_r])
                        pend = cur
                    fw.op(fw.act, "copy", reads=[sb_r], writes=[score_r], out=score[:, s0:s0 + wn], in_=sb_[:, 0:wn])
                return score, score_r

            def select_group(blks):
                outm = {}
                todo = []
                for (n, score, score_r) in blks:
                    L = n * 128 + 128
                    mask, mask_r = masks.next()
                    outm[n] = (mask, mask_r)
                    fw.op(fw.dve, "memset", writes=[score_r], ap=score[0:64, L - 64:L], constant=-BIG)
                    if L > TOPK:
                        (thr, thr_r), (Dt, D_r), (sums, sums_r), (bm, bm_r) = bis.next()
                        m8, m8_r = mx.next()
                        fw.op(fw.dve, "max", reads=[score_r], writes=[m8_r], out=m8[:], in_=score[:, 0:L])
                        fw.op(fw.dve, "tensor_reduce", reads=[score_r], writes=[bm_r], out=bm[:, 0:1], in_=score[:, 0:L - 64],
                              axis=mybir.AxisListType.X, op=ALU.min)
                        fw.op(fw.dve, "tensor_tensor", reads=[m8_r], writes=[bm_r], out=bm[:, 1:2], in0=m8[:, 0:1], in1=bm[:, 0:1], op=ALU.subtract)
                        fw.op(fw.dve, "tensor_scalar", reads=[bm_r, cs_res], writes=[D_r], out=Dt[:, 0:NIT + 2], in0=C("pow2")[:, 0:NIT + 2],
                              scalar1=bm[:, 1:2], scalar2=None, op0=ALU.mult)
                        fw.op(fw.dve, "memset", writes=[sums_r], ap=sums[:], constant=0.0)
                        fw.op(fw.dve, "tensor_tensor", reads=[bm_r, D_r], writes=[thr_r], out=thr[:, 0:1], in0=bm[:, 0:1], in1=Dt[:, 0:1], op=ALU.add)
                        todo.append((n, L, score, score_r, mask, mask_r, thr, thr_r, Dt, D_r, sums, sums_r, bm, bm_r))
                    else:
                        fw.op(fw.dve, "memset", writes=[mask_r], ap=mask[:, 0:L], constant=1.0)
                        fw.op(fw.dve, "memset", writes=[mask_r], ap=mask[0:64, L - 64:L], constant=0.0)
                for i in range(NIT):
                    for (n, L, score, score_r, mask, mask_r, thr, thr_r, Dt, D_r, sums, sums_r, bm, bm_r) in todo:
                        jk, jk_r = sjunk.next()
                        fw.op(fw.act, "activation", reads=[score_r, thr_r], writes=[jk_r, sums_r], out=jk[:, 0:L], in_=score[:, 0:L],
                              func=AF.Sign, scale=-1.0, bias=thr[:, i:i + 1], accum_out=sums[:, i:i + 1])
                    for (n, L, score, score_r, mask, mask_r, thr, thr_r, Dt, D_r, sums, sums_r, bm, bm_r) in todo:
                        fw.op(fw.dve, "tensor_scalar", reads=[sums_r], writes=[bm_r], out=bm[:, 2:3], in0=sums[:, i:i + 1],
                              scalar1=float(L - 2 * TOPK), scalar2=0.5, op0=ALU.is_le, op1=ALU.subtract)
                        fw.op(fw.dve, "scalar_tensor_tensor", reads=[bm_r, D_r], writes=[thr_r], out=thr[:, i + 1:i + 2], in0=Dt[:, i:i + 1],
                              scalar=bm[:, 2:3], in1=thr[:, i:i + 1], op0=ALU.mult, op1=ALU.add)
                for (n, L, score, score_r, mask, mask_r, thr, thr_r, Dt, D_r, sums, sums_r, bm, bm_r) in todo:
                    fw.op(fw.dve, "tensor_scalar", reads=[score_r, thr_r, D_r], writes=[mask_r], out=mask[:, 0:L], in0=score[:, 0:L],
                          scalar1=Dt[:, NIT:NIT + 1], scalar2=thr[:, NIT:NIT + 1], op0=ALU.add, op1=ALU.is_ge)
                if debug:
                    for (n, score, score_r) in blks:
                        mask, mask_r = outm[n]
                        fw.dma("pool", out=dbg_score[n, :, :], in_=score[:, :], reads=[score_r], writes=[dbg_r])
                        fw.dma("pool", out=dbg_mask[n, :, :], in_=mask[:, :], reads=[mask_r], writes=[dbg_r])
                return outm

            def attend(n, mask, mask_r):
                t0 = n * 128
                for j0 in range(0, n + 1, 8):
                    nj = min(8, n + 1 - j0)
                    bt, br = fw.bank()
                    pt = bt[:].bitcast(BF16)
                    fw.pe_group([(pt[:, jj * 128:(jj + 1) * 128], mask[:, (j0 + jj) * 128:(j0 + jj + 1) * 128], C("ident"))
                                 for jj in range(nj)], reads=[mask_r, cs_res], writes=[br], transpose=True)
                    evac_copy(maskT[:, j0:j0 + nj, :], pt[:, 0:nj * 128].rearrange("p (j c) -> p j c", j=nj), [br], [maskT_r])
                oat, oat_r = oa.next()
                for half in range(2):
                    stt = {}

                    def s1_(j):
                        E, E_r = Eb.next()
                        P, P_r = Pb.next()
                        bt, br = fw.banks[4 + (att_i[0] % 2)]
                        att_i[0] += 1
                        fw.pe_group([(bt[:, :].rearrange("p (h t) -> p h t", h=4), cT[:, j * 128:(j + 1) * 128], qaT[:, half * 4:half * 4 + 4, t0:t0 + 128], True, True)],
                                    reads=[cT_r, qaT_r], writes=[br])
                        fw.op(fw.act, "activation", reads=[br], writes=[E_r], out=E[:],
                              in_=bt[:, :].rearrange("p (h t) -> p h t", h=4), func=AF.Exp, scale=float(128 ** -0.5))
                        fw.op(fw.dve, "tensor_tensor", reads=[E_r, maskT_r], writes=[P_r],
                              out=P[:], in0=E[:], in1=maskT[:, j:j + 1, :].to_broadcast([128, 4, 128]), op=ALU.mult)
                        stt[j] = (P, P_r)

                    def s2_(j):
                        P, P_r = stt.pop(j)
                        for h4 in range(4):
                            ob, ob_r = fw.banks[h4]
                            fw.pe_group([(ob[:, 0:130], P[:, h4, :], ctm[:, j, :], j == 0, j == n)],
                                        reads=[P_r, ctm_r], writes=[ob_r])

                    for j in range(n + 1 + 2):
                        if j <= n:
                            s1_(j)
                        if 0 <= j - 2 <= n:
                            s2_(j - 2)
                    for h4 in range(4):
                        hd = half * 4 + h4
                        ob, ob_r = fw.banks[h4]
                        fw.op(fw.dve, "reciprocal", reads=[ob_r], writes=[rden_r], out=rden[:, hd:hd + 1], in_=ob[:, 128:129])
                        fw.op(fw.act, "mul", reads=[ob_r, rden_r], writes=[oat_r], out=oat[:, hd * 128:(hd + 1) * 128],
                              in_=ob[:, 0:128], mul=rden[:, hd:hd + 1])
                fw.dma("pool", out=o0[b, t0:t0 + 128, 0:1024], in_=oat[:], reads=[oat_r], writes=[R["o0"][b]])

            GS = 2
            groups = [list(range(g0, min(NT, g0 + GS))) for g0 in range(0, NT, GS)]
            pend = [(n,) + tuple(indexer(n)) for n in groups[0]]
            for gi, grp in enumerate(groups):
                cur = pend
                if gi + 1 < len(groups):
                    pend = [(n,) + tuple(indexer(n)) for n in groups[gi + 1]]
                for _ in range(per_blk * len(grp)):
                    if pieces:
                        conv_issue(pieces.pop(0))
                ms = select_group(cur)
                for n in grp:
                    attend(n, *ms[n])
            while pieces:
                conv_issue(pieces.pop(0))

    def phase_gla(sc, b):
        NCH = S // 64
        if True:
            Sf, Sf_r = sc.sb([128, 4, 256], F32, "Sf")
            Sb, Sb_r = sc.sb([128, 4, 256], BF16, "Sb")
            wg, wg_r = sc.sb([32, 512], BF16, "wgk")
            gn, gn_r = sc.sb([64, 256], F32, "gn")
            gkT, gkT_r = sc.sb([32, S], BF16, "gkT")
            qk = Ring([sc.sb([64, 1024], F32, "qk") for _ in range(2)])
            vb = Ring([sc.sb([64, 1024], BF16, "vb") for _ in range(2)])
            gb = Ring([sc.sb([64, 1024], F32, "gb") for _ in range(2)])
            e1 = Ring([sc.sb([64, 512], F32, "e1") for _ in range(1)])
            sp_ = Ring([sc.sb([64, 512], BF16, "sp") for _ in range(2)])
            eG = Ring([sc.sb([64, 3, 512], F32, "eG") for _ in range(1)])
            dec = Ring([sc.sb([128, 4], F32, "dec") for _ in range(2)])
            qkd = Ring([sc.sb([64, 3, 512], BF16, "qkd") for _ in range(2)])
            qkT = Ring([sc.sb([128, 8, 64], BF16, "qkT") for _ in range(2)])
            attm = Ring([sc.sb([64, 4, 64], BF16, "attm") for _ in range(2)])
            ssr = Ring([sc.sb([64, 4], F32, "ssr") for _ in range(2)])
            junk = Ring([sc.sb([64, 256], BF16, "gjunk") for _ in range(2)])
            on = Ring([sc.sb([64, 1024], F32, "on") for _ in range(1)])
            sg = Ring([sc.sb([64, 1024], F32, "sg") for _ in range(1)])
            ob = Ring([sc.sb([64, 1024], BF16, "ob") for _ in range(1)])
            fw.op(fw.dve, "memset", writes=[Sf_r], ap=Sf[:], constant=0.0)
            fw.op(fw.dve, "memset", writes=[Sb_r], ap=Sb[:], constant=0.0)
            fw.op(fw.dve, "memset", writes=[gkT_r], ap=gkT[:], constant=1.0)
            fw.dma("pool", out=wg[0:17, :], in_=wgk[:, :], writes=[wg_r])
            fw.dma("sp", out=gn[:], in_=smalls[:, :], writes=[gn_r])
            fw.dma("sp", out=gkT[0:16, :], in_=fm0[b, 18, 0:16, :], reads=[R["fm0"][b]], writes=[gkT_r])
            sc_q = float(128 ** -0.5)
            for ci in range(NCH):
                t0 = ci * 64
                qkt, qkt_r = qk.next()
                vbt, vbt_r = vb.next()
                gbt, gbt_r = gb.next()
                fw.dma("sp", out=qkt[:], in_=tm0[b, t0:t0 + 64, 144:1168], reads=[R["tm0"][b]], writes=[qkt_r])
                fw.dma("pool", out=vbt[:], in_=tm0[b, t0:t0 + 64, 1168:2192], reads=[R["tm0"][b]], writes=[vbt_r])
                fw.dma("sp", out=gbt[:], in_=tm0[b, t0:t0 + 64, 2192:3216], reads=[R["tm0"][b]], writes=[gbt_r])
                bz, bz_r = fw.bank()
                fw.pe_group([(bz[0:64, :], gkT[0:17, t0:t0 + 64], wg[0:17, :], True, True)], reads=[gkT_r, wg_r], writes=[bz_r])
                e1t, e1_r = e1.next()
                spt, sp_r = sp_.next()
                fw.op(fw.act, "activation", reads=[bz_r], writes=[e1_r], out=e1t[:], in_=bz[0:64, :], func=AF.Exp, scale=-1.0)
                fw.op(fw.act, "activation", reads=[e1_r], writes=[sp_r], out=spt[:], in_=e1t[:], func=AF.Ln, bias=1.0)
                bG, bG_r = fw.bank()
                bD, bD_r = fw.bank()
                bL, bL_r = fw.bank()
                fw.pe_group([(bG[0:64, :], C("triM", 64), spt[:], True, True)], reads=[sp_r, cs_res], writes=[bG_r])
                fw.pe_group([(bD[0:64, :], C("triM2", 64), spt[:], True, True)], reads=[sp_r, cs_res], writes=[bD_r])
                fw.pe_group([(bL[:, hh * 2:hh * 2 + 2], spt[:, hh * 128:(hh + 1) * 128], C("chunkind", 64), True, True) for hh in range(4)],
                            reads=[sp_r, cs_res], writes=[bL_r])
                eGt, eG_r = eG.next()
                dct, dc_r = dec.next()
                fw.op(fw.act, "activation", reads=[bG_r], writes=[eG_r], out=eGt[:, 0, :], in_=bG[0:64, :], func=AF.Exp)
                fw.op(fw.act, "activation", reads=[bG_r], writes=[eG_r], out=eGt[:, 1, :], in_=bG[0:64, :], func=AF.Exp, scale=-1.0)
                fw.op(fw.act, "activation", reads=[bD_r], writes=[eG_r], out=eGt[:, 2, :], in_=bD[0:64, :], func=AF.Exp)
                fw.op(fw.act, "activation", reads=[bL_r], writes=[dc_r], out=dct[:, :], in_=bL[:, 0:8].rearrange("p (h t) -> p h t", t=2)[:, :, 0],
                      func=AF.Exp)
                qd, qd_r = qkd.next()
                fw.op(fw.dve, "scalar_tensor_tensor", reads=[qkt_r, eG_r], writes=[qd_r], out=qd[:, 0, :], in0=qkt[:, 0:512], scalar=sc_q,
                      in1=eGt[:, 0, :], op0=ALU.mult, op1=ALU.mult)
                fw.op(fw.dve, "tensor_tensor", reads=[qkt_r, eG_r], writes=[qd_r], out=qd[:, 1, :], in0=qkt[:, 512:1024], in1=eGt[:, 1, :], op=ALU.mult)
                fw.op(fw.pool, "tensor_tensor", reads=[qkt_r, eG_r], writes=[qd_r], out=qd[:, 2, :], in0=qkt[:, 512:1024], in1=eGt[:, 2, :], op=ALU.mult)
                bt, br = fw.bank()
                pt = bt[:].bitcast(BF16)
                fw.pe_group([(pt[:, (a * 4 + hh) * 64:(a * 4 + hh + 1) * 64], qd[:, a, hh * 128:(hh + 1) * 128], C("ident", 64)[:, 0:64])
                             for a in range(2) for hh in range(4)], reads=[qd_r, cs_res], writes=[br], transpose=True)
                qT, qT_r = qkT.next()
                evac_copy(qT[:], pt[:, 0:512].rearrange("p (j c) -> p j c", j=8), [br], [qT_r])
                ba, ba_r = fw.bank()
                fw.pe_group([(ba[0:64, hh * 64:(hh + 1) * 64], qT[:, 4 + hh, :], qT[:, hh, :], True, True) for hh in range(4)],
                            reads=[qT_r], writes=[ba_r])
                am, am_r = attm.next()
                for hh in range(4):
                    fw.op(fw.dve, "tensor_tensor", reads=[ba_r, cs_res], writes=[am_r], out=am[:, hh, :], in0=ba[0:64, hh * 64:(hh + 1) * 64],
                          in1=C("maskG", 64), op=ALU.mult)
                bo = [fw.bank(), fw.bank()]
                for hh in range(4):
                    bt2, br2 = bo[hh // 2]
                    cs0 = (hh % 2) * 256
                    fw.pe_group([(bt2[0:64, cs0:cs0 + 256], am[:, hh, :], vbt[:, hh * 256:(hh + 1) * 256], True, False),
                                 (bt2[0:64, cs0:cs0 + 256], qT[:, hh, :], Sb[:, hh, :], False, True)],
                                reads=[am_r, vbt_r, qT_r, Sb_r], writes=[br2])
                bkv = [fw.bank(), fw.bank()]
                for hh in range(4):
                    bt3, br3 = bkv[hh // 2]
                    cs0 = (hh % 2) * 256
                    fw.pe_group([(bt3[:, cs0:cs0 + 256], qd[:, 2, hh * 128:(hh + 1) * 128], vbt[:, hh * 256:(hh + 1) * 256], True, True)],
                                reads=[qd_r, vbt_r], writes=[br3])
                for hh in range(4):
                    bt3, br3 = bkv[hh // 2]
                    cs0 = (hh % 2) * 256
                    fw.op(fw.dve, "scalar_tensor_tensor", reads=[br3, dc_r], writes=[Sf_r], out=Sf[:, hh, :], in0=Sf[:, hh, :],
                          scalar=dct[:, hh:hh + 1], in1=bt3[:, cs0:cs0 + 256], op0=ALU.mult, op1=ALU.add)
                fw.op(fw.act, "copy", reads=[Sf_r], writes=[Sb_r], out=Sb[:], in_=Sf[:])
                sst, ss_r = ssr.next()
                fw.op(fw.dve, "memset", writes=[ss_r], ap=sst[:], constant=0.0)
                for hh in range(4):
                    bt2, br2 = bo[hh // 2]
                    cs0 = (hh % 2) * 256
                    jk, jk_r = junk.next()
                    fw.op(fw.act, "activation", reads=[br2], writes=[jk_r, ss_r], out=jk[:], in_=bt2[0:64, cs0:cs0 + 256], func=AF.Square,
                          accum_out=sst[:, hh:hh + 1])
                fw.op(fw.dve, "tensor_scalar", writes=[ss_r], out=sst[:], in0=sst[:], scalar1=1.0 / 256, scalar2=EPS, op0=ALU.mult, op1=ALU.add)
                fw.op(fw.act, "sqrt", writes=[ss_r], out=sst[:], in_=sst[:])
                fw.op(fw.dve, "reciprocal", writes=[ss_r], out=sst[:], in_=sst[:])
                ont, on_r = on.next()
                for hh in range(4):
                    bt2, br2 = bo[hh // 2]
                    cs0 = (hh % 2) * 256
                    fw.op(fw.dve, "scalar_tensor_tensor", reads=[br2, ss_r, gn_r], writes=[on_r], out=ont[:, hh * 256:(hh + 1) * 256],
                          in0=bt2[0:64, cs0:cs0 + 256], scalar=sst[:, hh:hh + 1], in1=gn[:], op0=ALU.mult, op1=ALU.mult)
                sgt, sg_r = sg.next()
                fw.op(fw.act, "activation", reads=[gbt_r], writes=[sg_r], out=sgt[:], in_=gbt[:], func=AF.Silu)
                obt, ob_r = ob.next()
                fw.op(fw.pool, "tensor_tensor", reads=[on_r, sg_r], writes=[ob_r], out=obt[:], in0=ont[:], in1=sgt[:], op=ALU.mult)
                fw.dma("pool", out=o0[b, t0:t0 + 64, 1024:2048], in_=obt[:], reads=[ob_r], writes=[R["o0"][b]])

    def phase_inproj1(sc, b):
        if True:
            convert(["in1", "out1", "g1", "u1", "d1", "pg1", "pe1"])
            dn = Dense(sc)
            load_gain(dn.gain, dn.gain_r, 1)
            sc_q = float(128 ** -0.5)
            for g in range(NG):
                for tt in range(4):
                    fw.dma("sp", out=dn.hres[tt][:], in_=h[b, g * G + tt * 128:g * G + (tt + 1) * 128, :],
                           reads=[R["h"][b]], writes=[dn.hres_r[tt]])
                    norm_tile(dn, dn.hres[tt], dn.hres_r[tt], dn.xnT, dn.xnT_r, tt * 128)

                def ep_fm(chunk, bt, br):
                    st, sr = dn.ev16.next()
                    if chunk < 16:
                        fw.op(fw.act, "mul", reads=[br], writes=[sr], out=st[:, 0:G], in_=bt[:, 0:G], mul=sc_q)
                    else:
                        evac_copy(st[:, 0:G], bt[:, 0:G], [br], [sr])
                    fw.dma("pool", out=fm1[b, chunk, :, g * G:(g + 1) * G], in_=st[:, 0:G], reads=[sr], writes=[R["fm1"][b]])
                proj_fm(dn, dn.xnT, dn.xnT_r, 16, "in1", 32, G, ep_fm)

                def ep_tm(c0, cw, tt, bt, br):
                    st, sr = dn.ev16.next()
                    evac_copy(st[:, 0:cw], bt[:, 0:cw], [br], [sr])
                    fw.dma("pool", out=tm1[b, g * G + tt * 128:g * G + (tt + 1) * 128, c0 - 2 * D:c0 - 2 * D + cw], in_=st[:, 0:cw],
                           reads=[sr], writes=[R["tm1"][b]])
                proj_tm(dn, dn.xnT, dn.xnT_r, 16, 16, "in1", 3 * D, 4, ep_tm, c_start=2 * D)

    def phase_sb(sc, b):
        QB = 512
        if True:
            qT = Ring([sc.sb([128, S], BF16, "qT") for _ in range(2)])
            kT = Ring([sc.sb([128, S], BF16, "kT") for _ in range(2)])
            vv = Ring([sc.sb([128, NT, 128], BF16, "vv") for _ in range(2)])
            Ebuf = Ring([sc.sb([128, 512], F32, "sbE") for _ in range(4)])
            Lp = Ring([sc.sb([128, 512], BF16, "Lp") for _ in range(3)])
            acc, acc_r = sc.sb([128, 512], BF16, "acc")
            Wt = Ring([sc.sb([128, 512], BF16, "Wt") for _ in range(3)])
            Xt = Ring([sc.sb([128, 512], BF16, "Xt") for _ in range(3)])
            ost = Ring([sc.sb([128, 512], BF16, "ost") for _ in range(2)])
            cb = list(fw.cur_banks)
            if len(cb) >= 8:
                b1_banks, b2_bank, bo_bank = [cb[0], cb[1], cb[2]], cb[3], cb[4]
            else:
                b1_banks, b2_bank, bo_bank = [cb[0], cb[1]], cb[2], cb[3]
            heads = {}

            def load_head(hd):
                if hd >= 16 or hd in heads:
                    return
                q_, q_r = qT.next()
                k_, k_r = kT.next()
                v_, v_r = vv.next()
                fw.dma("sp", out=q_[:], in_=fm1[b, hd], reads=[R["fm1"][b]], writes=[q_r])
                fw.dma("sp", out=k_[:], in_=fm1[b, 16 + hd], reads=[R["fm1"][b]], writes=[k_r])
                fw.dma("sp", out=v_[:], in_=tm1[b, :, hd * 128:(hd + 1) * 128].rearrange("(j p) c -> p j c", p=128),
                       reads=[R["tm1"][b]], writes=[v_r])
                heads[hd] = (q_, q_r, k_, k_r, v_, v_r)

            items = []
            for hd in range(16):
                for qb in range(S // QB):
                    for c in range(4 * qb + 3, -1, -1):
                        items.append((hd, qb, c))
            NI = len(items)
            st = {}

            def geo(it):
                hd, qb, c = it
                k_d = c - 4 * qb
                diag = k_d >= 0
                lo = 128 * k_d if diag else 0
                return hd, qb, c, qb * QB, 4 * qb + 3, k_d, diag, lo

            def stage_a(i):
                hd, qb, c, t0, cmax, k_d, diag, lo = geo(items[i])
                load_head(hd)
                q_, q_r, k_, k_r, v_, v_r = heads[hd]
                b1, b1_r = fw.banks[b1_banks[i % len(b1_banks)]]
                fw.pe_group([(b1[:, lo:QB], k_[:, c * 128:(c + 1) * 128], q_[:, t0 + lo:t0 + QB], True, True)], reads=[k_r, q_r], writes=[b1_r])
                Et, E_r = Ebuf.next()
                fw.op(fw.act, "activation", reads=[b1_r], writes=[E_r], out=Et[:, lo:QB], in_=b1[:, lo:QB], func=AF.Exp)
                Lt, L_r = Lp.next()
                fw.op(fw.act, "activation", reads=[E_r], writes=[L_r], out=Lt[:, lo:QB], in_=Et[:, lo:QB], func=AF.Ln, bias=1.0)
                if diag:
                    fw.op(fw.pool, "tensor_tensor", reads=[L_r, cs_res], writes=[L_r], out=Lt[:, lo:QB], in0=Lt[:, lo:QB],
                          in1=C("m01_%d" % k_d)[:, lo:QB], op=ALU.mult)
                st[i] = {"E": (Et, E_r), "L": (Lt, L_r)}

            def stage_b(i):
                hd, qb, c, t0, cmax, k_d, diag, lo = geo(items[i])
                Lt, L_r = st[i]["L"]
                b2, b2_r = fw.banks[b2_bank]
                mms = [(b2[:, lo:QB], C("triNeg"), Lt[:, lo:QB], True, (c == cmax) and not diag)]
                rds = [L_r, cs_res]
                if c != cmax:
                    mms.append((b2[:, lo:QB], C("onesNeg"), acc[:, lo:QB], False, not diag))
                    rds.append(acc_r)
                if diag:
                    mms.append((b2[:, lo:QB], C("ident"), C("mneg_%d" % k_d)[:, lo:QB], False, True))
                fw.pe_group(mms, reads=rds, writes=[b2_r])
                if c == cmax:
                    fw.op(fw.dve, "memset", writes=[acc_r], ap=acc[:], constant=0.0)
                if c > 0:
                    fw.op(fw.dve, "tensor_tensor", reads=[L_r], writes=[acc_r], out=acc[:, lo:QB], in0=acc[:, lo:QB], in1=Lt[:, lo:QB], op=ALU.add)
                Xtt, X_r = Xt.next()
                fw.op(fw.act, "activation", reads=[b2_r], writes=[X_r], out=Xtt[:, lo:QB], in_=b2[:, lo:QB], func=AF.Exp)
                st[i]["X"] = (Xtt, X_r)

            def stage_c(i):
                hd, qb, c, t0, cmax, k_d, diag, lo = geo(items[i])
                Et, E_r = st[i]["E"]
                Xtt, X_r = st[i]["X"]
                Wtt, W_r = Wt.next()
                fw.op(fw.dve, "tensor_tensor", reads=[X_r, E_r], writes=[W_r], out=Wtt[:, lo:QB], in0=Xtt[:, lo:QB], in1=Et[:, lo:QB], op=ALU.mult)
                st[i]["W"] = (Wtt, W_r)

            def stage_d(i):
                hd, qb, c, t0, cmax, k_d, diag, lo = geo(items[i])
                q_, q_r, k_, k_r, v_, v_r = heads[hd]
                Wtt, W_r = st[i]["W"]
                bo, bo_r = fw.banks[bo_bank]
                fw.pe_group([(bo[:, lo:QB], v_[:, c, :], Wtt[:, lo:QB], c == cmax, c == 0)], reads=[v_r, W_r], writes=[bo_r])
                if c == 0:
                    st_, st_r = ost.next()
                    evac_copy(st_[:], bo[:, :], [bo_r], [st_r])
                    fw.dma("pool", out=oT1[b, hd, :, t0:t0 + QB], in_=st_[:], reads=[st_r], writes=[R["oT1"][b]])
                    load_head(hd + 1)
                del st[i]

            for i in range(NI + 3):
                if i < NI:
                    stage_a(i)
                if 0 <= i - 3 < NI:
                    stage_d(i - 3)
                if 0 <= i - 1 < NI:
                    stage_b(i - 1)
                if 0 <= i - 2 < NI:
                    stage_c(i - 2)

    def phase_post(sc, b, layer):
        if True:
            dn = Dense(sc, evs=False, small=True)
            h1T, h1T_r = sc.sb([128, 22, G], BF16, "h1T")
            otile = dn.xn
            sgl = Ring([sc.sb([128, 512], F32, "sgl") for _ in range(2)])
            pt32 = Ring([sc.sb([128, PLE], F32, "pt32") for _ in range(2)])
            pt16 = Ring([sc.sb([128, PLE], BF16, "pt16") for _ in range(2)])
            pT, pT_r = sc.sb([128, 2, G], BF16, "pT")
            wpeb = Ring([sc.sb([128, 2, 512], BF16, "wpe") for _ in range(2)])
            cur_wpe = [None]
            pe_sb = Ring([sc.sb([128, 512], F32, "pesb") for _ in range(1)])
            sig = Ring([sc.sb([128, 512], F32, "sig") for _ in range(1)])
            hsrc = x if layer == 0 else h
            for g in range(NG):
                tok0 = g * G
                if layer == 0:
                    for tt in range(4):
                        ot, ot_r = otile.next()
                        fw.dma("sp", out=ot[:], in_=o0[b, tok0 + tt * 128:tok0 + (tt + 1) * 128, :], reads=[R["o0"][b]], writes=[ot_r])
                        transpose_to(dn, ot, ot_r, dn.xnT, dn.xnT_r, tt * 128)
                else:
                    fw.dma("sp", out=dn.xnT[:], in_=oT1[b, :, :, tok0:tok0 + G].rearrange("c p s -> p c s"),
                           reads=[R["oT1"][b]], writes=[dn.xnT_r])
                for tt in range(4):
                    rds = [R["h"][b]] if layer == 1 else []
                    fw.dma("sp", out=dn.hres[tt][:], in_=hsrc[b, tok0 + tt * 128:tok0 + (tt + 1) * 128, :], reads=rds, writes=[dn.hres_r[tt]])

                def ep_res(c0, cw, tt, bt, br):
                    fw.op(fw.dve, "tensor_tensor", reads=[br], writes=[dn.hres_r[tt]], out=dn.hres[tt][:, c0:c0 + cw],
                          in0=dn.hres[tt][:, c0:c0 + cw], in1=bt[:, 0:cw], op=ALU.add)
                proj_tm(dn, dn.xnT, dn.xnT_r, 16, 16, "out%d" % layer, D, 4, ep_res)
                load_gain(dn.gain, dn.gain_r, 2 + layer)
                for tt in range(4):
                    norm_tile(dn, dn.hres[tt], dn.hres_r[tt], dn.xnT, dn.xnT_r, tt * 128)
                for half in range(2):
                    h0 = half * 22
                    for hb in range(h0, h0 + 22, 4):
                        nb = min(4, h0 + 22 - hb)
                        wgt, wg_r = load_w(dn, "g%d" % layer, 0, 16, hb * 128, nb * 128)
                        wut, wu_r = load_w(dn, "u%d" % layer, 0, 16, hb * 128, nb * 128)
                        for cc in range(nb):
                            bg, bg_r = fw.bank()
                            bu, bu_r = fw.bank()
                            fw.pe_group([(bg[:, 0:G], wgt[:, kc, cc * 128:(cc + 1) * 128], dn.xnT[:, kc, 0:G], kc == 0, kc == 15) for kc in range(16)],
                                        reads=[dn.xnT_r, wg_r], writes=[bg_r])
                            fw.pe_group([(bu[:, 0:G], wut[:, kc, cc * 128:(cc + 1) * 128], dn.xnT[:, kc, 0:G], kc == 0, kc == 15) for kc in range(16)],
                                        reads=[dn.xnT_r, wu_r], writes=[bu_r])
                            s_, s_r = sgl.next()
                            fw.op(fw.act, "activation", reads=[bg_r], writes=[s_r], out=s_[:, 0:G], in_=bg[:, 0:G], func=AF.Silu)
                            fw.op(fw.dve, "tensor_tensor", reads=[s_r, bu_r], writes=[h1T_r], out=h1T[:, hb - h0 + cc, :], in0=s_[:, 0:G], in1=bu[:, 0:G], op=ALU.mult)
                    proj_tm(dn, h1T, h1T_r, 22, 11, "d%d" % layer, D, 4, ep_res, r_off=h0 * 128)
                load_gain(dn.gain, dn.gain_r, 4 + layer)
                for tt in range(4):
                    norm_tile(dn, dn.hres[tt], dn.hres_r[tt], dn.xnT, dn.xnT_r, tt * 128)
                    p32, p32_r = pt32.next()
                    p16, p16_r = pt16.next()
                    fw.dma("sp", out=p32[:], in_=p[layer, b, tok0 + tt * 128:tok0 + (tt + 1) * 128, :], writes=[p32_r])
                    fw.op(fw.pool, "tensor_copy", reads=[p32_r], writes=[p16_r], out=p16[:], in_=p32[:])
                    bt, br = fw.bank()
                    ptb = bt[:].bitcast(BF16)
                    fw.pe_group([(ptb[:, j * 128:(j + 1) * 128], p16[:, j * 128:(j + 1) * 128], C("ident")) for j in range(2)],
                                reads=[p16_r, cs_res], writes=[br], transpose=True)
                    evac_copy(pT[:, :, tt * 128:(tt + 1) * 128], ptb[:, 0:256].rearrange("p (j c) -> p j c", j=2), [br], [pT_r])

                def ep_ple(c0, cw, tt, bt, br):
                    if tt == 0:
                        wpe_t, wpe_r = wpeb.next()
                        fw.dma("sp", out=wpe_t[:, :, 0:cw], in_=wb["pe%d" % layer][:, c0:c0 + cw].rearrange("(kc p) c -> p kc c", p=128),
                               reads=[wres["pe%d" % layer]], writes=[wpe_r])
                        cur_wpe[0] = (wpe_t, wpe_r)
                    wpe_t, wpe_r = cur_wpe[0]
                    b2, b2_r = fw.bank()
                    fw.pe_group([(b2[:, 0:cw], pT[:, kc, tt * 128:(tt + 1) * 128], wpe_t[:, kc, 0:cw], kc == 0, kc == 1) for kc in range(2)],
                                reads=[pT_r, wpe_r], writes=[b2_r])
                    sg_, sg_r = sig.next()
                    fw.op(fw.act, "activation", reads=[br], writes=[sg_r], out=sg_[:, 0:cw], in_=bt[:, 0:cw], func=AF.Sigmoid)
                    pe_, pe_r = pe_sb.next()
                    fw.op(fw.dve, "tensor_tensor", reads=[sg_r, b2_r], writes=[pe_r], out=pe_[:, 0:cw], in0=sg_[:, 0:cw], in1=b2[:, 0:cw], op=ALU.mult)
                    fw.op(fw.pool, "tensor_tensor", reads=[pe_r], writes=[dn.hres_r[tt]], out=dn.hres[tt][:, c0:c0 + cw],
                          in0=dn.hres[tt][:, c0:c0 + cw], in1=pe_[:, 0:cw], op=ALU.add)
                proj_tm(dn, dn.xnT, dn.xnT_r, 16, 16, "pg%d" % layer, D, 4, ep_ple)
                if layer == 0:
                    for tt in range(4):
                        fw.dma("pool", out=h[b, tok0 + tt * 128:tok0 + (tt + 1) * 128, :], in_=dn.hres[tt][:], reads=[dn.hres_r[tt]], writes=[R["h"][b]])
                else:
                    load_gain(dn.gain, dn.gain_r, 6)
                    for tt in range(4):
                        ss, ss_r = dn.ss.next()
                        junk, junk_r = dn.junk.next()
                        rms_rstd(dn, dn.hres[tt][:], dn.hres_r[tt], 128, D, ss, ss_r, junk[:], junk_r)
                        fw.op(fw.dve, "scalar_tensor_tensor", reads=[ss_r, dn.gain_r], writes=[dn.hres_r[tt]], out=dn.hres[tt][:], in0=dn.hres[tt][:],
                              scalar=ss[:, 0:1], in1=dn.gain[:], op0=ALU.mult, op1=ALU.mult)
                        fw.dma("pool", out=out[b, tok0 + tt * 128:tok0 + (tt + 1) * 128, :], in_=dn.hres[tt][:], reads=[dn.hres_r[tt]], writes=[R["out"][b]])

    ALL8 = list(range(8))

    def run_group(streams):
        with Scope(fw) as sc:
            recs = []
            for fn, args, banks in streams:
                fw.rec = []
                fw.bank_ids = list(banks)
                fw.cur_banks = list(banks)
                fw.bank_i = 0
                fn(sc, *args)
                recs.append(fw.rec)
                fw.rec = None
            pos = [0] * len(recs)
            while True:
                best, bf = None, None
                for i, r in enumerate(recs):
                    if pos[i] < len(r):
                        f = pos[i] / float(len(r))
                        if bf is None or f < bf:
                            best, bf = i, f
                if best is None:
                    break
                fnc, a, kw = recs[best][pos[best]]
                fnc(*a, **kw)
                pos[best] += 1
        fw.bank_ids = list(ALL8)

    sched = []
    if NSEQ == 1:
        sched = [("inproj0", [(phase_inproj0, (0,), ALL8)]), ("dsa", [(phase_dsa, (0,), ALL8)]), ("gla", [(phase_gla, (0,), ALL8)]),
                 ("post0", [(phase_post, (0, 0), ALL8)]), ("inproj1", [(phase_inproj1, (0,), ALL8)]), ("sb", [(phase_sb, (0,), ALL8)]),
                 ("post1", [(phase_post, (0, 1), ALL8)])]
    else:
        assert NSEQ == 2
        LO, HI = [0, 1, 2, 3], [4, 5, 6, 7]
        sched = [
            ("a", [(phase_inproj0, (0,), ALL8)]),
            ("b", [(phase_dsa, (0,), ALL8)]),
            ("c", [(phase_inproj0, (1,), LO), (phase_gla, (0,), HI)]),
            ("e", [(phase_post, (0, 0), LO), (phase_gla, (1,), HI)]),
            ("f", [(phase_dsa, (1,), ALL8)]),
            ("g", [(phase_inproj1, (0,), ALL8)]),
            ("h", [(phase_post, (1, 0), LO), (phase_sb, (0,), HI)]),
            ("i", [(phase_inproj1, (1,), ALL8)]),
            ("j", [(phase_post, (0, 1), LO), (phase_sb, (1,), HI)]),
            ("k", [(phase_post, (1, 1), ALL8)]),
        ]
    for name, streams in sched:
        run_group(streams)
        if stop is not None and name == stop:
            break
    fw.barrier()
    es.close()
    return nc


def prep_weights(inp):
    f = lambda a: np.ascontiguousarray(np.asarray(a, dtype=np.float32))
    w0 = f(inp["even_w_in"])[0]
    wfm0 = np.zeros((D, NFM0 * 128), np.float32)
    wfm0[:, 0:1024] = w0[:, OFF_QA:OFF_QA + 1024]
    wfm0[:, 1024:1152] = w0[:, OFF_CA:OFF_CA + 128]
    wfm0[:, 1152:2176] = w0[:, OFF_QI:OFF_QI + 1024]
    wfm0[:, 2176:2240] = w0[:, OFF_KI:OFF_KI + 64]
    wfm0[:, 2240:2304] = w0[:, OFF_KI:OFF_KI + 64]
    wfm0[:, 2304:2320] = w0[:, OFF_GK:OFF_GK + 16]
    wtm0 = np.concatenate([w0[:, OFF_CA:OFF_CA + 128], w0[:, OFF_WI:OFF_WI + 16], w0[:, OFF_QB:OFF_GK]], axis=1)
    assert wtm0.shape[1] == TMW0
    gains = np.stack([f(inp["even_norm"])[0], f(inp["odd_norm"])[0], f(inp["ffn_norm"])[0], f(inp["ffn_norm"])[1],
                      f(inp["ple_norm"])[0], f(inp["ple_norm"])[1], f(inp["final_norm"])], axis=0)
    gains = np.ascontiguousarray(np.broadcast_to(gains[:, None, :], (7, 128, D)))
    smalls = np.ascontiguousarray(np.broadcast_to(f(inp["even_gla_norm"])[0][None, :], (64, 256)))
    wgk = np.concatenate([f(inp["even_w_gk"])[0], f(inp["even_b_gk"])[0][None, :]], axis=0)
    m = {"gains": gains, "smalls": smalls, "cst": CONSTS, "wgk": np.ascontiguousarray(wgk),
         "wfm0": wfm0, "wtm0": np.ascontiguousarray(wtm0), "wout0": f(inp["even_w_out"])[0],
         "win1": f(inp["odd_w_in"])[0], "wout1": f(inp["odd_w_out"])[0]}
    for i in range(2):
        m["wg%d" % i] = f(inp["ffn_w_gate"])[i]
        m["wu%d" % i] = f(inp["ffn_w_up"])[i]
        m["wd%d" % i] = f(inp["ffn_w_down"])[i]
        m["wpg%d" % i] = f(inp["ple_w_gate"])[i]
        m["wpe%d" % i] = f(inp["ple_w_proj"])[i]
    return m


_CACHE = {}


def kernel(**inputs):
    x = np.asarray(inputs["x"], dtype=np.float32)
    p = np.asarray(inputs["p"], dtype=np.float32)
    B, S, _ = x.shape
    ncores = 8
    nseq = B // ncores
    wm = prep_weights(inputs)
    key = (S, nseq)
    if key not in _CACHE:
        _CACHE[key] = build(S, nseq)
    nc = _CACHE[key]
    in_maps = []
    for c in range(ncores):
        m = dict(wm)
        m["x"] = np.ascontiguousarray(x[c * nseq:(c + 1) * nseq])
        m["p"] = np.ascontiguousarray(p[:, c * nseq:(c + 1) * nseq])
        in_maps.append(m)
    res = run_bass_kernel_spmd(nc, in_maps, core_ids=list(range(ncores)))
    return np.concatenate([r["out"] for r in res.results], axis=0).astype(np.float32)
```

```python
import contextlib
import numpy as np
import concourse.bass as bass
import concourse.mybir as mybir
from concourse.bass_utils import run_bass_kernel_spmd

F32 = mybir.dt.float32
BF16 = mybir.dt.bfloat16
AF = mybir.ActivationFunctionType
ALU = mybir.AluOpType

D = 2048
FFH = 5632
PLE = 256
EPS = 1e-6
NEG = -30000.0
BIG = 1.0e30


class Ev:
    __slots__ = ("sem", "val", "key")

    def __init__(self, sem, val, key):
        self.sem, self.val, self.key = sem, val, key


class Res:
    __slots__ = ("w", "r", "name", "multi")

    def __init__(self, name="", multi=False):
        self.w = {}
        self.r = {}
        self.name = name
        self.multi = multi


class Eng:
    def __init__(self, fw, name, obj, is_pe=False):
        self.fw, self.name, self.obj, self.is_pe = fw, name, obj, is_pe
        self.seen = {}
        self.ownkeys = set()
        self._newsem()

    def _newsem(self):
        self.sem = self.fw.newsem(self.name)
        self.ownkeys.add(id(self.sem))
        self.cnt = 0

    def signal(self, inst):
        if self.cnt >= 30000:
            self._newsem()
        self.cnt += 1
        inst.then_inc(self.sem, 1)
        return Ev(self.sem, self.cnt, id(self.sem))


class FW:
    SAME_ENGINE_SYNC = True

    def __init__(self, nc, es):
        self.nc, self.es = nc, es
        self.nsem = 0
        self.pe = Eng(self, "pe", nc.tensor, True)
        self.act = Eng(self, "act", nc.scalar)
        self.dve = Eng(self, "dve", nc.vector)
        self.pool = Eng(self, "pool", nc.gpsimd)
        self.sp = Eng(self, "sp", nc.sync)
        self.engs = [self.pe, self.act, self.dve, self.pool, self.sp]
        self.slots = {}
        for q in ("sp", "pool"):
            self.slots[q] = [[self.newsem("dq%s" % q), 0] for _ in range(12)]
        self.slot_i = {"sp": 0, "pool": 0}
        self.banks = []
        self.bank_i = 0
        self.bank_ids = list(range(8))
        self.ninst = 0
        self.rec = None

    def newsem(self, name):
        self.nsem += 1
        return self.es.enter_context(self.nc.semaphore("s%s%d" % (name, self.nsem)))

    def wait(self, eng, ev):
        if eng.seen.get(ev.key, 0) >= ev.val:
            return
        eng.obj.wait_ge(ev.sem, ev.val)
        eng.seen[ev.key] = ev.val
        self.ninst += 1

    def _w(self, eng, ev, raw, is_dma):
        if ev.key in eng.ownkeys and not is_dma:
            if eng.is_pe or not raw or not self.SAME_ENGINE_SYNC:
                return
        self.wait(eng, ev)

    def deps(self, eng, reads, writes, is_dma=False):
        for r in reads:
            for ev in r.w.values():
                self._w(eng, ev, True, is_dma)
        for w in writes:
            if not w.multi:
                for ev in w.w.values():
                    self._w(eng, ev, True, is_dma)
            for ev in w.r.values():
                self._w(eng, ev, False, is_dma)

    def done(self, ev, reads, writes):
        for r in reads:
            old = r.r.get(ev.key)
            if old is None or old.val < ev.val:
                r.r[ev.key] = ev
        for w in writes:
            if w.multi:
                w.w[ev.key] = ev
            else:
                w.w = {ev.key: ev}
            w.r = {}

    def op(self, eng, fn, reads=(), writes=(), **kw):
        if self.rec is not None:
            self.rec.append((self._op, (eng, fn, reads, writes), kw))
            return None
        return self._op(eng, fn, reads, writes, **kw)

    def pe_group(self, mms, reads=(), writes=(), transpose=False):
        if self.rec is not None:
            self.rec.append((self._pe_group, (mms, reads, writes, transpose), {}))
            return None
        return self._pe_group(mms, reads, writes, transpose)

    def dma(self, q, out, in_, reads=(), writes=(), sem=None):
        if self.rec is not None:
            self.rec.append((self._dma, (q, out, in_, reads, writes, sem), {}))
            return None
        return self._dma(q, out, in_, reads, writes, sem)

    def _op(self, eng, fn, reads=(), writes=(), **kw):
        self.deps(eng, reads, writes)
        inst = getattr(eng.obj, fn)(**kw)
        ev = eng.signal(inst)
        self.done(ev, reads, writes)
        self.ninst += 1
        return ev

    def _pe_group(self, mms, reads=(), writes=(), transpose=False):
        eng = self.pe
        self.deps(eng, reads, writes)
        inst = None
        for m in mms:
            if transpose:
                inst = self.nc.tensor.transpose(out=m[0], in_=m[1], identity=m[2])
            else:
                inst = self.nc.tensor.matmul(m[0], lhsT=m[1], rhs=m[2], start=m[3], stop=m[4])
            self.ninst += 1
        ev = eng.signal(inst)
        self.done(ev, reads, writes)
        return ev

    def _dma(self, q, out, in_, reads=(), writes=(), sem=None):
        eng = self.sp if q == "sp" else self.pool
        if sem is None:
            sl = self.slots[q][self.slot_i[q] % len(self.slots[q])]
            self.slot_i[q] += 1
            if sl[1] > 0:
                self.wait(eng, Ev(sl[0], sl[1], id(sl[0])))
        else:
            sl = sem
        self.deps(eng, reads, writes, is_dma=True)
        eng.obj.dma_start(out=out, in_=in_).then_inc(sl[0], 16)
        sl[1] += 16
        ev = Ev(sl[0], sl[1], id(sl[0]))
        self.done(ev, reads, writes)
        self.ninst += 1
        return ev

    def barrier(self):
        evs = []
        for e in self.engs:
            if e.cnt > 0:
                evs.append(Ev(e.sem, e.cnt, id(e.sem)))
        for q in self.slots:
            for sl in self.slots[q]:
                if sl[1] > 0:
                    evs.append(Ev(sl[0], sl[1], id(sl[0])))
        for e in self.engs:
            for ev in evs:
                if ev.key in e.ownkeys:
                    continue
                self.wait(e, ev)

    def bank(self):
        i = self.bank_ids[self.bank_i % len(self.bank_ids)]
        self.bank_i += 1
        return self.banks[i]


class Scope:
    def __init__(self, fw):
        self.fw = fw

    def __enter__(self):
        self.fw.barrier()
        self.es = contextlib.ExitStack()
        self.es.__enter__()
        self.n = 0
        return self

    def sb(self, shape, dtype, name="t"):
        self.n += 1
        self.fw.nsem += 1
        t = self.es.enter_context(self.fw.nc.sbuf_tensor("%s_%d_%d" % (name, self.fw.nsem, self.n), list(shape), dtype))
        return t, Res(name)

    def __exit__(self, *a):
        self.fw.barrier()
        self.es.__exit__(*a)
        return False


class Ring:
    def __init__(self, items):
        self.items, self.i = items, 0

    def next(self):
        it = self.items[self.i % len(self.items)]
        self.i += 1
        return it


def make_consts():
    cols = {}
    parts = []
    off = 0

    def add(name, arr):
        nonlocal off
        a = np.zeros((128, arr.shape[1]), np.float32)
        a[: arr.shape[0]] = arr
        cols[name] = (off, arr.shape[1])
        parts.append(a)
        off += arr.shape[1]

    add("ident", np.eye(128, dtype=np.float32))
    s = np.arange(64)[:, None]
    t = np.arange(64)[None, :]
    add("triM", np.where(s <= t, -1.0 / 16, 0.0).astype(np.float32))
    add("triM2", np.where(s > t, -1.0 / 16, 0.0).astype(np.float32))
    add("chunkind", np.full((64, 2), -1.0 / 16, np.float32))
    add("maskG", np.where(s <= t, 1.0, 0.0).astype(np.float32))
    j = np.arange(128)[:, None]
    s2 = np.arange(128)[None, :]
    add("triNeg", np.where(j >= s2, -1.0, 0.0).astype(np.float32))
    add("onesNeg", np.full((128, 128), -1.0, np.float32))
    tl = np.arange(512)[None, :]
    for k in range(4):
        valid = tl > (j + 128 * k)
        add("m01_%d" % k, valid.astype(np.float32))
    for k in range(4):
        valid = tl > (j + 128 * k)
        add("mneg_%d" % k, np.where(valid, 0.0, NEG).astype(np.float32))
    add("pow2", np.tile((0.5 ** np.arange(1, 33))[None, :], (128, 1)).astype(np.float32))
    return np.concatenate(parts, axis=1), cols


CONSTS, CCOL = make_consts()
NCC = CONSTS.shape[1]

OFF_QA, OFF_CA, OFF_QI, OFF_KI, OFF_WI, OFF_QB, OFF_KB, OFF_VB, OFF_GB, OFF_GK = 0, 1024, 1152, 2176, 2240, 2256, 2768, 3280, 4304, 5328
NFM0 = 19
TMW0 = 3216


def build(S, NSEQ, debug=False, stop=None):
    TOPK = min(256, S // 4)
    NT = S // 128
    G = 512
    NG = S // G
    nc = bass.Bass("TRN2", target_bir_lowering=False)
    es = contextlib.ExitStack()
    fw = FW(nc, es)
    dbgkind = "ExternalOutput" if debug else "Internal"

    def din(name, shape, dt=F32):
        return nc.dram_tensor(name, list(shape), dt, kind="ExternalInput").ap()

    def dscr(name, shape, dt):
        return nc.dram_tensor(name, list(shape), dt, kind=dbgkind).ap()

    x = din("x", [NSEQ, S, D])
    p = din("p", [2, NSEQ, S, PLE])
    gains = din("gains", [7, 128, D])
    smalls = din("smalls", [64, 256])
    cst = din("cst", [128, NCC])
    wgk = din("wgk", [17, 512])
    wsrc = {
        "fm0": din("wfm0", [D, NFM0 * 128]), "tm0": din("wtm0", [D, TMW0]), "out0": din("wout0", [D, D]),
        "in1": din("win1", [D, 3 * D]), "out1": din("wout1", [D, D]),
    }
    for i in range(2):
        wsrc["g%d" % i] = din("wg%d" % i, [D, FFH])
        wsrc["u%d" % i] = din("wu%d" % i, [D, FFH])
        wsrc["d%d" % i] = din("wd%d" % i, [FFH, D])
        wsrc["pg%d" % i] = din("wpg%d" % i, [D, D])
        wsrc["pe%d" % i] = din("wpe%d" % i, [PLE, D])
    out = nc.dram_tensor("out", [NSEQ, S, D], F32, kind="ExternalOutput").ap()

    wb = {}
    wres = {}
    for k, a in wsrc.items():
        wb[k] = nc.dram_tensor("wb_" + k, list(a.shape), BF16, kind="Internal").ap()
        wres[k] = Res("w" + k, multi=True)
    h = dscr("h", [NSEQ, S, D], F32)
    fm0 = dscr("fm0", [NSEQ, NFM0, 128, S], BF16)
    tm0 = dscr("tm0", [NSEQ, S, TMW0], F32)
    o0 = dscr("o0", [NSEQ, S, D], BF16)
    fm1 = dscr("fm1", [NSEQ, 32, 128, S], BF16)
    tm1 = dscr("tm1", [NSEQ, S, D], BF16)
    oT1 = dscr("oT1", [NSEQ, 16, 128, S], BF16)
    R = {k: [Res(k + str(i), multi=True) for i in range(NSEQ)] for k in ("h", "fm0", "tm0", "o0", "fm1", "tm1", "oT1", "out")}

    if debug:
        dbg_score = dscr("dbg_score", [NT, 128, S], F32)
        dbg_mask = dscr("dbg_mask", [NT, 128, S], BF16)
        dbg_r = Res("dbg", multi=True)
    for i in range(8):
        t = es.enter_context(nc.psum_tensor("bank%d" % i, [128, 512], F32))
        fw.banks.append((t, Res("bank%d" % i)))

    cs = es.enter_context(nc.sbuf_tensor("cs", [128, NCC], BF16))
    cs_res = Res("cs")
    ident = None

    def C(name, rows=128):
        o, n = CCOL[name]
        return cs[0:rows, o:o + n]

    fw.dma("pool", out=cs[:], in_=cst[:, :], writes=[cs_res])
    conv_done = set()
    conv_sems = {}

    def conv_pieces(keys):
        out_ = []
        for k in keys:
            if k in conv_done:
                continue
            conv_done.add(k)
            a = wsrc[k]
            conv_sems[k] = [fw.newsem("cv" + k), 0]
            rows = a.shape[0]
            rb = 256
            for r0 in range(0, rows, rb):
                r1 = min(rows, r0 + rb)
                out_.append((k, r0, r1))
        return out_

    def conv_issue(piece):
        k, r0, r1 = piece
        fw.dma("pool", out=wb[k][r0:r1, :], in_=wsrc[k][r0:r1, :], writes=[wres[k]], sem=conv_sems[k])

    def convert(keys):
        for pc in conv_pieces(keys):
            conv_issue(pc)

    convert(["fm0", "tm0"])

    def load_gain(sc_t, sc_res, idx):
        fw.dma("sp", out=sc_t[:], in_=gains[idx, :, :], writes=[sc_res])

    class Dense:
        def __init__(self, sc, evs=True, small=False):
            self.hres, self.hres_r = [], []
            for i in range(4):
                t, r = sc.sb([128, D], F32, "hres")
                self.hres.append(t)
                self.hres_r.append(r)
            self.gain, self.gain_r = sc.sb([128, D], F32, "gain")
            if small:
                self.xn = Ring([sc.sb([128, D], BF16, "xn") for _ in range(2)])
                self.junk = Ring(list(self.xn.items))
            else:
                self.xn = Ring([sc.sb([128, D], BF16, "xn") for _ in range(2)])
                self.junk = Ring([sc.sb([128, D], BF16, "junk") for _ in range(1)])
            self.ss = Ring([sc.sb([128, 2], F32, "ss") for _ in range(4)])
            self.xnT, self.xnT_r = sc.sb([128, 16, G], BF16, "xnT")
            self.wbuf = Ring([sc.sb([128, 16, 512], BF16, "wbuf") for _ in range(2 if small else 3)])
            if evs:
                self.ev32 = Ring([sc.sb([128, 512], F32, "ev32") for _ in range(3)])
                self.ev16 = Ring([sc.sb([128, 512], BF16, "ev16") for _ in range(3)])

    def rms_rstd(dn, src, src_r, nparts, width, ss, ss_r, junk, junk_r):
        fw.op(fw.dve, "memset", writes=[ss_r], ap=ss[0:nparts, 0:1], constant=0.0)
        fw.op(fw.act, "activation", reads=[src_r], writes=[junk_r, ss_r], out=junk, in_=src, func=AF.Square,
              accum_out=ss[0:nparts, 0:1])
        fw.op(fw.dve, "tensor_scalar", reads=[], writes=[ss_r], out=ss[0:nparts, 0:1], in0=ss[0:nparts, 0:1],
              scalar1=1.0 / width, scalar2=EPS, op0=ALU.mult, op1=ALU.add)
        fw.op(fw.act, "sqrt", reads=[], writes=[ss_r], out=ss[0:nparts, 0:1], in_=ss[0:nparts, 0:1])
        fw.op(fw.dve, "reciprocal", reads=[], writes=[ss_r], out=ss[0:nparts, 0:1], in_=ss[0:nparts, 0:1])

    tog = [0]

    def evac_copy(out_ap, in_ap, reads, writes):
        tog[0] ^= 1
        if tog[0]:
            fw.op(fw.act, "copy", reads=reads, writes=writes, out=out_ap, in_=in_ap)
        else:
            fw.op(fw.dve, "tensor_copy", reads=reads, writes=writes, out=out_ap, in_=in_ap)

    def transpose_to(dn, src_bf, src_r, dstT, dst_r, col0):
        for half in range(2):
            bt, br = fw.bank()
            pt = bt[:].bitcast(BF16)
            mms = []
            for j in range(8):
                kc = half * 8 + j
                mms.append((pt[:, j * 128:(j + 1) * 128], src_bf[:, kc * 128:(kc + 1) * 128], C("ident")))
            fw.pe_group(mms, reads=[src_r, cs_res], writes=[br], transpose=True)
            evac_copy(dstT[:, half * 8:half * 8 + 8, col0:col0 + 128],
                      pt[:, 0:1024].rearrange("p (j c) -> p j c", j=8), [br], [dst_r])

    def norm_tile(dn, src, src_r, dstT, dst_r, col0):
        ss, ss_r = dn.ss.next()
        junk, junk_r = dn.junk.next()
        xn, xn_r = dn.xn.next()
        rms_rstd(dn, src[:], src_r, 128, D, ss, ss_r, junk[:], junk_r)
        fw.op(fw.dve, "scalar_tensor_tensor", reads=[src_r, ss_r, dn.gain_r], writes=[xn_r], out=xn[:], in0=src[:],
              scalar=ss[:, 0:1], in1=dn.gain[:], op0=ALU.mult, op1=ALU.mult)
        transpose_to(dn, xn, xn_r, dstT, dst_r, col0)

    def load_w(dn, key, r0, nkc, c0, cw):
        wt, wr = dn.wbuf.next()
        fw.dma("sp", out=wt[:, 0:nkc, 0:cw],
               in_=wb[key][r0:r0 + nkc * 128, c0:c0 + cw].rearrange("(kc p) c -> p kc c", p=128),
               reads=[wres[key]], writes=[wr])
        return wt, wr

    def proj_tm(dn, aT, aT_r, KC, KT, key, N, ntt, epilogue, c_start=0, r_off=0):
        nkt = KC // KT
        for c0 in range(c_start, N, 512):
            cw = min(512, N - c0)
            if nkt == 1:
                wt, wr = load_w(dn, key, r_off, KT, c0, cw)
                for tt in range(ntt):
                    bt, br = fw.bank()
                    mms = [(bt[:, 0:cw], aT[:, kc, tt * 128:(tt + 1) * 128], wt[:, kc, 0:cw], kc == 0, kc == KT - 1) for kc in range(KT)]
                    fw.pe_group(mms, reads=[aT_r, wr], writes=[br])
                    epilogue(c0, cw, tt, bt, br)
                continue
            bks = [fw.bank() for _ in range(ntt)]
            for kt in range(nkt):
                wt, wr = load_w(dn, key, r_off + kt * KT * 128, KT, c0, cw)
                for tt in range(ntt):
                    bt, br = bks[tt]
                    mms = [(bt[:, 0:cw], aT[:, kt * KT + kc, tt * 128:(tt + 1) * 128], wt[:, kc, 0:cw],
                            kt == 0 and kc == 0, kt == nkt - 1 and kc == KT - 1) for kc in range(KT)]
                    fw.pe_group(mms, reads=[aT_r, wr], writes=[br])
            for tt in range(ntt):
                epilogue(c0, cw, tt, bks[tt][0], bks[tt][1])

    def proj_fm(dn, aT, aT_r, KC, key, nchunks, T, epilogue, chunk0=0):
        for cb in range(chunk0, nchunks, 4):
            ncb = min(4, nchunks - cb)
            wt, wr = load_w(dn, key, 0, KC, cb * 128, ncb * 128)
            for cc in range(ncb):
                bt, br = fw.bank()
                mms = [(bt[:, 0:T], wt[:, kc, cc * 128:(cc + 1) * 128], aT[:, kc, 0:T], kc == 0, kc == KC - 1)
                       for kc in range(KC)]
                fw.pe_group(mms, reads=[aT_r, wr], writes=[br])
                epilogue(cb + cc, bt, br)

    def phase_inproj0(sc, b):
        if True:
            dn = Dense(sc, small=(len(fw.cur_banks) < 8))
            load_gain(dn.gain, dn.gain_r, 0)
            for g in range(NG):
                for tt in range(4):
                    fw.dma("sp", out=dn.hres[tt][:], in_=x[b, g * G + tt * 128:g * G + (tt + 1) * 128, :],
                           writes=[dn.hres_r[tt]])
                    norm_tile(dn, dn.hres[tt], dn.hres_r[tt], dn.xnT, dn.xnT_r, tt * 128)

                def ep_fm(chunk, bt, br):
                    st, sr = dn.ev16.next()
                    evac_copy(st[:, 0:G], bt[:, 0:G], [br], [sr])
                    fw.dma("pool", out=fm0[b, chunk, :, g * G:(g + 1) * G], in_=st[:, 0:G], reads=[sr], writes=[R["fm0"][b]])
                proj_fm(dn, dn.xnT, dn.xnT_r, 16, "fm0", NFM0, G, ep_fm)

                def ep_tm(c0, cw, tt, bt, br):
                    st, sr = dn.ev32.next()
                    evac_copy(st[:, 0:cw], bt[:, 0:cw], [br], [sr])
                    fw.dma("pool", out=tm0[b, g * G + tt * 128:g * G + (tt + 1) * 128, c0:c0 + cw], in_=st[:, 0:cw],
                           reads=[sr], writes=[R["tm0"][b]])
                proj_tm(dn, dn.xnT, dn.xnT_r, 16, 16, "tm0", TMW0, 4, ep_tm)

    def phase_dsa(sc, b):
        if True:
            qiT, qiT_r = sc.sb([128, 8, S], BF16, "qiT")
            kiT, kiT_r = sc.sb([128, S], BF16, "kiT")
            qaT, qaT_r = sc.sb([128, 8, S], BF16, "qaT")
            cT, cT_r = sc.sb([128, S], BF16, "cT")
            ctm, ctm_r = sc.sb([128, NT, 130], BF16, "ctm")
            wi, wi_r = sc.sb([128, NT, 16], F32, "wi")
            id32, id32_r = sc.sb([128, 128], F32, "id32")
            scores = Ring([sc.sb([128, S], F32, "score") for _ in range(4)])
            NIT = 26
            bis = Ring([(sc.sb([128, NIT + 2], F32, "thr"), sc.sb([128, NIT + 2], F32, "Dst"), sc.sb([128, NIT + 2], F32, "sums"),
                         sc.sb([128, 4], F32, "bmisc")) for _ in range(2)])
            sjunk = Ring([sc.sb([128, S], BF16, "sjunk") for _ in range(2)])
            dgs = Ring([sc.sb([128, 16, 128], BF16, "dg") for _ in range(2)])
            mx = Ring([sc.sb([128, 8], F32, "mx") for _ in range(2)])
            relu = Ring([sc.sb([128, 512], BF16, "relu") for _ in range(4)])
            masks = Ring([sc.sb([128, S], BF16, "mask") for _ in range(2)])
            maskT, maskT_r = sc.sb([128, NT, 128], BF16, "maskT")
            Eb = Ring([sc.sb([128, 4, 128], BF16, "E") for _ in range(4)])
            Pb = Ring([sc.sb([128, 4, 128], BF16, "P") for _ in range(4)])
            att_i = [0]
            rden, rden_r = sc.sb([128, 8], F32, "rden")
            oa = Ring([sc.sb([128, 1024], BF16, "oa") for _ in range(3)])
            rd = [R["fm0"][b]]
            fw.dma("sp", out=qiT[:], in_=fm0[b, 9:17].rearrange("c p s -> p c s"), reads=rd, writes=[qiT_r])
            fw.dma("sp", out=kiT[:], in_=fm0[b, 17], reads=rd, writes=[kiT_r])
            fw.dma("sp", out=wi[:], in_=tm0[b, :, 128:144].rearrange("(j p) c -> p j c", p=128),
                   reads=[R["tm0"][b]], writes=[wi_r])
            o_id, n_id = CCOL["ident"]
            fw.dma("sp", out=id32[:], in_=cst[:, o_id:o_id + n_id], writes=[id32_r])
            fw.dma("sp", out=qaT[:], in_=fm0[b, 0:8].rearrange("c p s -> p c s"), reads=rd, writes=[qaT_r])
            fw.dma("sp", out=cT[:], in_=fm0[b, 8], reads=rd, writes=[cT_r])
            fw.op(fw.dve, "memset", writes=[ctm_r], ap=ctm[:, :, 128:130], constant=1.0)
            fw.dma("pool", out=ctm[:, :, 0:128], in_=tm0[b, :, 0:128].rearrange("(j p) c -> p j c", p=128),
                   reads=[R["tm0"][b]], writes=[ctm_r])
            pieces = conv_pieces(["out0", "g0", "u0", "d0", "pg0", "pe0", "in1", "out1", "g1", "u1", "d1", "pg1", "pe1"])
            per_blk = -(-len(pieces) // max(1, NT - 1))
            fw.bank_ids = [6, 7]

            def indexer(n):
                t0 = n * 128
                L = t0 + 128
                score, score_r = scores.next()
                dg, dg_r = dgs.next()
                fw.op(fw.dve, "tensor_tensor", reads=[id32_r, wi_r], writes=[dg_r], out=dg[:],
                      in0=id32[:, :].unsqueeze(1).to_broadcast([128, 16, 128]),
                      in1=wi[:, n, :].unsqueeze(2).to_broadcast([128, 16, 128]), op=ALU.mult)
                sb_, sb_r = fw.banks[5]
                for s0 in range(0, L, 512):
                    wn = min(512, L - s0)
                    pend = None
                    for hh in range(17):
                        cur = None
                        if hh < 16:
                            m, e = hh // 2, hh % 2
                            bt, br = fw.bank()
                            fw.pe_group([(bt[:, 0:wn], qiT[64 * e:64 * e + 64, m, t0:t0 + 128], kiT[64 * e:64 * e + 64, s0:s0 + wn],
                                          True, True)], reads=[qiT_r, kiT_r], writes=[br])
                            rl, rl_r = relu.next()
                            if hh % 3 != 2:
                                fw.op(fw.act, "activation", reads=[br], writes=[rl_r], out=rl[:, 0:wn], in_=bt[:, 0:wn], func=AF.Relu)
                            else:
                                fw.op(fw.dve, "tensor_scalar", reads=[br], writes=[rl_r], out=rl[:, 0:wn], in0=bt[:, 0:wn],
                                      scalar1=0.0, scalar2=None, op0=ALU.max)
                            cur = (hh, rl, rl_r)
                        if pend is not None:
                            ph, prl, prl_r = pend
                            fw.pe_group([(sb_[:, 0:wn], dg[:, ph, :], prl[:, 0:wn], ph == 0, ph == 15)], reads=[dg_r, prl_r], writes=[sb_r])
                        pend = cur
                    fw.op(fw.act, "copy", reads=[sb_r], writes=[score_r], out=score[:, s0:s0 + wn], in_=sb_[:, 0:wn])
                return score, score_r

            def select_group(blks):
                outm = {}
                todo = []
                for (n, score, score_r) in blks:
                    L = n * 128 + 128
                    mask, mask_r = masks.next()
                    outm[n] = (mask, mask_r)
                    fw.op(fw.dve, "memset", writes=[score_r], ap=score[0:64, L - 64:L], constant=-BIG)
                    if L > TOPK:
                        (thr, thr_r), (Dt, D_r), (sums, sums_r), (bm, bm_r) = bis.next()
                        m8, m8_r = mx.next()
                        fw.op(fw.dve, "max", reads=[score_r], writes=[m8_r], out=m8[:], in_=score[:, 0:L])
                        fw.op(fw.dve, "tensor_reduce", reads=[score_r], writes=[bm_r], out=bm[:, 0:1], in_=score[:, 0:L - 64],
                              axis=mybir.AxisListType.X, op=ALU.min)
                        fw.op(fw.dve, "tensor_tensor", reads=[m8_r], writes=[bm_r], out=bm[:, 1:2], in0=m8[:, 0:1], in1=bm[:, 0:1], op=ALU.subtract)
                        fw.op(fw.dve, "tensor_scalar", reads=[bm_r, cs_res], writes=[D_r], out=Dt[:, 0:NIT + 2], in0=C("pow2")[:, 0:NIT + 2],
                              scalar1=bm[:, 1:2], scalar2=None, op0=ALU.mult)
                        fw.op(fw.dve, "memset", writes=[sums_r], ap=sums[:], constant=0.0)
                        fw.op(fw.dve, "tensor_tensor", reads=[bm_r, D_r], writes=[thr_r], out=thr[:, 0:1], in0=bm[:, 0:1], in1=Dt[:, 0:1], op=ALU.add)
                        todo.append((n, L, score, score_r, mask, mask_r, thr, thr_r, Dt, D_r, sums, sums_r, bm, bm_r))
                    else:
                        fw.op(fw.dve, "memset", writes=[mask_r], ap=mask[:, 0:L], constant=1.0)
                        fw.op(fw.dve, "memset", writes=[mask_r], ap=mask[0:64, L - 64:L], constant=0.0)
                for i in range(NIT):
                    for (n, L, score, score_r, mask, mask_r, thr, thr_r, Dt, D_r, sums, sums_r, bm, bm_r) in todo:
                        jk, jk_r = sjunk.next()
                        fw.op(fw.act, "activation", reads=[score_r, thr_r], writes=[jk_r, sums_r], out=jk[:, 0:L], in_=score[:, 0:L],
                              func=AF.Sign, scale=-1.0, bias=thr[:, i:i + 1], accum_out=sums[:, i:i + 1])
                    for (n, L, score, score_r, mask, mask_r, thr, thr_r, Dt, D_r, sums, sums_r, bm, bm_r) in todo:
                        fw.op(fw.dve, "tensor_scalar", reads=[sums_r], writes=[bm_r], out=bm[:, 2:3], in0=sums[:, i:i + 1],
                              scalar1=float(L - 2 * TOPK), scalar2=0.5, op0=ALU.is_le, op1=ALU.subtract)
                        fw.op(fw.dve, "scalar_tensor_tensor", reads=[bm_r, D_r], writes=[thr_r], out=thr[:, i + 1:i + 2], in0=Dt[:, i:i + 1],
                              scalar=bm[:, 2:3], in1=thr[:, i:i + 1], op0=ALU.mult, op1=ALU.add)
                for (n, L, score, score_r, mask, mask_r, thr, thr_r, Dt, D_r, sums, sums_r, bm, bm_r) in todo:
                    fw.op(fw.dve, "tensor_scalar", reads=[score_r, thr_r, D_r], writes=[mask_r], out=mask[:, 0:L], in0=score[:, 0:L],
                          scalar1=Dt[:, NIT:NIT + 1], scalar2=thr[:, NIT:NIT + 1], op0=ALU.add, op1=ALU.is_ge)
                if debug:
                    for (n, score, score_r) in blks:
                        mask, mask_r = outm[n]
                        fw.dma("pool", out=dbg_score[n, :, :], in_=score[:, :], reads=[score_r], writes=[dbg_r])
                        fw.dma("pool", out=dbg_mask[n, :, :], in_=mask[:, :], reads=[mask_r], writes=[dbg_r])
                return outm

            def attend(n, mask, mask_r):
                t0 = n * 128
                for j0 in range(0, n + 1, 8):
                    nj = min(8, n + 1 - j0)
                    bt, br = fw.bank()
                    pt = bt[:].bitcast(BF16)
                    fw.pe_group([(pt[:, jj * 128:(jj + 1) * 128], mask[:, (j0 + jj) * 128:(j0 + jj + 1) * 128], C("ident"))
                                 for jj in range(nj)], reads=[mask_r, cs_res], writes=[br], transpose=True)
                    evac_copy(maskT[:, j0:j0 + nj, :], pt[:, 0:nj * 128].rearrange("p (j c) -> p j c", j=nj), [br], [maskT_r])
                oat, oat_r = oa.next()
                for half in range(2):
                    stt = {}

                    def s1_(j):
                        E, E_r = Eb.next()
                        P, P_r = Pb.next()
                        bt, br = fw.banks[4 + (att_i[0] % 2)]
                        att_i[0] += 1
                        fw.pe_group([(bt[:, :].rearrange("p (h t) -> p h t", h=4), cT[:, j * 128:(j + 1) * 128], qaT[:, half * 4:half * 4 + 4, t0:t0 + 128], True, True)],
                                    reads=[cT_r, qaT_r], writes=[br])
                        fw.op(fw.act, "activation", reads=[br], writes=[E_r], out=E[:],
                              in_=bt[:, :].rearrange("p (h t) -> p h t", h=4), func=AF.Exp, scale=float(128 ** -0.5))
                        fw.op(fw.dve, "tensor_tensor", reads=[E_r, maskT_r], writes=[P_r],
                              out=P[:], in0=E[:], in1=maskT[:, j:j + 1, :].to_broadcast([128, 4, 128]), op=ALU.mult)
                        stt[j] = (P, P_r)

                    def s2_(j):
                        P, P_r = stt.pop(j)
                        for h4 in range(4):
                            ob, ob_r = fw.banks[h4]
                            fw.pe_group([(ob[:, 0:130], P[:, h4, :], ctm[:, j, :], j == 0, j == n)],
                                        reads=[P_r, ctm_r], writes=[ob_r])

                    for j in range(n + 1 + 2):
                        if j <= n:
                            s1_(j)
                        if 0 <= j - 2 <= n:
                            s2_(j - 2)
                    for h4 in range(4):
                        hd = half * 4 + h4
                        ob, ob_r = fw.banks[h4]
                        fw.op(fw.dve, "reciprocal", reads=[ob_r], writes=[rden_r], out=rden[:, hd:hd + 1], in_=ob[:, 128:129])
                        fw.op(fw.act, "mul", reads=[ob_r, rden_r], writes=[oat_r], out=oat[:, hd * 128:(hd + 1) * 128],
                              in_=ob[:, 0:128], mul=rden[:, hd:hd + 1])
                fw.dma("pool", out=o0[b, t0:t0 + 128, 0:1024], in_=oat[:], reads=[oat_r], writes=[R["o0"][b]])

            GS = 2
            groups = [list(range(g0, min(NT, g0 + GS))) for g0 in range(0, NT, GS)]
            pend = [(n,) + tuple(indexer(n)) for n in groups[0]]
            for gi, grp in enumerate(groups):
                cur = pend
                if gi + 1 < len(groups):
                    pend = [(n,) + tuple(indexer(n)) for n in groups[gi + 1]]
                for _ in range(per_blk * len(grp)):
                    if pieces:
                        conv_issue(pieces.pop(0))
                ms = select_group(cur)
                for n in grp:
                    attend(n, *ms[n])
            while pieces:
                conv_issue(pieces.pop(0))

    def phase_gla(sc, b):
        NCH = S // 64
        if True:
            Sf, Sf_r = sc.sb([128, 4, 256], F32, "Sf")
            Sb, Sb_r = sc.sb([128, 4, 256], BF16, "Sb")
            wg, wg_r = sc.sb([32, 512], BF16, "wgk")
            gn, gn_r = sc.sb([64, 256], F32, "gn")
            gkT, gkT_r = sc.sb([32, S], BF16, "gkT")
            qk = Ring([sc.sb([64, 1024], F32, "qk") for _ in range(2)])
            vb = Ring([sc.sb([64, 1024], BF16, "vb") for _ in range(2)])
            gb = Ring([sc.sb([64, 1024], F32, "gb") for _ in range(1)])
            e1 = Ring([sc.sb([64, 512], F32, "e1") for _ in range(1)])
            sp_ = Ring([sc.sb([64, 512], BF16, "sp") for _ in range(2)])
            eG = Ring([sc.sb([64, 3, 512], F32, "eG") for _ in range(1)])
            dec = Ring([sc.sb([128, 4], F32, "dec") for _ in range(2)])
            qkd = Ring([sc.sb([64, 3, 512], BF16, "qkd") for _ in range(2)])
            qkT = Ring([sc.sb([128, 8, 64], BF16, "qkT") for _ in range(2)])
            attm = Ring([sc.sb([64, 4, 64], BF16, "attm") for _ in range(2)])
            ssr = Ring([sc.sb([64, 4], F32, "ssr") for _ in range(2)])
            junk = Ring([sc.sb([64, 256], BF16, "gjunk") for _ in range(2)])
            on = Ring([sc.sb([64, 1024], F32, "on") for _ in range(1)])
            sg = Ring([sc.sb([64, 1024], F32, "sg") for _ in range(1)])
            ob = Ring([sc.sb([64, 1024], BF16, "ob") for _ in range(1)])
            fw.op(fw.dve, "memset", writes=[Sf_r], ap=Sf[:], constant=0.0)
            fw.op(fw.dve, "memset", writes=[Sb_r], ap=Sb[:], constant=0.0)
            fw.op(fw.dve, "memset", writes=[gkT_r], ap=gkT[:], constant=1.0)
            fw.dma("pool", out=wg[0:17, :], in_=wgk[:, :], writes=[wg_r])
            fw.dma("sp", out=gn[:], in_=smalls[:, :], writes=[gn_r])
            fw.dma("sp", out=gkT[0:16, :], in_=fm0[b, 18, 0:16, :], reads=[R["fm0"][b]], writes=[gkT_r])
            sc_q = float(128 ** -0.5)
            for ci in range(NCH):
                t0 = ci * 64
                qkt, qkt_r = qk.next()
                vbt, vbt_r = vb.next()
                gbt, gbt_r = gb.next()
                fw.dma("sp", out=qkt[:], in_=tm0[b, t0:t0 + 64, 144:1168], reads=[R["tm0"][b]], writes=[qkt_r])
                fw.dma("pool", out=vbt[:], in_=tm0[b, t0:t0 + 64, 1168:2192], reads=[R["tm0"][b]], writes=[vbt_r])
                fw.dma("sp", out=gbt[:], in_=tm0[b, t0:t0 + 64, 2192:3216], reads=[R["tm0"][b]], writes=[gbt_r])
                bz, bz_r = fw.bank()
                fw.pe_group([(bz[0:64, :], gkT[0:17, t0:t0 + 64], wg[0:17, :], True, True)], reads=[gkT_r, wg_r], writes=[bz_r])
                e1t, e1_r = e1.next()
                spt, sp_r = sp_.next()
                fw.op(fw.act, "activation", reads=[bz_r], writes=[e1_r], out=e1t[:], in_=bz[0:64, :], func=AF.Exp, scale=-1.0)
                fw.op(fw.act, "activation", reads=[e1_r], writes=[sp_r], out=spt[:], in_=e1t[:], func=AF.Ln, bias=1.0)
                bG, bG_r = fw.bank()
                bD, bD_r = fw.bank()
                bL, bL_r = fw.bank()
                fw.pe_group([(bG[0:64, :], C("triM", 64), spt[:], True, True)], reads=[sp_r, cs_res], writes=[bG_r])
                fw.pe_group([(bD[0:64, :], C("triM2", 64), spt[:], True, True)], reads=[sp_r, cs_res], writes=[bD_r])
                fw.pe_group([(bL[:, hh * 2:hh * 2 + 2], spt[:, hh * 128:(hh + 1) * 128], C("chunkind", 64), True, True) for hh in range(4)],
                            reads=[sp_r, cs_res], writes=[bL_r])
                eGt, eG_r = eG.next()
                dct, dc_r = dec.next()
                fw.op(fw.act, "activation", reads=[bG_r], writes=[eG_r], out=eGt[:, 0, :], in_=bG[0:64, :], func=AF.Exp)
                fw.op(fw.act, "activation", reads=[bG_r], writes=[eG_r], out=eGt[:, 1, :], in_=bG[0:64, :], func=AF.Exp, scale=-1.0)
                fw.op(fw.act, "activation", reads=[bD_r], writes=[eG_r], out=eGt[:, 2, :], in_=bD[0:64, :], func=AF.Exp)
                fw.op(fw.act, "activation", reads=[bL_r], writes=[dc_r], out=dct[:, :], in_=bL[:, 0:8].rearrange("p (h t) -> p h t", t=2)[:, :, 0],
                      func=AF.Exp)
                qd, qd_r = qkd.next()
                fw.op(fw.dve, "scalar_tensor_tensor", reads=[qkt_r, eG_r], writes=[qd_r], out=qd[:, 0, :], in0=qkt[:, 0:512], scalar=sc_q,
                      in1=eGt[:, 0, :], op0=ALU.mult, op1=ALU.mult)
                fw.op(fw.dve, "tensor_tensor", reads=[qkt_r, eG_r], writes=[qd_r], out=qd[:, 1, :], in0=qkt[:, 512:1024], in1=eGt[:, 1, :], op=ALU.mult)
                fw.op(fw.pool, "tensor_tensor", reads=[qkt_r, eG_r], writes=[qd_r], out=qd[:, 2, :], in0=qkt[:, 512:1024], in1=eGt[:, 2, :], op=ALU.mult)
                bt, br = fw.bank()
                pt = bt[:].bitcast(BF16)
                fw.pe_group([(pt[:, (a * 4 + hh) * 64:(a * 4 + hh + 1) * 64], qd[:, a, hh * 128:(hh + 1) * 128], C("ident", 64)[:, 0:64])
                             for a in range(2) for hh in range(4)], reads=[qd_r, cs_res], writes=[br], transpose=True)
                qT, qT_r = qkT.next()
                evac_copy(qT[:], pt[:, 0:512].rearrange("p (j c) -> p j c", j=8), [br], [qT_r])
                ba, ba_r = fw.bank()
                fw.pe_group([(ba[0:64, hh * 64:(hh + 1) * 64], qT[:, 4 + hh, :], qT[:, hh, :], True, True) for hh in range(4)],
                            reads=[qT_r], writes=[ba_r])
                am, am_r = attm.next()
                for hh in range(4):
                    fw.op(fw.dve, "tensor_tensor", reads=[ba_r, cs_res], writes=[am_r], out=am[:, hh, :], in0=ba[0:64, hh * 64:(hh + 1) * 64],
                          in1=C("maskG", 64), op=ALU.mult)
                bo = [fw.bank(), fw.bank()]
                for hh in range(4):
                    bt2, br2 = bo[hh // 2]
                    cs0 = (hh % 2) * 256
                    fw.pe_group([(bt2[0:64, cs0:cs0 + 256], am[:, hh, :], vbt[:, hh * 256:(hh + 1) * 256], True, False),
                                 (bt2[0:64, cs0:cs0 + 256], qT[:, hh, :], Sb[:, hh, :], False, True)],
                                reads=[am_r, vbt_r, qT_r, Sb_r], writes=[br2])
                bkv = [fw.bank(), fw.bank()]
                for hh in range(4):
                    bt3, br3 = bkv[hh // 2]
                    cs0 = (hh % 2) * 256
                    fw.pe_group([(bt3[:, cs0:cs0 + 256], qd[:, 2, hh * 128:(hh + 1) * 128], vbt[:, hh * 256:(hh + 1) * 256], True, True)],
                                reads=[qd_r, vbt_r], writes=[br3])
                for hh in range(4):
                    bt3, br3 = bkv[hh // 2]
                    cs0 = (hh % 2) * 256
                    fw.op(fw.dve, "scalar_tensor_tensor", reads=[br3, dc_r], writes=[Sf_r], out=Sf[:, hh, :], in0=Sf[:, hh, :],
                          scalar=dct[:, hh:hh + 1], in1=bt3[:, cs0:cs0 + 256], op0=ALU.mult, op1=ALU.add)
                fw.op(fw.act, "copy", reads=[Sf_r], writes=[Sb_r], out=Sb[:], in_=Sf[:])
                sst, ss_r = ssr.next()
                fw.op(fw.dve, "memset", writes=[ss_r], ap=sst[:], constant=0.0)
                for hh in range(4):
                    bt2, br2 = bo[hh // 2]
                    cs0 = (hh % 2) * 256
                    jk, jk_r = junk.next()
                    fw.op(fw.act, "activation", reads=[br2], writes=[jk_r, ss_r], out=jk[:], in_=bt2[0:64, cs0:cs0 + 256], func=AF.Square,
                          accum_out=sst[:, hh:hh + 1])
                fw.op(fw.dve, "tensor_scalar", writes=[ss_r], out=sst[:], in0=sst[:], scalar1=1.0 / 256, scalar2=EPS, op0=ALU.mult, op1=ALU.add)
                fw.op(fw.act, "sqrt", writes=[ss_r], out=sst[:], in_=sst[:])
                fw.op(fw.dve, "reciprocal", writes=[ss_r], out=sst[:], in_=sst[:])
                ont, on_r = on.next()
                for hh in range(4):
                    bt2, br2 = bo[hh // 2]
                    cs0 = (hh % 2) * 256
                    fw.op(fw.dve, "scalar_tensor_tensor", reads=[br2, ss_r, gn_r], writes=[on_r], out=ont[:, hh * 256:(hh + 1) * 256],
                          in0=bt2[0:64, cs0:cs0 + 256], scalar=sst[:, hh:hh + 1], in1=gn[:], op0=ALU.mult, op1=ALU.mult)
                sgt, sg_r = sg.next()
                fw.op(fw.act, "activation", reads=[gbt_r], writes=[sg_r], out=sgt[:], in_=gbt[:], func=AF.Silu)
                obt, ob_r = ob.next()
                fw.op(fw.pool, "tensor_tensor", reads=[on_r, sg_r], writes=[ob_r], out=obt[:], in0=ont[:], in1=sgt[:], op=ALU.mult)
                fw.dma("pool", out=o0[b, t0:t0 + 64, 1024:2048], in_=obt[:], reads=[ob_r], writes=[R["o0"][b]])

    def phase_inproj1(sc, b):
        if True:
            convert(["in1", "out1", "g1", "u1", "d1", "pg1", "pe1"])
            dn = Dense(sc)
            load_gain(dn.gain, dn.gain_r, 1)
            sc_q = float(128 ** -0.5)
            for g in range(NG):
                for tt in range(4):
                    fw.dma("sp", out=dn.hres[tt][:], in_=h[b, g * G + tt * 128:g * G + (tt + 1) * 128, :],
                           reads=[R["h"][b]], writes=[dn.hres_r[tt]])
                    norm_tile(dn, dn.hres[tt], dn.hres_r[tt], dn.xnT, dn.xnT_r, tt * 128)

                def ep_fm(chunk, bt, br):
                    st, sr = dn.ev16.next()
                    if chunk < 16:
                        fw.op(fw.act, "mul", reads=[br], writes=[sr], out=st[:, 0:G], in_=bt[:, 0:G], mul=sc_q)
                    else:
                        evac_copy(st[:, 0:G], bt[:, 0:G], [br], [sr])
                    fw.dma("pool", out=fm1[b, chunk, :, g * G:(g + 1) * G], in_=st[:, 0:G], reads=[sr], writes=[R["fm1"][b]])
                proj_fm(dn, dn.xnT, dn.xnT_r, 16, "in1", 32, G, ep_fm)

                def ep_tm(c0, cw, tt, bt, br):
                    st, sr = dn.ev16.next()
                    evac_copy(st[:, 0:cw], bt[:, 0:cw], [br], [sr])
                    fw.dma("pool", out=tm1[b, g * G + tt * 128:g * G + (tt + 1) * 128, c0 - 2 * D:c0 - 2 * D + cw], in_=st[:, 0:cw],
                           reads=[sr], writes=[R["tm1"][b]])
                proj_tm(dn, dn.xnT, dn.xnT_r, 16, 16, "in1", 3 * D, 4, ep_tm, c_start=2 * D)

    def phase_sb(sc, b):
        QB = 512
        if True:
            qT = Ring([sc.sb([128, S], BF16, "qT") for _ in range(2)])
            kT = Ring([sc.sb([128, S], BF16, "kT") for _ in range(2)])
            vv = Ring([sc.sb([128, NT, 128], BF16, "vv") for _ in range(2)])
            Ebuf = Ring([sc.sb([128, 512], F32, "sbE") for _ in range(4)])
            Lp = Ring([sc.sb([128, 512], BF16, "Lp") for _ in range(3)])
            acc, acc_r = sc.sb([128, 512], BF16, "acc")
            Wt = Ring([sc.sb([128, 512], BF16, "Wt") for _ in range(3)])
            Xt = Ring([sc.sb([128, 512], BF16, "Xt") for _ in range(3)])
            ost = Ring([sc.sb([128, 512], BF16, "ost") for _ in range(2)])
            cb = list(fw.cur_banks)
            if len(cb) >= 8:
                b1_banks, b2_bank, bo_bank = [cb[0], cb[1], cb[2]], cb[3], cb[4]
            else:
                b1_banks, b2_bank, bo_bank = [cb[0], cb[1]], cb[2], cb[3]
            heads = {}

            def load_head(hd):
                if hd >= 16 or hd in heads:
                    return
                q_, q_r = qT.next()
                k_, k_r = kT.next()
                v_, v_r = vv.next()
                fw.dma("sp", out=q_[:], in_=fm1[b, hd], reads=[R["fm1"][b]], writes=[q_r])
                fw.dma("sp", out=k_[:], in_=fm1[b, 16 + hd], reads=[R["fm1"][b]], writes=[k_r])
                fw.dma("sp", out=v_[:], in_=tm1[b, :, hd * 128:(hd + 1) * 128].rearrange("(j p) c -> p j c", p=128),
                       reads=[R["tm1"][b]], writes=[v_r])
                heads[hd] = (q_, q_r, k_, k_r, v_, v_r)

            items = []
            for hd in range(16):
                for qb in range(S // QB):
                    for c in range(4 * qb + 3, -1, -1):
                        items.append((hd, qb, c))
            NI = len(items)
            st = {}

            def geo(it):
                hd, qb, c = it
                k_d = c - 4 * qb
                diag = k_d >= 0
                lo = 128 * k_d if diag else 0
                return hd, qb, c, qb * QB, 4 * qb + 3, k_d, diag, lo

            def stage_a(i):
                hd, qb, c, t0, cmax, k_d, diag, lo = geo(items[i])
                load_head(hd)
                q_, q_r, k_, k_r, v_, v_r = heads[hd]
                b1, b1_r = fw.banks[b1_banks[i % len(b1_banks)]]
                fw.pe_group([(b1[:, lo:QB], k_[:, c * 128:(c + 1) * 128], q_[:, t0 + lo:t0 + QB], True, True)], reads=[k_r, q_r], writes=[b1_r])
                Et, E_r = Ebuf.next()
                fw.op(fw.act, "activation", reads=[b1_r], writes=[E_r], out=Et[:, lo:QB], in_=b1[:, lo:QB], func=AF.Exp)
                Lt, L_r = Lp.next()
                fw.op(fw.act, "activation", reads=[E_r], writes=[L_r], out=Lt[:, lo:QB], in_=Et[:, lo:QB], func=AF.Ln, bias=1.0)
                if diag:
                    fw.op(fw.pool, "tensor_tensor", reads=[L_r, cs_res], writes=[L_r], out=Lt[:, lo:QB], in0=Lt[:, lo:QB],
                          in1=C("m01_%d" % k_d)[:, lo:QB], op=ALU.mult)
                st[i] = {"E": (Et, E_r), "L": (Lt, L_r)}

            def stage_b(i):
                hd, qb, c, t0, cmax, k_d, diag, lo = geo(items[i])
                Lt, L_r = st[i]["L"]
                b2, b2_r = fw.banks[b2_bank]
                mms = [(b2[:, lo:QB], C("triNeg"), Lt[:, lo:QB], True, (c == cmax) and not diag)]
                rds = [L_r, cs_res]
                if c != cmax:
                    mms.append((b2[:, lo:QB], C("onesNeg"), acc[:, lo:QB], False, not diag))
                    rds.append(acc_r)
                if diag:
                    mms.append((b2[:, lo:QB], C("ident"), C("mneg_%d" % k_d)[:, lo:QB], False, True))
                fw.pe_group(mms, reads=rds, writes=[b2_r])
                if c == cmax:
                    fw.op(fw.dve, "memset", writes=[acc_r], ap=acc[:], constant=0.0)
                if c > 0:
                    fw.op(fw.dve, "tensor_tensor", reads=[L_r], writes=[acc_r], out=acc[:, lo:QB], in0=acc[:, lo:QB], in1=Lt[:, lo:QB], op=ALU.add)
                Xtt, X_r = Xt.next()
                fw.op(fw.act, "activation", reads=[b2_r], writes=[X_r], out=Xtt[:, lo:QB], in_=b2[:, lo:QB], func=AF.Exp)
                st[i]["X"] = (Xtt, X_r)

            def stage_c(i):
                hd, qb, c, t0, cmax, k_d, diag, lo = geo(items[i])
                Et, E_r = st[i]["E"]
                Xtt, X_r = st[i]["X"]
                Wtt, W_r = Wt.next()
                fw.op(fw.dve, "tensor_tensor", reads=[X_r, E_r], writes=[W_r], out=Wtt[:, lo:QB], in0=Xtt[:, lo:QB], in1=Et[:, lo:QB], op=ALU.mult)
                st[i]["W"] = (Wtt, W_r)

            def stage_d(i):
                hd, qb, c, t0, cmax, k_d, diag, lo = geo(items[i])
                q_, q_r, k_, k_r, v_, v_r = heads[hd]
                Wtt, W_r = st[i]["W"]
                bo, bo_r = fw.banks[bo_bank]
                fw.pe_group([(bo[:, lo:QB], v_[:, c, :], Wtt[:, lo:QB], c == cmax, c == 0)], reads=[v_r, W_r], writes=[bo_r])
                if c == 0:
                    st_, st_r = ost.next()
                    evac_copy(st_[:], bo[:, :], [bo_r], [st_r])
                    fw.dma("pool", out=oT1[b, hd, :, t0:t0 + QB], in_=st_[:], reads=[st_r], writes=[R["oT1"][b]])
                    load_head(hd + 1)
                del st[i]

            for i in range(NI + 3):
                if i < NI:
                    stage_a(i)
                if 0 <= i - 3 < NI:
                    stage_d(i - 3)
                if 0 <= i - 1 < NI:
                    stage_b(i - 1)
                if 0 <= i - 2 < NI:
                    stage_c(i - 2)

    def phase_post(sc, b, layer):
        if True:
            dn = Dense(sc, evs=False, small=(len(fw.cur_banks) < 8))
            h1T, h1T_r = sc.sb([128, 22, G], BF16, "h1T")
            otile = dn.xn
            sgl = Ring([sc.sb([128, 512], F32, "sgl") for _ in range(2)])
            pt32 = Ring([sc.sb([128, PLE], F32, "pt32") for _ in range(2)])
            pt16 = Ring([sc.sb([128, PLE], BF16, "pt16") for _ in range(2)])
            pT, pT_r = sc.sb([128, 2, G], BF16, "pT")
            wpeb = Ring([sc.sb([128, 2, 512], BF16, "wpe") for _ in range(2)])
            cur_wpe = [None]
            pe_sb = Ring([sc.sb([128, 512], F32, "pesb") for _ in range(1)])
            sig = Ring([sc.sb([128, 512], F32, "sig") for _ in range(1)])
            hsrc = x if layer == 0 else h
            for g in range(NG):
                tok0 = g * G
                if layer == 0:
                    for tt in range(4):
                        ot, ot_r = otile.next()
                        fw.dma("sp", out=ot[:], in_=o0[b, tok0 + tt * 128:tok0 + (tt + 1) * 128, :], reads=[R["o0"][b]], writes=[ot_r])
                        transpose_to(dn, ot, ot_r, dn.xnT, dn.xnT_r, tt * 128)
                else:
                    fw.dma("sp", out=dn.xnT[:], in_=oT1[b, :, :, tok0:tok0 + G].rearrange("c p s -> p c s"),
                           reads=[R["oT1"][b]], writes=[dn.xnT_r])
                for tt in range(4):
                    rds = [R["h"][b]] if layer == 1 else []
                    fw.dma("sp", out=dn.hres[tt][:], in_=hsrc[b, tok0 + tt * 128:tok0 + (tt + 1) * 128, :], reads=rds, writes=[dn.hres_r[tt]])

                def ep_res(c0, cw, tt, bt, br):
                    fw.op(fw.dve, "tensor_tensor", reads=[br], writes=[dn.hres_r[tt]], out=dn.hres[tt][:, c0:c0 + cw],
                          in0=dn.hres[tt][:, c0:c0 + cw], in1=bt[:, 0:cw], op=ALU.add)
                proj_tm(dn, dn.xnT, dn.xnT_r, 16, 16, "out%d" % layer, D, 4, ep_res)
                load_gain(dn.gain, dn.gain_r, 2 + layer)
                for tt in range(4):
                    norm_tile(dn, dn.hres[tt], dn.hres_r[tt], dn.xnT, dn.xnT_r, tt * 128)
                for half in range(2):
                    h0 = half * 22
                    for hb in range(h0, h0 + 22, 4):
                        nb = min(4, h0 + 22 - hb)
                        wgt, wg_r = load_w(dn, "g%d" % layer, 0, 16, hb * 128, nb * 128)
                        wut, wu_r = load_w(dn, "u%d" % layer, 0, 16, hb * 128, nb * 128)
                        for cc in range(nb):
                            bg, bg_r = fw.bank()
                            bu, bu_r = fw.bank()
                            fw.pe_group([(bg[:, 0:G], wgt[:, kc, cc * 128:(cc + 1) * 128], dn.xnT[:, kc, 0:G], kc == 0, kc == 15) for kc in range(16)],
                                        reads=[dn.xnT_r, wg_r], writes=[bg_r])
                            fw.pe_group([(bu[:, 0:G], wut[:, kc, cc * 128:(cc + 1) * 128], dn.xnT[:, kc, 0:G], kc == 0, kc == 15) for kc in range(16)],
                                        reads=[dn.xnT_r, wu_r], writes=[bu_r])
                            s_, s_r = sgl.next()
                            fw.op(fw.act, "activation", reads=[bg_r], writes=[s_r], out=s_[:, 0:G], in_=bg[:, 0:G], func=AF.Silu)
                            fw.op(fw.dve, "tensor_tensor", reads=[s_r, bu_r], writes=[h1T_r], out=h1T[:, hb - h0 + cc, :], in0=s_[:, 0:G], in1=bu[:, 0:G], op=ALU.mult)
                    proj_tm(dn, h1T, h1T_r, 22, 11, "d%d" % layer, D, 4, ep_res, r_off=h0 * 128)
                load_gain(dn.gain, dn.gain_r, 4 + layer)
                for tt in range(4):
                    norm_tile(dn, dn.hres[tt], dn.hres_r[tt], dn.xnT, dn.xnT_r, tt * 128)
                    p32, p32_r = pt32.next()
                    p16, p16_r = pt16.next()
                    fw.dma("sp", out=p32[:], in_=p[layer, b, tok0 + tt * 128:tok0 + (tt + 1) * 128, :], writes=[p32_r])
                    fw.op(fw.pool, "tensor_copy", reads=[p32_r], writes=[p16_r], out=p16[:], in_=p32[:])
                    bt, br = fw.bank()
                    ptb = bt[:].bitcast(BF16)
                    fw.pe_group([(ptb[:, j * 128:(j + 1) * 128], p16[:, j * 128:(j + 1) * 128], C("ident")) for j in range(2)],
                                reads=[p16_r, cs_res], writes=[br], transpose=True)
                    evac_copy(pT[:, :, tt * 128:(tt + 1) * 128], ptb[:, 0:256].rearrange("p (j c) -> p j c", j=2), [br], [pT_r])

                def ep_ple(c0, cw, tt, bt, br):
                    if tt == 0:
                        wpe_t, wpe_r = wpeb.next()
                        fw.dma("sp", out=wpe_t[:, :, 0:cw], in_=wb["pe%d" % layer][:, c0:c0 + cw].rearrange("(kc p) c -> p kc c", p=128),
                               reads=[wres["pe%d" % layer]], writes=[wpe_r])
                        cur_wpe[0] = (wpe_t, wpe_r)
                    wpe_t, wpe_r = cur_wpe[0]
                    b2, b2_r = fw.bank()
                    fw.pe_group([(b2[:, 0:cw], pT[:, kc, tt * 128:(tt + 1) * 128], wpe_t[:, kc, 0:cw], kc == 0, kc == 1) for kc in range(2)],
                                reads=[pT_r, wpe_r], writes=[b2_r])
                    sg_, sg_r = sig.next()
                    fw.op(fw.act, "activation", reads=[br], writes=[sg_r], out=sg_[:, 0:cw], in_=bt[:, 0:cw], func=AF.Sigmoid)
                    pe_, pe_r = pe_sb.next()
                    fw.op(fw.dve, "tensor_tensor", reads=[sg_r, b2_r], writes=[pe_r], out=pe_[:, 0:cw], in0=sg_[:, 0:cw], in1=b2[:, 0:cw], op=ALU.mult)
                    fw.op(fw.pool, "tensor_tensor", reads=[pe_r], writes=[dn.hres_r[tt]], out=dn.hres[tt][:, c0:c0 + cw],
                          in0=dn.hres[tt][:, c0:c0 + cw], in1=pe_[:, 0:cw], op=ALU.add)
                proj_tm(dn, dn.xnT, dn.xnT_r, 16, 16, "pg%d" % layer, D, 4, ep_ple)
                if layer == 0:
                    for tt in range(4):
                        fw.dma("pool", out=h[b, tok0 + tt * 128:tok0 + (tt + 1) * 128, :], in_=dn.hres[tt][:], reads=[dn.hres_r[tt]], writes=[R["h"][b]])
                else:
                    load_gain(dn.gain, dn.gain_r, 6)
                    for tt in range(4):
                        ss, ss_r = dn.ss.next()
                        junk, junk_r = dn.junk.next()
                        rms_rstd(dn, dn.hres[tt][:], dn.hres_r[tt], 128, D, ss, ss_r, junk[:], junk_r)
                        fw.op(fw.dve, "scalar_tensor_tensor", reads=[ss_r, dn.gain_r], writes=[dn.hres_r[tt]], out=dn.hres[tt][:], in0=dn.hres[tt][:],
                              scalar=ss[:, 0:1], in1=dn.gain[:], op0=ALU.mult, op1=ALU.mult)
                        fw.dma("pool", out=out[b, tok0 + tt * 128:tok0 + (tt + 1) * 128, :], in_=dn.hres[tt][:], reads=[dn.hres_r[tt]], writes=[R["out"][b]])

    ALL8 = list(range(8))

    def run_group(streams):
        with Scope(fw) as sc:
            recs = []
            for fn, args, banks in streams:
                fw.rec = []
                fw.bank_ids = list(banks)
                fw.cur_banks = list(banks)
                fw.bank_i = 0
                fn(sc, *args)
                recs.append(fw.rec)
                fw.rec = None
            pos = [0] * len(recs)
            while True:
                best, bf = None, None
                for i, r in enumerate(recs):
                    if pos[i] < len(r):
                        f = pos[i] / float(len(r))
                        if bf is None or f < bf:
                            best, bf = i, f
                if best is None:
                    break
                fnc, a, kw = recs[best][pos[best]]
                fnc(*a, **kw)
                pos[best] += 1
        fw.bank_ids = list(ALL8)

    sched = []
    if NSEQ == 1:
        sched = [("inproj0", [(phase_inproj0, (0,), ALL8)]), ("dsa", [(phase_dsa, (0,), ALL8)]), ("gla", [(phase_gla, (0,), ALL8)]),
                 ("post0", [(phase_post, (0, 0), ALL8)]), ("inproj1", [(phase_inproj1, (0,), ALL8)]), ("sb", [(phase_sb, (0,), ALL8)]),
                 ("post1", [(phase_post, (0, 1), ALL8)])]
    else:
        assert NSEQ == 2
        LO, HI = [0, 1, 2, 3], [4, 5, 6, 7]
        sched = [
            ("a", [(phase_inproj0, (0,), ALL8)]),
            ("b", [(phase_dsa, (0,), ALL8)]),
            ("c", [(phase_inproj0, (1,), LO), (phase_gla, (0,), HI)]),
            ("e", [(phase_post, (0, 0), LO), (phase_gla, (1,), HI)]),
            ("f", [(phase_dsa, (1,), ALL8)]),
            ("g", [(phase_inproj1, (0,), ALL8)]),
            ("h", [(phase_post, (1, 0), LO), (phase_sb, (0,), HI)]),
            ("i", [(phase_inproj1, (1,), ALL8)]),
            ("j", [(phase_post, (0, 1), LO), (phase_sb, (1,), HI)]),
            ("k", [(phase_post, (1, 1), ALL8)]),
        ]
    for name, streams in sched:
        run_group(streams)
        if stop is not None and name == stop:
            break
    fw.barrier()
    es.close()
    return nc


def prep_weights(inp):
    f = lambda a: np.ascontiguousarray(np.asarray(a, dtype=np.float32))
    w0 = f(inp["even_w_in"])[0]
    wfm0 = np.zeros((D, NFM0 * 128), np.float32)
    wfm0[:, 0:1024] = w0[:, OFF_QA:OFF_QA + 1024]
    wfm0[:, 1024:1152] = w0[:, OFF_CA:OFF_CA + 128]
    wfm0[:, 1152:2176] = w0[:, OFF_QI:OFF_QI + 1024]
    wfm0[:, 2176:2240] = w0[:, OFF_KI:OFF_KI + 64]
    wfm0[:, 2240:2304] = w0[:, OFF_KI:OFF_KI + 64]
    wfm0[:, 2304:2320] = w0[:, OFF_GK:OFF_GK + 16]
    wtm0 = np.concatenate([w0[:, OFF_CA:OFF_CA + 128], w0[:, OFF_WI:OFF_WI + 16], w0[:, OFF_QB:OFF_GK]], axis=1)
    assert wtm0.shape[1] == TMW0
    gains = np.stack([f(inp["even_norm"])[0], f(inp["odd_norm"])[0], f(inp["ffn_norm"])[0], f(inp["ffn_norm"])[1],
                      f(inp["ple_norm"])[0], f(inp["ple_norm"])[1], f(inp["final_norm"])], axis=0)
    gains = np.ascontiguousarray(np.broadcast_to(gains[:, None, :], (7, 128, D)))
    smalls = np.ascontiguousarray(np.broadcast_to(f(inp["even_gla_norm"])[0][None, :], (64, 256)))
    wgk = np.concatenate([f(inp["even_w_gk"])[0], f(inp["even_b_gk"])[0][None, :]], axis=0)
    m = {"gains": gains, "smalls": smalls, "cst": CONSTS, "wgk": np.ascontiguousarray(wgk),
         "wfm0": wfm0, "wtm0": np.ascontiguousarray(wtm0), "wout0": f(inp["even_w_out"])[0],
         "win1": f(inp["odd_w_in"])[0], "wout1": f(inp["odd_w_out"])[0]}
    for i in range(2):
        m["wg%d" % i] = f(inp["ffn_w_gate"])[i]
        m["wu%d" % i] = f(inp["ffn_w_up"])[i]
        m["wd%d" % i] = f(inp["ffn_w_down"])[i]
        m["wpg%d" % i] = f(inp["ple_w_gate"])[i]
        m["wpe%d" % i] = f(inp["ple_w_proj"])[i]
    return m


_CACHE = {}


def kernel(**inputs):
    x = np.asarray(inputs["x"], dtype=np.float32)
    p = np.asarray(inputs["p"], dtype=np.float32)
    B, S, _ = x.shape
    ncores = 8
    nseq = B // ncores
    wm = prep_weights(inputs)
    key = (S, nseq)
    if key not in _CACHE:
        _CACHE[key] = build(S, nseq)
    nc = _CACHE[key]
    in_maps = []
    for c in range(ncores):
        m = dict(wm)
        m["x"] = np.ascontiguousarray(x[c * nseq:(c + 1) * nseq])
        m["p"] = np.ascontiguousarray(p[:, c * nseq:(c + 1) * nseq])
        in_maps.append(m)
    res = run_bass_kernel_spmd(nc, in_maps, core_ids=list(range(ncores)))
    return np.concatenate([r["out"] for r in res.results], axis=0).astype(np.float32)
```

```python
import contextlib
import numpy as np
import concourse.bass as bass
import concourse.mybir as mybir
from concourse.bass_utils import run_bass_kernel_spmd

F32 = mybir.dt.float32
BF16 = mybir.dt.bfloat16
AF = mybir.ActivationFunctionType
ALU = mybir.AluOpType

D = 2048
FFH = 5632
PLE = 256
EPS = 1e-6
NEG = -30000.0
BIG = 1.0e30


class Ev:
    __slots__ = ("sem", "val", "key")

    def __init__(self, sem, val, key):
        self.sem, self.val, self.key = sem, val, key


class Res:
    __slots__ = ("w", "r", "name", "multi")

    def __init__(self, name="", multi=False):
        self.w = {}
        self.r = {}
        self.name = name
        self.multi = multi


class Eng:
    def __init__(self, fw, name, obj, is_pe=False):
        self.fw, self.name, self.obj, self.is_pe = fw, name, obj, is_pe
        self.seen = {}
        self.ownkeys = set()
        self._newsem()

    def _newsem(self):
        self.sem = self.fw.newsem(self.name)
        self.ownkeys.add(id(self.sem))
        self.cnt = 0

    def signal(self, inst):
        if self.cnt >= 30000:
            self._newsem()
        self.cnt += 1
        inst.then_inc(self.sem, 1)
        return Ev(self.sem, self.cnt, id(self.sem))


class FW:
    SAME_ENGINE_SYNC = True

    def __init__(self, nc, es):
        self.nc, self.es = nc, es
        self.nsem = 0
        self.pe = Eng(self, "pe", nc.tensor, True)
        self.act = Eng(self, "act", nc.scalar)
        self.dve = Eng(self, "dve", nc.vector)
        self.pool = Eng(self, "pool", nc.gpsimd)
        self.sp = Eng(self, "sp", nc.sync)
        self.engs = [self.pe, self.act, self.dve, self.pool, self.sp]
        self.slots = {}
        for q in ("sp", "pool"):
            self.slots[q] = [[self.newsem("dq%s" % q), 0] for _ in range(12)]
        self.slot_i = {"sp": 0, "pool": 0}
        self.banks = []
        self.bank_i = 0
        self.bank_ids = list(range(8))
        self.ninst = 0
        self.rec = None

    def newsem(self, name):
        self.nsem += 1
        return self.es.enter_context(self.nc.semaphore("s%s%d" % (name, self.nsem)))

    def wait(self, eng, ev):
        if eng.seen.get(ev.key, 0) >= ev.val:
            return
        eng.obj.wait_ge(ev.sem, ev.val)
        eng.seen[ev.key] = ev.val
        self.ninst += 1

    def _w(self, eng, ev, raw, is_dma):
        if ev.key in eng.ownkeys and not is_dma:
            if eng.is_pe or not raw or not self.SAME_ENGINE_SYNC:
                return
        self.wait(eng, ev)

    def deps(self, eng, reads, writes, is_dma=False):
        for r in reads:
            for ev in r.w.values():
                self._w(eng, ev, True, is_dma)
        for w in writes:
            if not w.multi:
                for ev in w.w.values():
                    self._w(eng, ev, True, is_dma)
            for ev in w.r.values():
                self._w(eng, ev, False, is_dma)

    def done(self, ev, reads, writes):
        for r in reads:
            old = r.r.get(ev.key)
            if old is None or old.val < ev.val:
                r.r[ev.key] = ev
        for w in writes:
            if w.multi:
                w.w[ev.key] = ev
            else:
                w.w = {ev.key: ev}
            w.r = {}

    def op(self, eng, fn, reads=(), writes=(), **kw):
        if self.rec is not None:
            self.rec.append((self._op, (eng, fn, reads, writes), kw))
            return None
        return self._op(eng, fn, reads, writes, **kw)

    def pe_group(self, mms, reads=(), writes=(), transpose=False):
        if self.rec is not None:
            self.rec.append((self._pe_group, (mms, reads, writes, transpose), {}))
            return None
        return self._pe_group(mms, reads, writes, transpose)

    def dma(self, q, out, in_, reads=(), writes=(), sem=None):
        if self.rec is not None:
            self.rec.append((self._dma, (q, out, in_, reads, writes, sem), {}))
            return None
        return self._dma(q, out, in_, reads, writes, sem)

    def _op(self, eng, fn, reads=(), writes=(), **kw):
        self.deps(eng, reads, writes)
        inst = getattr(eng.obj, fn)(**kw)
        ev = eng.signal(inst)
        self.done(ev, reads, writes)
        self.ninst += 1
        return ev

    def _pe_group(self, mms, reads=(), writes=(), transpose=False):
        eng = self.pe
        self.deps(eng, reads, writes)
        inst = None
        for m in mms:
            if transpose:
                inst = self.nc.tensor.transpose(out=m[0], in_=m[1], identity=m[2])
            else:
                inst = self.nc.tensor.matmul(m[0], lhsT=m[1], rhs=m[2], start=m[3], stop=m[4])
            self.ninst += 1
        ev = eng.signal(inst)
        self.done(ev, reads, writes)
        return ev

    def _dma(self, q, out, in_, reads=(), writes=(), sem=None):
        eng = self.sp if q == "sp" else self.pool
        if sem is None:
            sl = self.slots[q][self.slot_i[q] % len(self.slots[q])]
            self.slot_i[q] += 1
            if sl[1] > 0:
                self.wait(eng, Ev(sl[0], sl[1], id(sl[0])))
        else:
            sl = sem
        self.deps(eng, reads, writes, is_dma=True)
        eng.obj.dma_start(out=out, in_=in_).then_inc(sl[0], 16)
        sl[1] += 16
        ev = Ev(sl[0], sl[1], id(sl[0]))
        self.done(ev, reads, writes)
        self.ninst += 1
        return ev

    def barrier(self):
        evs = []
        for e in self.engs:
            if e.cnt > 0:
                evs.append(Ev(e.sem, e.cnt, id(e.sem)))
        for q in self.slots:
            for sl in self.slots[q]:
                if sl[1] > 0:
                    evs.append(Ev(sl[0], sl[1], id(sl[0])))
        for e in self.engs:
            for ev in evs:
                if ev.key in e.ownkeys:
                    continue
                self.wait(e, ev)

    def bank(self):
        i = self.bank_ids[self.bank_i % len(self.bank_ids)]
        self.bank_i += 1
        return self.banks[i]


class Scope:
    def __init__(self, fw):
        self.fw = fw

    def __enter__(self):
        self.fw.barrier()
        self.es = contextlib.ExitStack()
        self.es.__enter__()
        self.n = 0
        return self

    def sb(self, shape, dtype, name="t"):
        self.n += 1
        self.fw.nsem += 1
        t = self.es.enter_context(self.fw.nc.sbuf_tensor("%s_%d_%d" % (name, self.fw.nsem, self.n), list(shape), dtype))
        return t, Res(name)

    def __exit__(self, *a):
        self.fw.barrier()
        self.es.__exit__(*a)
        return False


class Ring:
    def __init__(self, items):
        self.items, self.i = items, 0

    def next(self):
        it = self.items[self.i % len(self.items)]
        self.i += 1
        return it


def make_consts():
    cols = {}
    parts = []
    off = 0

    def add(name, arr):
        nonlocal off
        a = np.zeros((128, arr.shape[1]), np.float32)
        a[: arr.shape[0]] = arr
        cols[name] = (off, arr.shape[1])
        parts.append(a)
        off += arr.shape[1]

    add("ident", np.eye(128, dtype=np.float32))
    s = np.arange(64)[:, None]
    t = np.arange(64)[None, :]
    add("triM", np.where(s <= t, -1.0 / 16, 0.0).astype(np.float32))
    add("triM2", np.where(s > t, -1.0 / 16, 0.0).astype(np.float32))
    add("chunkind", np.full((64, 2), -1.0 / 16, np.float32))
    add("maskG", np.where(s <= t, 1.0, 0.0).astype(np.float32))
    j = np.arange(128)[:, None]
    s2 = np.arange(128)[None, :]
    add("triNeg", np.where(j >= s2, -1.0, 0.0).astype(np.float32))
    add("onesNeg", np.full((128, 128), -1.0, np.float32))
    tl = np.arange(512)[None, :]
    for k in range(4):
        valid = tl > (j + 128 * k)
        add("m01_%d" % k, valid.astype(np.float32))
    for k in range(4):
        valid = tl > (j + 128 * k)
        add("mneg_%d" % k, np.where(valid, 0.0, NEG).astype(np.float32))
    add("pow2", np.tile((0.5 ** np.arange(1, 33))[None, :], (128, 1)).astype(np.float32))
    return np.concatenate(parts, axis=1), cols


CONSTS, CCOL = make_consts()
NCC = CONSTS.shape[1]

OFF_QA, OFF_CA, OFF_QI, OFF_KI, OFF_WI, OFF_QB, OFF_KB, OFF_VB, OFF_GB, OFF_GK = 0, 1024, 1152, 2176, 2240, 2256, 2768, 3280, 4304, 5328
NFM0 = 19
TMW0 = 3216


def build(S, NSEQ, debug=False, stop=None):
    TOPK = min(256, S // 4)
    NT = S // 128
    G = 512
    NG = S // G
    nc = bass.Bass("TRN2", target_bir_lowering=False)
    es = contextlib.ExitStack()
    fw = FW(nc, es)
    dbgkind = "ExternalOutput" if debug else "Internal"

    def din(name, shape, dt=F32):
        return nc.dram_tensor(name, list(shape), dt, kind="ExternalInput").ap()

    def dscr(name, shape, dt):
        return nc.dram_tensor(name, list(shape), dt, kind=dbgkind).ap()

    x = din("x", [NSEQ, S, D])
    p = din("p", [2, NSEQ, S, PLE])
    gains = din("gains", [7, 128, D])
    smalls = din("smalls", [64, 256])
    cst = din("cst", [128, NCC])
    wgk = din("wgk", [17, 512])
    wsrc = {
        "fm0": din("wfm0", [D, NFM0 * 128]), "tm0": din("wtm0", [D, TMW0]), "out0": din("wout0", [D, D]),
        "in1": din("win1", [D, 3 * D]), "out1": din("wout1", [D, D]),
    }
    for i in range(2):
        wsrc["g%d" % i] = din("wg%d" % i, [D, FFH])
        wsrc["u%d" % i] = din("wu%d" % i, [D, FFH])
        wsrc["d%d" % i] = din("wd%d" % i, [FFH, D])
        wsrc["pg%d" % i] = din("wpg%d" % i, [D, D])
        wsrc["pe%d" % i] = din("wpe%d" % i, [PLE, D])
    out = nc.dram_tensor("out", [NSEQ, S, D], F32, kind="ExternalOutput").ap()

    wb = {}
    wres = {}
    for k, a in wsrc.items():
        wb[k] = nc.dram_tensor("wb_" + k, list(a.shape), BF16, kind="Internal").ap()
        wres[k] = Res("w" + k, multi=True)
    h = dscr("h", [NSEQ, S, D], F32)
    fm0 = dscr("fm0", [NSEQ, NFM0, 128, S], BF16)
    tm0 = dscr("tm0", [NSEQ, S, TMW0], F32)
    o0 = dscr("o0", [NSEQ, S, D], BF16)
    fm1 = dscr("fm1", [NSEQ, 32, 128, S], BF16)
    tm1 = dscr("tm1", [NSEQ, S, D], BF16)
    oT1 = dscr("oT1", [NSEQ, 16, 128, S], BF16)
    R = {k: [Res(k + str(i), multi=True) for i in range(NSEQ)] for k in ("h", "fm0", "tm0", "o0", "fm1", "tm1", "oT1", "out")}

    if debug:
        dbg_score = dscr("dbg_score", [NT, 128, S], F32)
        dbg_mask = dscr("dbg_mask", [NT, 128, S], BF16)
        dbg_r = Res("dbg", multi=True)
    for i in range(8):
        t = es.enter_context(nc.psum_tensor("bank%d" % i, [128, 512], F32))
        fw.banks.append((t, Res("bank%d" % i)))

    cs = es.enter_context(nc.sbuf_tensor("cs", [128, NCC], BF16))
    cs_res = Res("cs")
    ident = None

    def C(name, rows=128):
        o, n = CCOL[name]
        return cs[0:rows, o:o + n]

    fw.dma("pool", out=cs[:], in_=cst[:, :], writes=[cs_res])
    conv_done = set()
    conv_sems = {}

    def conv_pieces(keys):
        out_ = []
        for k in keys:
            if k in conv_done:
                continue
            conv_done.add(k)
            a = wsrc[k]
            conv_sems[k] = [fw.newsem("cv" + k), 0]
            rows = a.shape[0]
            rb = 256
            for r0 in range(0, rows, rb):
                r1 = min(rows, r0 + rb)
                out_.append((k, r0, r1))
        return out_

    def conv_issue(piece):
        k, r0, r1 = piece
        fw.dma("pool", out=wb[k][r0:r1, :], in_=wsrc[k][r0:r1, :], writes=[wres[k]], sem=conv_sems[k])

    def convert(keys):
        for pc in conv_pieces(keys):
            conv_issue(pc)

    convert(["fm0", "tm0"])

    def load_gain(sc_t, sc_res, idx):
        fw.dma("sp", out=sc_t[:], in_=gains[idx, :, :], writes=[sc_res])

    class Dense:
        def __init__(self, sc, evs=True, small=False):
            self.hres, self.hres_r = [], []
            for i in range(4):
                t, r = sc.sb([128, D], F32, "hres")
                self.hres.append(t)
                self.hres_r.append(r)
            self.gain, self.gain_r = sc.sb([128, D], F32, "gain")
            if small:
                self.xn = Ring([sc.sb([128, D], BF16, "xn") for _ in range(2)])
                self.junk = Ring(list(self.xn.items))
            else:
                self.xn = Ring([sc.sb([128, D], BF16, "xn") for _ in range(2)])
                self.junk = Ring([sc.sb([128, D], BF16, "junk") for _ in range(1)])
            self.ss = Ring([sc.sb([128, 2], F32, "ss") for _ in range(4)])
            self.xnT, self.xnT_r = sc.sb([128, 16, G], BF16, "xnT")
            self.wbuf = Ring([sc.sb([128, 16, 512], BF16, "wbuf") for _ in range(2 if small else 3)])
            if evs:
                self.ev32 = Ring([sc.sb([128, 512], F32, "ev32") for _ in range(3)])
                self.ev16 = Ring([sc.sb([128, 512], BF16, "ev16") for _ in range(3)])

    def rms_rstd(dn, src, src_r, nparts, width, ss, ss_r, junk, junk_r):
        fw.op(fw.dve, "memset", writes=[ss_r], ap=ss[0:nparts, 0:1], constant=0.0)
        fw.op(fw.act, "activation", reads=[src_r], writes=[junk_r, ss_r], out=junk, in_=src, func=AF.Square,
              accum_out=ss[0:nparts, 0:1])
        fw.op(fw.dve, "tensor_scalar", reads=[], writes=[ss_r], out=ss[0:nparts, 0:1], in0=ss[0:nparts, 0:1],
              scalar1=1.0 / width, scalar2=EPS, op0=ALU.mult, op1=ALU.add)
        fw.op(fw.act, "sqrt", reads=[], writes=[ss_r], out=ss[0:nparts, 0:1], in_=ss[0:nparts, 0:1])
        fw.op(fw.dve, "reciprocal", reads=[], writes=[ss_r], out=ss[0:nparts, 0:1], in_=ss[0:nparts, 0:1])

    tog = [0]

    def evac_copy(out_ap, in_ap, reads, writes):
        tog[0] ^= 1
        if tog[0]:
            fw.op(fw.act, "copy", reads=reads, writes=writes, out=out_ap, in_=in_ap)
        else:
            fw.op(fw.dve, "tensor_copy", reads=reads, writes=writes, out=out_ap, in_=in_ap)

    def transpose_to(dn, src_bf, src_r, dstT, dst_r, col0):
        for half in range(2):
            bt, br = fw.bank()
            pt = bt[:].bitcast(BF16)
            mms = []
            for j in range(8):
                kc = half * 8 + j
                mms.append((pt[:, j * 128:(j + 1) * 128], src_bf[:, kc * 128:(kc + 1) * 128], C("ident")))
            fw.pe_group(mms, reads=[src_r, cs_res], writes=[br], transpose=True)
            evac_copy(dstT[:, half * 8:half * 8 + 8, col0:col0 + 128],
                      pt[:, 0:1024].rearrange("p (j c) -> p j c", j=8), [br], [dst_r])

    def norm_tile(dn, src, src_r, dstT, dst_r, col0):
        ss, ss_r = dn.ss.next()
        junk, junk_r = dn.junk.next()
        xn, xn_r = dn.xn.next()
        rms_rstd(dn, src[:], src_r, 128, D, ss, ss_r, junk[:], junk_r)
        fw.op(fw.dve, "scalar_tensor_tensor", reads=[src_r, ss_r, dn.gain_r], writes=[xn_r], out=xn[:], in0=src[:],
              scalar=ss[:, 0:1], in1=dn.gain[:], op0=ALU.mult, op1=ALU.mult)
        transpose_to(dn, xn, xn_r, dstT, dst_r, col0)

    def load_w(dn, key, r0, nkc, c0, cw):
        wt, wr = dn.wbuf.next()
        fw.dma("sp", out=wt[:, 0:nkc, 0:cw],
               in_=wb[key][r0:r0 + nkc * 128, c0:c0 + cw].rearrange("(kc p) c -> p kc c", p=128),
               reads=[wres[key]], writes=[wr])
        return wt, wr

    def proj_tm(dn, aT, aT_r, KC, KT, key, N, ntt, epilogue, c_start=0, r_off=0):
        nkt = KC // KT
        for c0 in range(c_start, N, 512):
            cw = min(512, N - c0)
            if nkt == 1:
                wt, wr = load_w(dn, key, r_off, KT, c0, cw)
                for tt in range(ntt):
                    bt, br = fw.bank()
                    mms = [(bt[:, 0:cw], aT[:, kc, tt * 128:(tt + 1) * 128], wt[:, kc, 0:cw], kc == 0, kc == KT - 1) for kc in range(KT)]
                    fw.pe_group(mms, reads=[aT_r, wr], writes=[br])
                    epilogue(c0, cw, tt, bt, br)
                continue
            bks = [fw.bank() for _ in range(ntt)]
            for kt in range(nkt):
                wt, wr = load_w(dn, key, r_off + kt * KT * 128, KT, c0, cw)
                for tt in range(ntt):
                    bt, br = bks[tt]
                    mms = [(bt[:, 0:cw], aT[:, kt * KT + kc, tt * 128:(tt + 1) * 128], wt[:, kc, 0:cw],
                            kt == 0 and kc == 0, kt == nkt - 1 and kc == KT - 1) for kc in range(KT)]
                    fw.pe_group(mms, reads=[aT_r, wr], writes=[br])
            for tt in range(ntt):
                epilogue(c0, cw, tt, bks[tt][0], bks[tt][1])

    def proj_fm(dn, aT, aT_r, KC, key, nchunks, T, epilogue, chunk0=0):
        for cb in range(chunk0, nchunks, 4):
            ncb = min(4, nchunks - cb)
            wt, wr = load_w(dn, key, 0, KC, cb * 128, ncb * 128)
            for cc in range(ncb):
                bt, br = fw.bank()
                mms = [(bt[:, 0:T], wt[:, kc, cc * 128:(cc + 1) * 128], aT[:, kc, 0:T], kc == 0, kc == KC - 1)
                       for kc in range(KC)]
                fw.pe_group(mms, reads=[aT_r, wr], writes=[br])
                epilogue(cb + cc, bt, br)

    def phase_inproj0(sc, b):
        if True:
            dn = Dense(sc, small=(len(fw.cur_banks) < 8))
            load_gain(dn.gain, dn.gain_r, 0)
            for g in range(NG):
                for tt in range(4):
                    fw.dma("sp", out=dn.hres[tt][:], in_=x[b, g * G + tt * 128:g * G + (tt + 1) * 128, :],
                           writes=[dn.hres_r[tt]])
                    norm_tile(dn, dn.hres[tt], dn.hres_r[tt], dn.xnT, dn.xnT_r, tt * 128)

                def ep_fm(chunk, bt, br):
                    st, sr = dn.ev16.next()
                    evac_copy(st[:, 0:G], bt[:, 0:G], [br], [sr])
                    fw.dma("pool", out=fm0[b, chunk, :, g * G:(g + 1) * G], in_=st[:, 0:G], reads=[sr], writes=[R["fm0"][b]])
                proj_fm(dn, dn.xnT, dn.xnT_r, 16, "fm0", NFM0, G, ep_fm)

                def ep_tm(c0, cw, tt, bt, br):
                    st, sr = dn.ev32.next()
                    evac_copy(st[:, 0:cw], bt[:, 0:cw], [br], [sr])
                    fw.dma("pool", out=tm0[b, g * G + tt * 128:g * G + (tt + 1) * 128, c0:c0 + cw], in_=st[:, 0:cw],
                           reads=[sr], writes=[R["tm0"][b]])
                proj_tm(dn, dn.xnT, dn.xnT_r, 16, 16, "tm0", TMW0, 4, ep_tm)

    def phase_dsa(sc, b):
        if True:
            qiT, qiT_r = sc.sb([128, 8, S], BF16, "qiT")
            kiT, kiT_r = sc.sb([128, S], BF16, "kiT")
            qaT, qaT_r = sc.sb([128, 8, S], BF16, "qaT")
            cT, cT_r = sc.sb([128, S], BF16, "cT")
            ctm, ctm_r = sc.sb([128, NT, 130], BF16, "ctm")
            wi, wi_r = sc.sb([128, NT, 16], F32, "wi")
            id32, id32_r = sc.sb([128, 128], F32, "id32")
            scores = Ring([sc.sb([128, S], F32, "score") for _ in range(4)])
            NIT = 26
            bis = Ring([(sc.sb([128, NIT + 2], F32, "thr"), sc.sb([128, NIT + 2], F32, "Dst"), sc.sb([128, NIT + 2], F32, "sums"),
                         sc.sb([128, 4], F32, "bmisc")) for _ in range(2)])
            sjunk = Ring([sc.sb([128, S], BF16, "sjunk") for _ in range(2)])
            dgs = Ring([sc.sb([128, 16, 128], BF16, "dg") for _ in range(2)])
            mx = Ring([sc.sb([128, 8], F32, "mx") for _ in range(2)])
            relu = Ring([sc.sb([128, 512], BF16, "relu") for _ in range(4)])
            masks = Ring([sc.sb([128, S], BF16, "mask") for _ in range(4)])
            pending_att = []

            def pump(k):
                for _ in range(k):
                    while pending_att:
                        try:
                            next(pending_att[0])
                            break
                        except StopIteration:
                            pending_att.pop(0)
            maskT, maskT_r = sc.sb([128, NT, 128], BF16, "maskT")
            Eb = Ring([sc.sb([128, 4, 128], BF16, "E") for _ in range(4)])
            Pb = Ring([sc.sb([128, 4, 128], BF16, "P") for _ in range(4)])
            att_i = [0]
            rden, rden_r = sc.sb([128, 8], F32, "rden")
            oa = Ring([sc.sb([128, 1024], BF16, "oa") for _ in range(3)])
            rd = [R["fm0"][b]]
            fw.dma("sp", out=qiT[:], in_=fm0[b, 9:17].rearrange("c p s -> p c s"), reads=rd, writes=[qiT_r])
            fw.dma("sp", out=kiT[:], in_=fm0[b, 17], reads=rd, writes=[kiT_r])
            fw.dma("sp", out=wi[:], in_=tm0[b, :, 128:144].rearrange("(j p) c -> p j c", p=128),
                   reads=[R["tm0"][b]], writes=[wi_r])
            o_id, n_id = CCOL["ident"]
            fw.dma("sp", out=id32[:], in_=cst[:, o_id:o_id + n_id], writes=[id32_r])
            fw.dma("sp", out=qaT[:], in_=fm0[b, 0:8].rearrange("c p s -> p c s"), reads=rd, writes=[qaT_r])
            fw.dma("sp", out=cT[:], in_=fm0[b, 8], reads=rd, writes=[cT_r])
            fw.op(fw.dve, "memset", writes=[ctm_r], ap=ctm[:, :, 128:130], constant=1.0)
            fw.dma("pool", out=ctm[:, :, 0:128], in_=tm0[b, :, 0:128].rearrange("(j p) c -> p j c", p=128),
                   reads=[R["tm0"][b]], writes=[ctm_r])
            pieces = conv_pieces(["out0", "g0", "u0", "d0", "pg0", "pe0", "in1", "out1", "g1", "u1", "d1", "pg1", "pe1"])
            per_blk = -(-len(pieces) // max(1, NT - 1))
            fw.bank_ids = [6, 7]

            def indexer(n):
                t0 = n * 128
                L = t0 + 128
                score, score_r = scores.next()
                dg, dg_r = dgs.next()
                fw.op(fw.dve, "tensor_tensor", reads=[id32_r, wi_r], writes=[dg_r], out=dg[:],
                      in0=id32[:, :].unsqueeze(1).to_broadcast([128, 16, 128]),
                      in1=wi[:, n, :].unsqueeze(2).to_broadcast([128, 16, 128]), op=ALU.mult)
                sb_, sb_r = fw.banks[5]
                for s0 in range(0, L, 512):
                    wn = min(512, L - s0)
                    pend = None
                    for hh in range(17):
                        cur = None
                        if hh < 16:
                            m, e = hh // 2, hh % 2
                            bt, br = fw.bank()
                            fw.pe_group([(bt[:, 0:wn], qiT[64 * e:64 * e + 64, m, t0:t0 + 128], kiT[64 * e:64 * e + 64, s0:s0 + wn],
                                          True, True)], reads=[qiT_r, kiT_r], writes=[br])
                            rl, rl_r = relu.next()
                            if hh % 3 != 2:
                                fw.op(fw.act, "activation", reads=[br], writes=[rl_r], out=rl[:, 0:wn], in_=bt[:, 0:wn], func=AF.Relu)
                            else:
                                fw.op(fw.dve, "tensor_scalar", reads=[br], writes=[rl_r], out=rl[:, 0:wn], in0=bt[:, 0:wn],
                                      scalar1=0.0, scalar2=None, op0=ALU.max)
                            cur = (hh, rl, rl_r)
                        if pend is not None:
                            ph, prl, prl_r = pend
                            fw.pe_group([(sb_[:, 0:wn], dg[:, ph, :], prl[:, 0:wn], ph == 0, ph == 15)], reads=[dg_r, prl_r], writes=[sb_r])
                        pend = cur
                    fw.op(fw.act, "copy", reads=[sb_r], writes=[score_r], out=score[:, s0:s0 + wn], in_=sb_[:, 0:wn])
                return score, score_r

            def select_group(blks):
                outm = {}
                todo = []
                for (n, score, score_r) in blks:
                    L = n * 128 + 128
                    mask, mask_r = masks.next()
                    outm[n] = (mask, mask_r)
                    fw.op(fw.dve, "memset", writes=[score_r], ap=score[0:64, L - 64:L], constant=-BIG)
                    if L > TOPK:
                        (thr, thr_r), (Dt, D_r), (sums, sums_r), (bm, bm_r) = bis.next()
                        m8, m8_r = mx.next()
                        fw.op(fw.dve, "max", reads=[score_r], writes=[m8_r], out=m8[:], in_=score[:, 0:L])
                        fw.op(fw.dve, "tensor_reduce", reads=[score_r], writes=[bm_r], out=bm[:, 0:1], in_=score[:, 0:L - 64],
                              axis=mybir.AxisListType.X, op=ALU.min)
                        fw.op(fw.dve, "tensor_tensor", reads=[m8_r], writes=[bm_r], out=bm[:, 1:2], in0=m8[:, 0:1], in1=bm[:, 0:1], op=ALU.subtract)
                        fw.op(fw.dve, "tensor_scalar", reads=[bm_r, cs_res], writes=[D_r], out=Dt[:, 0:NIT + 2], in0=C("pow2")[:, 0:NIT + 2],
                              scalar1=bm[:, 1:2], scalar2=None, op0=ALU.mult)
                        fw.op(fw.dve, "memset", writes=[sums_r], ap=sums[:], constant=0.0)
                        fw.op(fw.dve, "tensor_tensor", reads=[bm_r, D_r], writes=[thr_r], out=thr[:, 0:1], in0=bm[:, 0:1], in1=Dt[:, 0:1], op=ALU.add)
                        todo.append((n, L, score, score_r, mask, mask_r, thr, thr_r, Dt, D_r, sums, sums_r, bm, bm_r))
                    else:
                        fw.op(fw.dve, "memset", writes=[mask_r], ap=mask[:, 0:L], constant=1.0)
                        fw.op(fw.dve, "memset", writes=[mask_r], ap=mask[0:64, L - 64:L], constant=0.0)
                for i in range(NIT):
                    for (n, L, score, score_r, mask, mask_r, thr, thr_r, Dt, D_r, sums, sums_r, bm, bm_r) in todo:
                        jk, jk_r = sjunk.next()
                        fw.op(fw.act, "activation", reads=[score_r, thr_r], writes=[jk_r, sums_r], out=jk[:, 0:L], in_=score[:, 0:L],
                              func=AF.Sign, scale=-1.0, bias=thr[:, i:i + 1], accum_out=sums[:, i:i + 1])
                    for (n, L, score, score_r, mask, mask_r, thr, thr_r, Dt, D_r, sums, sums_r, bm, bm_r) in todo:
                        fw.op(fw.dve, "tensor_scalar", reads=[sums_r], writes=[bm_r], out=bm[:, 2:3], in0=sums[:, i:i + 1],
                              scalar1=float(L - 2 * TOPK), scalar2=0.5, op0=ALU.is_le, op1=ALU.subtract)
                        fw.op(fw.dve, "scalar_tensor_tensor", reads=[bm_r, D_r], writes=[thr_r], out=thr[:, i + 1:i + 2], in0=Dt[:, i:i + 1],
                              scalar=bm[:, 2:3], in1=thr[:, i:i + 1], op0=ALU.mult, op1=ALU.add)
                    pump(3)
                for (n, L, score, score_r, mask, mask_r, thr, thr_r, Dt, D_r, sums, sums_r, bm, bm_r) in todo:
                    fw.op(fw.dve, "tensor_scalar", reads=[score_r, thr_r, D_r], writes=[mask_r], out=mask[:, 0:L], in0=score[:, 0:L],
                          scalar1=Dt[:, NIT:NIT + 1], scalar2=thr[:, NIT:NIT + 1], op0=ALU.add, op1=ALU.is_ge)
                if debug:
                    for (n, score, score_r) in blks:
                        mask, mask_r = outm[n]
                        fw.dma("pool", out=dbg_score[n, :, :], in_=score[:, :], reads=[score_r], writes=[dbg_r])
                        fw.dma("pool", out=dbg_mask[n, :, :], in_=mask[:, :], reads=[mask_r], writes=[dbg_r])
                return outm

            def attend(n, mask, mask_r):
                t0 = n * 128
                for j0 in range(0, n + 1, 8):
                    nj = min(8, n + 1 - j0)
                    bt, br = fw.bank()
                    pt = bt[:].bitcast(BF16)
                    fw.pe_group([(pt[:, jj * 128:(jj + 1) * 128], mask[:, (j0 + jj) * 128:(j0 + jj + 1) * 128], C("ident"))
                                 for jj in range(nj)], reads=[mask_r, cs_res], writes=[br], transpose=True)
                    evac_copy(maskT[:, j0:j0 + nj, :], pt[:, 0:nj * 128].rearrange("p (j c) -> p j c", j=nj), [br], [maskT_r])
                    yield
                oat, oat_r = oa.next()
                for half in range(2):
                    stt = {}

                    def s1_(j):
                        E, E_r = Eb.next()
                        P, P_r = Pb.next()
                        bt, br = fw.banks[4 + (att_i[0] % 2)]
                        att_i[0] += 1
                        fw.pe_group([(bt[:, :].rearrange("p (h t) -> p h t", h=4), cT[:, j * 128:(j + 1) * 128], qaT[:, half * 4:half * 4 + 4, t0:t0 + 128], True, True)],
                                    reads=[cT_r, qaT_r], writes=[br])
                        fw.op(fw.act, "activation", reads=[br], writes=[E_r], out=E[:],
                              in_=bt[:, :].rearrange("p (h t) -> p h t", h=4), func=AF.Exp, scale=float(128 ** -0.5))
                        fw.op(fw.dve, "tensor_tensor", reads=[E_r, maskT_r], writes=[P_r],
                              out=P[:], in0=E[:], in1=maskT[:, j:j + 1, :].to_broadcast([128, 4, 128]), op=ALU.mult)
                        stt[j] = (P, P_r)

                    def s2_(j):
                        P, P_r = stt.pop(j)
                        for h4 in range(4):
                            ob, ob_r = fw.banks[h4]
                            fw.pe_group([(ob[:, 0:130], P[:, h4, :], ctm[:, j, :], j == 0, j == n)],
                                        reads=[P_r, ctm_r], writes=[ob_r])

                    for j in range(n + 1 + 2):
                        if j <= n:
                            s1_(j)
                        if 0 <= j - 2 <= n:
                            s2_(j - 2)
                        yield
                    for h4 in range(4):
                        hd = half * 4 + h4
                        ob, ob_r = fw.banks[h4]
                        fw.op(fw.dve, "reciprocal", reads=[ob_r], writes=[rden_r], out=rden[:, hd:hd + 1], in_=ob[:, 128:129])
                        fw.op(fw.act, "mul", reads=[ob_r, rden_r], writes=[oat_r], out=oat[:, hd * 128:(hd + 1) * 128],
                              in_=ob[:, 0:128], mul=rden[:, hd:hd + 1])
                fw.dma("pool", out=o0[b, t0:t0 + 128, 0:1024], in_=oat[:], reads=[oat_r], writes=[R["o0"][b]])

            GS = 2
            groups = [list(range(g0, min(NT, g0 + GS))) for g0 in range(0, NT, GS)]
            pend = [(n,) + tuple(indexer(n)) for n in groups[0]]
            for gi, grp in enumerate(groups):
                cur = pend
                if gi + 1 < len(groups):
                    pend = [(n,) + tuple(indexer(n)) for n in groups[gi + 1]]
                for _ in range(per_blk * len(grp)):
                    if pieces:
                        conv_issue(pieces.pop(0))
                ms = select_group(cur)
                for n in grp:
                    pending_att.append(attend(n, *ms[n]))
            pump(100000)
            while pieces:
                conv_issue(pieces.pop(0))

    def phase_gla(sc, b):
        NCH = S // 64
        if True:
            Sf, Sf_r = sc.sb([128, 4, 256], F32, "Sf")
            Sb, Sb_r = sc.sb([128, 4, 256], BF16, "Sb")
            wg, wg_r = sc.sb([32, 512], BF16, "wgk")
            gn, gn_r = sc.sb([64, 256], F32, "gn")
            gkT, gkT_r = sc.sb([32, S], BF16, "gkT")
            qk = Ring([sc.sb([64, 1024], F32, "qk") for _ in range(2)])
            vb = Ring([sc.sb([64, 1024], BF16, "vb") for _ in range(2)])
            gb = Ring([sc.sb([64, 1024], F32, "gb") for _ in range(1)])
            e1 = Ring([sc.sb([64, 512], F32, "e1") for _ in range(1)])
            sp_ = Ring([sc.sb([64, 512], BF16, "sp") for _ in range(2)])
            eG = Ring([sc.sb([64, 3, 512], F32, "eG") for _ in range(1)])
            dec = Ring([sc.sb([128, 4], F32, "dec") for _ in range(2)])
            qkd = Ring([sc.sb([64, 3, 512], BF16, "qkd") for _ in range(2)])
            qkT = Ring([sc.sb([128, 8, 64], BF16, "qkT") for _ in range(2)])
            attm = Ring([sc.sb([64, 4, 64], BF16, "attm") for _ in range(2)])
            ssr = Ring([sc.sb([64, 4], F32, "ssr") for _ in range(2)])
            junk = Ring([sc.sb([64, 256], BF16, "gjunk") for _ in range(2)])
            on = Ring([sc.sb([64, 1024], F32, "on") for _ in range(1)])
            sg = Ring([sc.sb([64, 1024], F32, "sg") for _ in range(1)])
            ob = Ring([sc.sb([64, 1024], BF16, "ob") for _ in range(1)])
            fw.op(fw.dve, "memset", writes=[Sf_r], ap=Sf[:], constant=0.0)
            fw.op(fw.dve, "memset", writes=[Sb_r], ap=Sb[:], constant=0.0)
            fw.op(fw.dve, "memset", writes=[gkT_r], ap=gkT[:], constant=1.0)
            fw.dma("pool", out=wg[0:17, :], in_=wgk[:, :], writes=[wg_r])
            fw.dma("sp", out=gn[:], in_=smalls[:, :], writes=[gn_r])
            fw.dma("sp", out=gkT[0:16, :], in_=fm0[b, 18, 0:16, :], reads=[R["fm0"][b]], writes=[gkT_r])
            sc_q = float(128 ** -0.5)
            for ci in range(NCH):
                t0 = ci * 64
                qkt, qkt_r = qk.next()
                vbt, vbt_r = vb.next()
                gbt, gbt_r = gb.next()
                fw.dma("sp", out=qkt[:], in_=tm0[b, t0:t0 + 64, 144:1168], reads=[R["tm0"][b]], writes=[qkt_r])
                fw.dma("pool", out=vbt[:], in_=tm0[b, t0:t0 + 64, 1168:2192], reads=[R["tm0"][b]], writes=[vbt_r])
                fw.dma("sp", out=gbt[:], in_=tm0[b, t0:t0 + 64, 2192:3216], reads=[R["tm0"][b]], writes=[gbt_r])
                bz, bz_r = fw.bank()
                fw.pe_group([(bz[0:64, :], gkT[0:17, t0:t0 + 64], wg[0:17, :], True, True)], reads=[gkT_r, wg_r], writes=[bz_r])
                e1t, e1_r = e1.next()
                spt, sp_r = sp_.next()
                fw.op(fw.act, "activation", reads=[bz_r], writes=[e1_r], out=e1t[:], in_=bz[0:64, :], func=AF.Exp, scale=-1.0)
                fw.op(fw.act, "activation", reads=[e1_r], writes=[sp_r], out=spt[:], in_=e1t[:], func=AF.Ln, bias=1.0)
                bG, bG_r = fw.bank()
                bD, bD_r = fw.bank()
                bL, bL_r = fw.bank()
                fw.pe_group([(bG[0:64, :], C("triM", 64), spt[:], True, True)], reads=[sp_r, cs_res], writes=[bG_r])
                fw.pe_group([(bD[0:64, :], C("triM2", 64), spt[:], True, True)], reads=[sp_r, cs_res], writes=[bD_r])
                fw.pe_group([(bL[:, hh * 2:hh * 2 + 2], spt[:, hh * 128:(hh + 1) * 128], C("chunkind", 64), True, True) for hh in range(4)],
                            reads=[sp_r, cs_res], writes=[bL_r])
                eGt, eG_r = eG.next()
                dct, dc_r = dec.next()
                fw.op(fw.act, "activation", reads=[bG_r], writes=[eG_r], out=eGt[:, 0, :], in_=bG[0:64, :], func=AF.Exp)
                fw.op(fw.act, "activation", reads=[bG_r], writes=[eG_r], out=eGt[:, 1, :], in_=bG[0:64, :], func=AF.Exp, scale=-1.0)
                fw.op(fw.act, "activation", reads=[bD_r], writes=[eG_r], out=eGt[:, 2, :], in_=bD[0:64, :], func=AF.Exp)
                fw.op(fw.act, "activation", reads=[bL_r], writes=[dc_r], out=dct[:, :], in_=bL[:, 0:8].rearrange("p (h t) -> p h t", t=2)[:, :, 0],
                      func=AF.Exp)
                qd, qd_r = qkd.next()
                fw.op(fw.dve, "scalar_tensor_tensor", reads=[qkt_r, eG_r], writes=[qd_r], out=qd[:, 0, :], in0=qkt[:, 0:512], scalar=sc_q,
                      in1=eGt[:, 0, :], op0=ALU.mult, op1=ALU.mult)
                fw.op(fw.dve, "tensor_tensor", reads=[qkt_r, eG_r], writes=[qd_r], out=qd[:, 1, :], in0=qkt[:, 512:1024], in1=eGt[:, 1, :], op=ALU.mult)
                fw.op(fw.pool, "tensor_tensor", reads=[qkt_r, eG_r], writes=[qd_r], out=qd[:, 2, :], in0=qkt[:, 512:1024], in1=eGt[:, 2, :], op=ALU.mult)
                bt, br = fw.bank()
                pt = bt[:].bitcast(BF16)
                fw.pe_group([(pt[:, (a * 4 + hh) * 64:(a * 4 + hh + 1) * 64], qd[:, a, hh * 128:(hh + 1) * 128], C("ident", 64)[:, 0:64])
                             for a in range(2) for hh in range(4)], reads=[qd_r, cs_res], writes=[br], transpose=True)
                qT, qT_r = qkT.next()
                evac_copy(qT[:], pt[:, 0:512].rearrange("p (j c) -> p j c", j=8), [br], [qT_r])
                ba, ba_r = fw.bank()
                fw.pe_group([(ba[0:64, hh * 64:(hh + 1) * 64], qT[:, 4 + hh, :], qT[:, hh, :], True, True) for hh in range(4)],
                            reads=[qT_r], writes=[ba_r])
                am, am_r = attm.next()
                for hh in range(4):
                    fw.op(fw.dve, "tensor_tensor", reads=[ba_r, cs_res], writes=[am_r], out=am[:, hh, :], in0=ba[0:64, hh * 64:(hh + 1) * 64],
                          in1=C("maskG", 64), op=ALU.mult)
                bo = [fw.bank(), fw.bank()]
                for hh in range(4):
                    bt2, br2 = bo[hh // 2]
                    cs0 = (hh % 2) * 256
                    fw.pe_group([(bt2[0:64, cs0:cs0 + 256], am[:, hh, :], vbt[:, hh * 256:(hh + 1) * 256], True, False),
                                 (bt2[0:64, cs0:cs0 + 256], qT[:, hh, :], Sb[:, hh, :], False, True)],
                                reads=[am_r, vbt_r, qT_r, Sb_r], writes=[br2])
                bkv = [fw.bank(), fw.bank()]
                for hh in range(4):
                    bt3, br3 = bkv[hh // 2]
                    cs0 = (hh % 2) * 256
                    fw.pe_group([(bt3[:, cs0:cs0 + 256], qd[:, 2, hh * 128:(hh + 1) * 128], vbt[:, hh * 256:(hh + 1) * 256], True, True)],
                                reads=[qd_r, vbt_r], writes=[br3])
                for hh in range(4):
                    bt3, br3 = bkv[hh // 2]
                    cs0 = (hh % 2) * 256
                    fw.op(fw.dve, "scalar_tensor_tensor", reads=[br3, dc_r], writes=[Sf_r], out=Sf[:, hh, :], in0=Sf[:, hh, :],
                          scalar=dct[:, hh:hh + 1], in1=bt3[:, cs0:cs0 + 256], op0=ALU.mult, op1=ALU.add)
                fw.op(fw.act, "copy", reads=[Sf_r], writes=[Sb_r], out=Sb[:], in_=Sf[:])
                sst, ss_r = ssr.next()
                fw.op(fw.dve, "memset", writes=[ss_r], ap=sst[:], constant=0.0)
                for hh in range(4):
                    bt2, br2 = bo[hh // 2]
                    cs0 = (hh % 2) * 256
                    jk, jk_r = junk.next()
                    fw.op(fw.act, "activation", reads=[br2], writes=[jk_r, ss_r], out=jk[:], in_=bt2[0:64, cs0:cs0 + 256], func=AF.Square,
                          accum_out=sst[:, hh:hh + 1])
                fw.op(fw.dve, "tensor_scalar", writes=[ss_r], out=sst[:], in0=sst[:], scalar1=1.0 / 256, scalar2=EPS, op0=ALU.mult, op1=ALU.add)
                fw.op(fw.act, "sqrt", writes=[ss_r], out=sst[:], in_=sst[:])
                fw.op(fw.dve, "reciprocal", writes=[ss_r], out=sst[:], in_=sst[:])
                ont, on_r = on.next()
                for hh in range(4):
                    bt2, br2 = bo[hh // 2]
                    cs0 = (hh % 2) * 256
                    fw.op(fw.dve, "scalar_tensor_tensor", reads=[br2, ss_r, gn_r], writes=[on_r], out=ont[:, hh * 256:(hh + 1) * 256],
                          in0=bt2[0:64, cs0:cs0 + 256], scalar=sst[:, hh:hh + 1], in1=gn[:], op0=ALU.mult, op1=ALU.mult)
                sgt, sg_r = sg.next()
                fw.op(fw.act, "activation", reads=[gbt_r], writes=[sg_r], out=sgt[:], in_=gbt[:], func=AF.Silu)
                obt, ob_r = ob.next()
                fw.op(fw.pool, "tensor_tensor", reads=[on_r, sg_r], writes=[ob_r], out=obt[:], in0=ont[:], in1=sgt[:], op=ALU.mult)
                fw.dma("pool", out=o0[b, t0:t0 + 64, 1024:2048], in_=obt[:], reads=[ob_r], writes=[R["o0"][b]])

    def phase_inproj1(sc, b):
        if True:
            convert(["in1", "out1", "g1", "u1", "d1", "pg1", "pe1"])
            dn = Dense(sc)
            load_gain(dn.gain, dn.gain_r, 1)
            sc_q = float(128 ** -0.5)
            for g in range(NG):
                for tt in range(4):
                    fw.dma("sp", out=dn.hres[tt][:], in_=h[b, g * G + tt * 128:g * G + (tt + 1) * 128, :],
                           reads=[R["h"][b]], writes=[dn.hres_r[tt]])
                    norm_tile(dn, dn.hres[tt], dn.hres_r[tt], dn.xnT, dn.xnT_r, tt * 128)

                def ep_fm(chunk, bt, br):
                    st, sr = dn.ev16.next()
                    if chunk < 16:
                        fw.op(fw.act, "mul", reads=[br], writes=[sr], out=st[:, 0:G], in_=bt[:, 0:G], mul=sc_q)
                    else:
                        evac_copy(st[:, 0:G], bt[:, 0:G], [br], [sr])
                    fw.dma("pool", out=fm1[b, chunk, :, g * G:(g + 1) * G], in_=st[:, 0:G], reads=[sr], writes=[R["fm1"][b]])
                proj_fm(dn, dn.xnT, dn.xnT_r, 16, "in1", 32, G, ep_fm)

                def ep_tm(c0, cw, tt, bt, br):
                    st, sr = dn.ev16.next()
                    evac_copy(st[:, 0:cw], bt[:, 0:cw], [br], [sr])
                    fw.dma("pool", out=tm1[b, g * G + tt * 128:g * G + (tt + 1) * 128, c0 - 2 * D:c0 - 2 * D + cw], in_=st[:, 0:cw],
                           reads=[sr], writes=[R["tm1"][b]])
                proj_tm(dn, dn.xnT, dn.xnT_r, 16, 16, "in1", 3 * D, 4, ep_tm, c_start=2 * D)

    def phase_sb(sc, b):
        QB = 512
        if True:
            qT = Ring([sc.sb([128, S], BF16, "qT") for _ in range(2)])
            kT = Ring([sc.sb([128, S], BF16, "kT") for _ in range(2)])
            vv = Ring([sc.sb([128, NT, 128], BF16, "vv") for _ in range(2)])
            Ebuf = Ring([sc.sb([128, 512], F32, "sbE") for _ in range(4)])
            Lp = Ring([sc.sb([128, 512], BF16, "Lp") for _ in range(3)])
            acc, acc_r = sc.sb([128, 512], BF16, "acc")
            Wt = Ring([sc.sb([128, 512], BF16, "Wt") for _ in range(3)])
            Xt = Ring([sc.sb([128, 512], BF16, "Xt") for _ in range(3)])
            ost = Ring([sc.sb([128, 512], BF16, "ost") for _ in range(2)])
            cb = list(fw.cur_banks)
            if len(cb) >= 8:
                b1_banks, b2_bank, bo_bank = [cb[0], cb[1], cb[2]], cb[3], cb[4]
            else:
                b1_banks, b2_bank, bo_bank = [cb[0], cb[1]], cb[2], cb[3]
            heads = {}

            def load_head(hd):
                if hd >= 16 or hd in heads:
                    return
                q_, q_r = qT.next()
                k_, k_r = kT.next()
                v_, v_r = vv.next()
                fw.dma("sp", out=q_[:], in_=fm1[b, hd], reads=[R["fm1"][b]], writes=[q_r])
                fw.dma("sp", out=k_[:], in_=fm1[b, 16 + hd], reads=[R["fm1"][b]], writes=[k_r])
                fw.dma("sp", out=v_[:], in_=tm1[b, :, hd * 128:(hd + 1) * 128].rearrange("(j p) c -> p j c", p=128),
                       reads=[R["tm1"][b]], writes=[v_r])
                heads[hd] = (q_, q_r, k_, k_r, v_, v_r)

            items = []
            for hd in range(16):
                for qb in range(S // QB):
                    for c in range(4 * qb + 3, -1, -1):
                        items.append((hd, qb, c))
            NI = len(items)
            st = {}

            def geo(it):
                hd, qb, c = it
                k_d = c - 4 * qb
                diag = k_d >= 0
                lo = 128 * k_d if diag else 0
                return hd, qb, c, qb * QB, 4 * qb + 3, k_d, diag, lo

            def stage_a(i):
                hd, qb, c, t0, cmax, k_d, diag, lo = geo(items[i])
                load_head(hd)
                q_, q_r, k_, k_r, v_, v_r = heads[hd]
                b1, b1_r = fw.banks[b1_banks[i % len(b1_banks)]]
                fw.pe_group([(b1[:, lo:QB], k_[:, c * 128:(c + 1) * 128], q_[:, t0 + lo:t0 + QB], True, True)], reads=[k_r, q_r], writes=[b1_r])
                Et, E_r = Ebuf.next()
                fw.op(fw.act, "activation", reads=[b1_r], writes=[E_r], out=Et[:, lo:QB], in_=b1[:, lo:QB], func=AF.Exp)
                Lt, L_r = Lp.next()
                fw.op(fw.act, "activation", reads=[E_r], writes=[L_r], out=Lt[:, lo:QB], in_=Et[:, lo:QB], func=AF.Ln, bias=1.0)
                if diag:
                    fw.op(fw.pool, "tensor_tensor", reads=[L_r, cs_res], writes=[L_r], out=Lt[:, lo:QB], in0=Lt[:, lo:QB],
                          in1=C("m01_%d" % k_d)[:, lo:QB], op=ALU.mult)
                st[i] = {"E": (Et, E_r), "L": (Lt, L_r)}

            def stage_b(i):
                hd, qb, c, t0, cmax, k_d, diag, lo = geo(items[i])
                Lt, L_r = st[i]["L"]
                b2, b2_r = fw.banks[b2_bank]
                mms = [(b2[:, lo:QB], C("triNeg"), Lt[:, lo:QB], True, (c == cmax) and not diag)]
                rds = [L_r, cs_res]
                if c != cmax:
                    mms.append((b2[:, lo:QB], C("onesNeg"), acc[:, lo:QB], False, not diag))
                    rds.append(acc_r)
                if diag:
                    mms.append((b2[:, lo:QB], C("ident"), C("mneg_%d" % k_d)[:, lo:QB], False, True))
                fw.pe_group(mms, reads=rds, writes=[b2_r])
                if c == cmax:
                    fw.op(fw.dve, "memset", writes=[acc_r], ap=acc[:], constant=0.0)
                if c > 0:
                    fw.op(fw.dve, "tensor_tensor", reads=[L_r], writes=[acc_r], out=acc[:, lo:QB], in0=acc[:, lo:QB], in1=Lt[:, lo:QB], op=ALU.add)
                Xtt, X_r = Xt.next()
                fw.op(fw.act, "activation", reads=[b2_r], writes=[X_r], out=Xtt[:, lo:QB], in_=b2[:, lo:QB], func=AF.Exp)
                st[i]["X"] = (Xtt, X_r)

            def stage_c(i):
                hd, qb, c, t0, cmax, k_d, diag, lo = geo(items[i])
                Et, E_r = st[i]["E"]
                Xtt, X_r = st[i]["X"]
                Wtt, W_r = Wt.next()
                fw.op(fw.dve, "tensor_tensor", reads=[X_r, E_r], writes=[W_r], out=Wtt[:, lo:QB], in0=Xtt[:, lo:QB], in1=Et[:, lo:QB], op=ALU.mult)
                st[i]["W"] = (Wtt, W_r)

            def stage_d(i):
                hd, qb, c, t0, cmax, k_d, diag, lo = geo(items[i])
                q_, q_r, k_, k_r, v_, v_r = heads[hd]
                Wtt, W_r = st[i]["W"]
                bo, bo_r = fw.banks[bo_bank]
                fw.pe_group([(bo[:, lo:QB], v_[:, c, :], Wtt[:, lo:QB], c == cmax, c == 0)], reads=[v_r, W_r], writes=[bo_r])
                if c == 0:
                    st_, st_r = ost.next()
                    evac_copy(st_[:], bo[:, :], [bo_r], [st_r])
                    fw.dma("pool", out=oT1[b, hd, :, t0:t0 + QB], in_=st_[:], reads=[st_r], writes=[R["oT1"][b]])
                    load_head(hd + 1)
                del st[i]

            for i in range(NI + 3):
                if i < NI:
                    stage_a(i)
                if 0 <= i - 3 < NI:
                    stage_d(i - 3)
                if 0 <= i - 1 < NI:
                    stage_b(i - 1)
                if 0 <= i - 2 < NI:
                    stage_c(i - 2)

    def phase_post(sc, b, layer):
        if True:
            dn = Dense(sc, evs=False, small=(len(fw.cur_banks) < 8))
            h1T, h1T_r = sc.sb([128, 22, G], BF16, "h1T")
            otile = dn.xn
            sgl = Ring([sc.sb([128, 512], F32, "sgl") for _ in range(2)])
            pt32 = Ring([sc.sb([128, PLE], F32, "pt32") for _ in range(2)])
            pt16 = Ring([sc.sb([128, PLE], BF16, "pt16") for _ in range(2)])
            pT, pT_r = sc.sb([128, 2, G], BF16, "pT")
            wpeb = Ring([sc.sb([128, 2, 512], BF16, "wpe") for _ in range(2)])
            cur_wpe = [None]
            pe_sb = Ring([sc.sb([128, 512], F32, "pesb") for _ in range(1)])
            sig = Ring([sc.sb([128, 512], F32, "sig") for _ in range(1)])
            hsrc = x if layer == 0 else h
            for g in range(NG):
                tok0 = g * G
                if layer == 0:
                    for tt in range(4):
                        ot, ot_r = otile.next()
                        fw.dma("sp", out=ot[:], in_=o0[b, tok0 + tt * 128:tok0 + (tt + 1) * 128, :], reads=[R["o0"][b]], writes=[ot_r])
                        transpose_to(dn, ot, ot_r, dn.xnT, dn.xnT_r, tt * 128)
                else:
                    fw.dma("sp", out=dn.xnT[:], in_=oT1[b, :, :, tok0:tok0 + G].rearrange("c p s -> p c s"),
                           reads=[R["oT1"][b]], writes=[dn.xnT_r])
                for tt in range(4):
                    rds = [R["h"][b]] if layer == 1 else []
                    fw.dma("sp", out=dn.hres[tt][:], in_=hsrc[b, tok0 + tt * 128:tok0 + (tt + 1) * 128, :], reads=rds, writes=[dn.hres_r[tt]])

                def ep_res(c0, cw, tt, bt, br):
                    fw.op(fw.dve, "tensor_tensor", reads=[br], writes=[dn.hres_r[tt]], out=dn.hres[tt][:, c0:c0 + cw],
                          in0=dn.hres[tt][:, c0:c0 + cw], in1=bt[:, 0:cw], op=ALU.add)
                proj_tm(dn, dn.xnT, dn.xnT_r, 16, 16, "out%d" % layer, D, 4, ep_res)
                load_gain(dn.gain, dn.gain_r, 2 + layer)
                for tt in range(4):
                    norm_tile(dn, dn.hres[tt], dn.hres_r[tt], dn.xnT, dn.xnT_r, tt * 128)
                for half in range(2):
                    h0 = half * 22
                    for hb in range(h0, h0 + 22, 4):
                        nb = min(4, h0 + 22 - hb)
                        wgt, wg_r = load_w(dn, "g%d" % layer, 0, 16, hb * 128, nb * 128)
                        wut, wu_r = load_w(dn, "u%d" % layer, 0, 16, hb * 128, nb * 128)
                        for cc in range(nb):
                            bg, bg_r = fw.bank()
                            bu, bu_r = fw.bank()
                            fw.pe_group([(bg[:, 0:G], wgt[:, kc, cc * 128:(cc + 1) * 128], dn.xnT[:, kc, 0:G], kc == 0, kc == 15) for kc in range(16)],
                                        reads=[dn.xnT_r, wg_r], writes=[bg_r])
                            fw.pe_group([(bu[:, 0:G], wut[:, kc, cc * 128:(cc + 1) * 128], dn.xnT[:, kc, 0:G], kc == 0, kc == 15) for kc in range(16)],
                                        reads=[dn.xnT_r, wu_r], writes=[bu_r])
                            s_, s_r = sgl.next()
                            fw.op(fw.act, "activation", reads=[bg_r], writes=[s_r], out=s_[:, 0:G], in_=bg[:, 0:G], func=AF.Silu)
                            fw.op(fw.dve, "tensor_tensor", reads=[s_r, bu_r], writes=[h1T_r], out=h1T[:, hb - h0 + cc, :], in0=s_[:, 0:G], in1=bu[:, 0:G], op=ALU.mult)
                    proj_tm(dn, h1T, h1T_r, 22, 11, "d%d" % layer, D, 4, ep_res, r_off=h0 * 128)
                load_gain(dn.gain, dn.gain_r, 4 + layer)
                for tt in range(4):
                    norm_tile(dn, dn.hres[tt], dn.hres_r[tt], dn.xnT, dn.xnT_r, tt * 128)
                    p32, p32_r = pt32.next()
                    p16, p16_r = pt16.next()
                    fw.dma("sp", out=p32[:], in_=p[layer, b, tok0 + tt * 128:tok0 + (tt + 1) * 128, :], writes=[p32_r])
                    fw.op(fw.pool, "tensor_copy", reads=[p32_r], writes=[p16_r], out=p16[:], in_=p32[:])
                    bt, br = fw.bank()
                    ptb = bt[:].bitcast(BF16)
                    fw.pe_group([(ptb[:, j * 128:(j + 1) * 128], p16[:, j * 128:(j + 1) * 128], C("ident")) for j in range(2)],
                                reads=[p16_r, cs_res], writes=[br], transpose=True)
                    evac_copy(pT[:, :, tt * 128:(tt + 1) * 128], ptb[:, 0:256].rearrange("p (j c) -> p j c", j=2), [br], [pT_r])

                def ep_ple(c0, cw, tt, bt, br):
                    if tt == 0:
                        wpe_t, wpe_r = wpeb.next()
                        fw.dma("sp", out=wpe_t[:, :, 0:cw], in_=wb["pe%d" % layer][:, c0:c0 + cw].rearrange("(kc p) c -> p kc c", p=128),
                               reads=[wres["pe%d" % layer]], writes=[wpe_r])
                        cur_wpe[0] = (wpe_t, wpe_r)
                    wpe_t, wpe_r = cur_wpe[0]
                    b2, b2_r = fw.bank()
                    fw.pe_group([(b2[:, 0:cw], pT[:, kc, tt * 128:(tt + 1) * 128], wpe_t[:, kc, 0:cw], kc == 0, kc == 1) for kc in range(2)],
                                reads=[pT_r, wpe_r], writes=[b2_r])
                    sg_, sg_r = sig.next()
                    fw.op(fw.act, "activation", reads=[br], writes=[sg_r], out=sg_[:, 0:cw], in_=bt[:, 0:cw], func=AF.Sigmoid)
                    pe_, pe_r = pe_sb.next()
                    fw.op(fw.dve, "tensor_tensor", reads=[sg_r, b2_r], writes=[pe_r], out=pe_[:, 0:cw], in0=sg_[:, 0:cw], in1=b2[:, 0:cw], op=ALU.mult)
                    fw.op(fw.pool, "tensor_tensor", reads=[pe_r], writes=[dn.hres_r[tt]], out=dn.hres[tt][:, c0:c0 + cw],
                          in0=dn.hres[tt][:, c0:c0 + cw], in1=pe_[:, 0:cw], op=ALU.add)
                proj_tm(dn, dn.xnT, dn.xnT_r, 16, 16, "pg%d" % layer, D, 4, ep_ple)
                if layer == 0:
                    for tt in range(4):
                        fw.dma("pool", out=h[b, tok0 + tt * 128:tok0 + (tt + 1) * 128, :], in_=dn.hres[tt][:], reads=[dn.hres_r[tt]], writes=[R["h"][b]])
                else:
                    load_gain(dn.gain, dn.gain_r, 6)
                    for tt in range(4):
                        ss, ss_r = dn.ss.next()
                        junk, junk_r = dn.junk.next()
                        rms_rstd(dn, dn.hres[tt][:], dn.hres_r[tt], 128, D, ss, ss_r, junk[:], junk_r)
                        fw.op(fw.dve, "scalar_tensor_tensor", reads=[ss_r, dn.gain_r], writes=[dn.hres_r[tt]], out=dn.hres[tt][:], in0=dn.hres[tt][:],
                              scalar=ss[:, 0:1], in1=dn.gain[:], op0=ALU.mult, op1=ALU.mult)
                        fw.dma("pool", out=out[b, tok0 + tt * 128:tok0 + (tt + 1) * 128, :], in_=dn.hres[tt][:], reads=[dn.hres_r[tt]], writes=[R["out"][b]])

    ALL8 = list(range(8))

    def run_group(streams):
        with Scope(fw) as sc:
            recs = []
            for fn, args, banks in streams:
                fw.rec = []
                fw.bank_ids = list(banks)
                fw.cur_banks = list(banks)
                fw.bank_i = 0
                fn(sc, *args)
                recs.append(fw.rec)
                fw.rec = None
            pos = [0] * len(recs)
            while True:
                best, bf = None, None
                for i, r in enumerate(recs):
                    if pos[i] < len(r):
                        f = pos[i] / float(len(r))
                        if bf is None or f < bf:
                            best, bf = i, f
                if best is None:
                    break
                fnc, a, kw = recs[best][pos[best]]
                fnc(*a, **kw)
                pos[best] += 1
        fw.bank_ids = list(ALL8)

    sched = []
    if NSEQ == 1:
        sched = [("inproj0", [(phase_inproj0, (0,), ALL8)]), ("dsa", [(phase_dsa, (0,), ALL8)]), ("gla", [(phase_gla, (0,), ALL8)]),
                 ("post0", [(phase_post, (0, 0), ALL8)]), ("inproj1", [(phase_inproj1, (0,), ALL8)]), ("sb", [(phase_sb, (0,), ALL8)]),
                 ("post1", [(phase_post, (0, 1), ALL8)])]
    else:
        assert NSEQ == 2
        LO, HI = [0, 1, 2, 3], [4, 5, 6, 7]
        sched = [
            ("a", [(phase_inproj0, (0,), ALL8)]),
            ("b", [(phase_dsa, (0,), ALL8)]),
            ("c", [(phase_inproj0, (1,), LO), (phase_gla, (0,), HI)]),
            ("e", [(phase_post, (0, 0), LO), (phase_gla, (1,), HI)]),
            ("f", [(phase_dsa, (1,), ALL8)]),
            ("g", [(phase_inproj1, (0,), ALL8)]),
            ("h", [(phase_post, (1, 0), LO), (phase_sb, (0,), HI)]),
            ("i", [(phase_inproj1, (1,), ALL8)]),
            ("j", [(phase_post, (0, 1), LO), (phase_sb, (1,), HI)]),
            ("k", [(phase_post, (1, 1), ALL8)]),
        ]
    for name, streams in sched:
        run_group(streams)
        if stop is not None and name == stop:
            break
    fw.barrier()
    es.close()
    return nc


def prep_weights(inp):
    f = lambda a: np.ascontiguousarray(np.asarray(a, dtype=np.float32))
    w0 = f(inp["even_w_in"])[0]
    wfm0 = np.zeros((D, NFM0 * 128), np.float32)
    wfm0[:, 0:1024] = w0[:, OFF_QA:OFF_QA + 1024]
    wfm0[:, 1024:1152] = w0[:, OFF_CA:OFF_CA + 128]
    wfm0[:, 1152:2176] = w0[:, OFF_QI:OFF_QI + 1024]
    wfm0[:, 2176:2240] = w0[:, OFF_KI:OFF_KI + 64]
    wfm0[:, 2240:2304] = w0[:, OFF_KI:OFF_KI + 64]
    wfm0[:, 2304:2320] = w0[:, OFF_GK:OFF_GK + 16]
    wtm0 = np.concatenate([w0[:, OFF_CA:OFF_CA + 128], w0[:, OFF_WI:OFF_WI + 16], w0[:, OFF_QB:OFF_GK]], axis=1)
    assert wtm0.shape[1] == TMW0
    gains = np.stack([f(inp["even_norm"])[0], f(inp["odd_norm"])[0], f(inp["ffn_norm"])[0], f(inp["ffn_norm"])[1],
                      f(inp["ple_norm"])[0], f(inp["ple_norm"])[1], f(inp["final_norm"])], axis=0)
    gains = np.ascontiguousarray(np.broadcast_to(gains[:, None, :], (7, 128, D)))
    smalls = np.ascontiguousarray(np.broadcast_to(f(inp["even_gla_norm"])[0][None, :], (64, 256)))
    wgk = np.concatenate([f(inp["even_w_gk"])[0], f(inp["even_b_gk"])[0][None, :]], axis=0)
    m = {"gains": gains, "smalls": smalls, "cst": CONSTS, "wgk": np.ascontiguousarray(wgk),
         "wfm0": wfm0, "wtm0": np.ascontiguousarray(wtm0), "wout0": f(inp["even_w_out"])[0],
         "win1": f(inp["odd_w_in"])[0], "wout1": f(inp["odd_w_out"])[0]}
    for i in range(2):
        m["wg%d" % i] = f(inp["ffn_w_gate"])[i]
        m["wu%d" % i] = f(inp["ffn_w_up"])[i]
        m["wd%d" % i] = f(inp["ffn_w_down"])[i]
        m["wpg%d" % i] = f(inp["ple_w_gate"])[i]
        m["wpe%d" % i] = f(inp["ple_w_proj"])[i]
    return m


_CACHE = {}


def kernel(**inputs):
    x = np.asarray(inputs["x"], dtype=np.float32)
    p = np.asarray(inputs["p"], dtype=np.float32)
    B, S, _ = x.shape
    ncores = 8
    nseq = B // ncores
    wm = prep_weights(inputs)
    key = (S, nseq)
    if key not in _CACHE:
        _CACHE[key] = build(S, nseq)
    nc = _CACHE[key]
    in_maps = []
    for c in range(ncores):
        m = dict(wm)
        m["x"] = np.ascontiguousarray(x[c * nseq:(c + 1) * nseq])
        m["p"] = np.ascontiguousarray(p[:, c * nseq:(c + 1) * nseq])
        in_maps.append(m)
    res = run_bass_kernel_spmd(nc, in_maps, core_ids=list(range(ncores)))
    return np.concatenate([r["out"] for r in res.results], axis=0).astype(np.float32)
```

```python
import contextlib
import numpy as np
import concourse.bass as bass
import concourse.mybir as mybir
from concourse.bass_utils import run_bass_kernel_spmd

F32 = mybir.dt.float32
BF16 = mybir.dt.bfloat16
AF = mybir.ActivationFunctionType
ALU = mybir.AluOpType

D = 2048
FFH = 5632
PLE = 256
EPS = 1e-6
NEG = -30000.0
BIG = 1.0e30


class Ev:
    __slots__ = ("sem", "val", "key")

    def __init__(self, sem, val, key):
        self.sem, self.val, self.key = sem, val, key


class Res:
    __slots__ = ("w", "r", "name", "multi")

    def __init__(self, name="", multi=False):
        self.w = {}
        self.r = {}
        self.name = name
        self.multi = multi


class Eng:
    def __init__(self, fw, name, obj, is_pe=False):
        self.fw, self.name, self.obj, self.is_pe = fw, name, obj, is_pe
        self.seen = {}
        self.ownkeys = set()
        self._newsem()

    def _newsem(self):
        self.sem = self.fw.newsem(self.name)
        self.ownkeys.add(id(self.sem))
        self.cnt = 0

    def signal(self, inst):
        if self.cnt >= 30000:
            self._newsem()
        self.cnt += 1
        inst.then_inc(self.sem, 1)
        return Ev(self.sem, self.cnt, id(self.sem))


class FW:
    SAME_ENGINE_SYNC = True

    def __init__(self, nc, es):
        self.nc, self.es = nc, es
        self.nsem = 0
        self.pe = Eng(self, "pe", nc.tensor, True)
        self.act = Eng(self, "act", nc.scalar)
        self.dve = Eng(self, "dve", nc.vector)
        self.pool = Eng(self, "pool", nc.gpsimd)
        self.sp = Eng(self, "sp", nc.sync)
        self.engs = [self.pe, self.act, self.dve, self.pool, self.sp]
        self.slots = {}
        for q in ("sp", "pool"):
            self.slots[q] = [[self.newsem("dq%s" % q), 0] for _ in range(12)]
        self.slot_i = {"sp": 0, "pool": 0}
        self.banks = []
        self.bank_i = 0
        self.bank_ids = list(range(8))
        self.ninst = 0
        self.rec = None

    def newsem(self, name):
        self.nsem += 1
        return self.es.enter_context(self.nc.semaphore("s%s%d" % (name, self.nsem)))

    def wait(self, eng, ev):
        if eng.seen.get(ev.key, 0) >= ev.val:
            return
        eng.obj.wait_ge(ev.sem, ev.val)
        eng.seen[ev.key] = ev.val
        self.ninst += 1

    def _w(self, eng, ev, raw, is_dma):
        if ev.key in eng.ownkeys and not is_dma:
            if eng.is_pe or not raw or not self.SAME_ENGINE_SYNC:
                return
        self.wait(eng, ev)

    def deps(self, eng, reads, writes, is_dma=False):
        for r in reads:
            for ev in r.w.values():
                self._w(eng, ev, True, is_dma)
        for w in writes:
            if not w.multi:
                for ev in w.w.values():
                    self._w(eng, ev, True, is_dma)
            for ev in w.r.values():
                self._w(eng, ev, False, is_dma)

    def done(self, ev, reads, writes):
        for r in reads:
            old = r.r.get(ev.key)
            if old is None or old.val < ev.val:
                r.r[ev.key] = ev
        for w in writes:
            if w.multi:
                w.w[ev.key] = ev
            else:
                w.w = {ev.key: ev}
            w.r = {}

    def op(self, eng, fn, reads=(), writes=(), **kw):
        if self.rec is not None:
            self.rec.append((self._op, (eng, fn, reads, writes), kw))
            return None
        return self._op(eng, fn, reads, writes, **kw)

    def pe_group(self, mms, reads=(), writes=(), transpose=False):
        if self.rec is not None:
            self.rec.append((self._pe_group, (mms, reads, writes, transpose), {}))
            return None
        return self._pe_group(mms, reads, writes, transpose)

    def dma(self, q, out, in_, reads=(), writes=(), sem=None):
        if self.rec is not None:
            self.rec.append((self._dma, (q, out, in_, reads, writes, sem), {}))
            return None
        return self._dma(q, out, in_, reads, writes, sem)

    def _op(self, eng, fn, reads=(), writes=(), **kw):
        self.deps(eng, reads, writes)
        inst = getattr(eng.obj, fn)(**kw)
        ev = eng.signal(inst)
        self.done(ev, reads, writes)
        self.ninst += 1
        return ev

    def _pe_group(self, mms, reads=(), writes=(), transpose=False):
        eng = self.pe
        self.deps(eng, reads, writes)
        inst = None
        for m in mms:
            if transpose:
                inst = self.nc.tensor.transpose(out=m[0], in_=m[1], identity=m[2])
            else:
                inst = self.nc.tensor.matmul(m[0], lhsT=m[1], rhs=m[2], start=m[3], stop=m[4])
            self.ninst += 1
        ev = eng.signal(inst)
        self.done(ev, reads, writes)
        return ev

    def _dma(self, q, out, in_, reads=(), writes=(), sem=None):
        eng = self.sp if q == "sp" else self.pool
        if sem is None:
            sl = self.slots[q][self.slot_i[q] % len(self.slots[q])]
            self.slot_i[q] += 1
            if sl[1] > 0:
                self.wait(eng, Ev(sl[0], sl[1], id(sl[0])))
        else:
            sl = sem
        self.deps(eng, reads, writes, is_dma=True)
        eng.obj.dma_start(out=out, in_=in_).then_inc(sl[0], 16)
        sl[1] += 16
        ev = Ev(sl[0], sl[1], id(sl[0]))
        self.done(ev, reads, writes)
        self.ninst += 1
        return ev

    def barrier(self):
        evs = []
        for e in self.engs:
            if e.cnt > 0:
                evs.append(Ev(e.sem, e.cnt, id(e.sem)))
        for q in self.slots:
            for sl in self.slots[q]:
                if sl[1] > 0:
                    evs.append(Ev(sl[0], sl[1], id(sl[0])))
        for e in self.engs:
            for ev in evs:
                if ev.key in e.ownkeys:
                    continue
                self.wait(e, ev)

    def bank(self):
        i = self.bank_ids[self.bank_i % len(self.bank_ids)]
        self.bank_i += 1
        return self.banks[i]


class Scope:
    def __init__(self, fw):
        self.fw = fw

    def __enter__(self):
        self.fw.barrier()
        self.es = contextlib.ExitStack()
        self.es.__enter__()
        self.n = 0
        return self

    def sb(self, shape, dtype, name="t"):
        self.n += 1
        self.fw.nsem += 1
        t = self.es.enter_context(self.fw.nc.sbuf_tensor("%s_%d_%d" % (name, self.fw.nsem, self.n), list(shape), dtype))
        return t, Res(name)

    def __exit__(self, *a):
        self.fw.barrier()
        self.es.__exit__(*a)
        return False


class Ring:
    def __init__(self, items):
        self.items, self.i = items, 0

    def next(self):
        it = self.items[self.i % len(self.items)]
        self.i += 1
        return it


def make_consts():
    cols = {}
    parts = []
    off = 0

    def add(name, arr):
        nonlocal off
        a = np.zeros((128, arr.shape[1]), np.float32)
        a[: arr.shape[0]] = arr
        cols[name] = (off, arr.shape[1])
        parts.append(a)
        off += arr.shape[1]

    add("ident", np.eye(128, dtype=np.float32))
    s = np.arange(64)[:, None]
    t = np.arange(64)[None, :]
    add("triM", np.where(s <= t, -1.0 / 16, 0.0).astype(np.float32))
    add("triM2", np.where(s > t, -1.0 / 16, 0.0).astype(np.float32))
    add("chunkind", np.full((64, 2), -1.0 / 16, np.float32))
    add("maskG", np.where(s <= t, 1.0, 0.0).astype(np.float32))
    j = np.arange(128)[:, None]
    s2 = np.arange(128)[None, :]
    add("triNeg", np.where(j >= s2, -1.0, 0.0).astype(np.float32))
    add("onesNeg", np.full((128, 128), -1.0, np.float32))
    tl = np.arange(512)[None, :]
    for k in range(4):
        valid = tl > (j + 128 * k)
        add("m01_%d" % k, valid.astype(np.float32))
    for k in range(4):
        valid = tl > (j + 128 * k)
        add("mneg_%d" % k, np.where(valid, 0.0, NEG).astype(np.float32))
    add("pow2", np.tile((0.5 ** np.arange(1, 33))[None, :], (128, 1)).astype(np.float32))
    return np.concatenate(parts, axis=1), cols


CONSTS, CCOL = make_consts()
NCC = CONSTS.shape[1]

OFF_QA, OFF_CA, OFF_QI, OFF_KI, OFF_WI, OFF_QB, OFF_KB, OFF_VB, OFF_GB, OFF_GK = 0, 1024, 1152, 2176, 2240, 2256, 2768, 3280, 4304, 5328
NFM0 = 19
TMW0 = 3216


def build(S, NSEQ, debug=False, stop=None):
    TOPK = min(256, S // 4)
    NT = S // 128
    G = 512
    NG = S // G
    nc = bass.Bass("TRN2", target_bir_lowering=False)
    es = contextlib.ExitStack()
    fw = FW(nc, es)
    dbgkind = "ExternalOutput" if debug else "Internal"

    def din(name, shape, dt=F32):
        return nc.dram_tensor(name, list(shape), dt, kind="ExternalInput").ap()

    def dscr(name, shape, dt):
        return nc.dram_tensor(name, list(shape), dt, kind=dbgkind).ap()

    x = din("x", [NSEQ, S, D])
    p = din("p", [2, NSEQ, S, PLE])
    gains = din("gains", [7, 128, D])
    smalls = din("smalls", [64, 256])
    cst = din("cst", [128, NCC])
    wgk = din("wgk", [17, 512])
    wsrc = {
        "fm0": din("wfm0", [D, NFM0 * 128]), "tm0": din("wtm0", [D, TMW0]), "out0": din("wout0", [D, D]),
        "in1": din("win1", [D, 3 * D]), "out1": din("wout1", [D, D]),
    }
    for i in range(2):
        wsrc["g%d" % i] = din("wg%d" % i, [D, FFH])
        wsrc["u%d" % i] = din("wu%d" % i, [D, FFH])
        wsrc["d%d" % i] = din("wd%d" % i, [FFH, D])
        wsrc["pg%d" % i] = din("wpg%d" % i, [D, D])
        wsrc["pe%d" % i] = din("wpe%d" % i, [PLE, D])
    out = nc.dram_tensor("out", [NSEQ, S, D], F32, kind="ExternalOutput").ap()

    wb = {}
    wres = {}
    for k, a in wsrc.items():
        wb[k] = nc.dram_tensor("wb_" + k, list(a.shape), BF16, kind="Internal").ap()
        wres[k] = Res("w" + k, multi=True)
    h = dscr("h", [NSEQ, S, D], F32)
    fm0 = dscr("fm0", [NSEQ, NFM0, 128, S], BF16)
    tm0 = dscr("tm0", [NSEQ, S, TMW0], F32)
    o0 = dscr("o0", [NSEQ, S, D], BF16)
    fm1 = dscr("fm1", [NSEQ, 32, 128, S], BF16)
    tm1 = dscr("tm1", [NSEQ, S, D], BF16)
    oT1 = dscr("oT1", [NSEQ, 16, 128, S], BF16)
    R = {k: [Res(k + str(i), multi=True) for i in range(NSEQ)] for k in ("h", "fm0", "tm0", "o0", "fm1", "tm1", "oT1", "out")}

    if debug:
        dbg_score = dscr("dbg_score", [NT, 128, S], F32)
        dbg_mask = dscr("dbg_mask", [NT, 128, S], BF16)
        dbg_r = Res("dbg", multi=True)
    for i in range(8):
        t = es.enter_context(nc.psum_tensor("bank%d" % i, [128, 512], F32))
        fw.banks.append((t, Res("bank%d" % i)))

    cs = es.enter_context(nc.sbuf_tensor("cs", [128, NCC], BF16))
    cs_res = Res("cs")
    ident = None

    def C(name, rows=128):
        o, n = CCOL[name]
        return cs[0:rows, o:o + n]

    fw.dma("pool", out=cs[:], in_=cst[:, :], writes=[cs_res])
    conv_done = set()
    conv_sems = {}

    def conv_pieces(keys):
        out_ = []
        for k in keys:
            if k in conv_done:
                continue
            conv_done.add(k)
            a = wsrc[k]
            conv_sems[k] = [fw.newsem("cv" + k), 0]
            rows = a.shape[0]
            rb = 256
            for r0 in range(0, rows, rb):
                r1 = min(rows, r0 + rb)
                out_.append((k, r0, r1))
        return out_

    def conv_issue(piece):
        k, r0, r1 = piece
        fw.dma("pool", out=wb[k][r0:r1, :], in_=wsrc[k][r0:r1, :], writes=[wres[k]], sem=conv_sems[k])

    def convert(keys):
        for pc in conv_pieces(keys):
            conv_issue(pc)

    convert(["fm0", "tm0"])

    def load_gain(sc_t, sc_res, idx):
        fw.dma("sp", out=sc_t[:], in_=gains[idx, :, :], writes=[sc_res])

    class Dense:
        def __init__(self, sc, evs=True, small=False):
            self.hres, self.hres_r = [], []
            for i in range(4):
                t, r = sc.sb([128, D], F32, "hres")
                self.hres.append(t)
                self.hres_r.append(r)
            self.gain, self.gain_r = sc.sb([128, D], F32, "gain")
            if small:
                self.xn = Ring([sc.sb([128, D], BF16, "xn") for _ in range(2)])
                self.junk = Ring(list(self.xn.items))
            else:
                self.xn = Ring([sc.sb([128, D], BF16, "xn") for _ in range(2)])
                self.junk = Ring([sc.sb([128, D], BF16, "junk") for _ in range(1)])
            self.ss = Ring([sc.sb([128, 2], F32, "ss") for _ in range(4)])
            self.xnT, self.xnT_r = sc.sb([128, 16, G], BF16, "xnT")
            self.wbuf = Ring([sc.sb([128, 16, 512], BF16, "wbuf") for _ in range(2 if small else 3)])
            if evs:
                self.ev32 = Ring([sc.sb([128, 512], F32, "ev32") for _ in range(3)])
                self.ev16 = Ring([sc.sb([128, 512], BF16, "ev16") for _ in range(3)])

    def rms_rstd(dn, src, src_r, nparts, width, ss, ss_r, junk, junk_r):
        fw.op(fw.dve, "memset", writes=[ss_r], ap=ss[0:nparts, 0:1], constant=0.0)
        fw.op(fw.act, "activation", reads=[src_r], writes=[junk_r, ss_r], out=junk, in_=src, func=AF.Square,
              accum_out=ss[0:nparts, 0:1])
        fw.op(fw.dve, "tensor_scalar", reads=[], writes=[ss_r], out=ss[0:nparts, 0:1], in0=ss[0:nparts, 0:1],
              scalar1=1.0 / width, scalar2=EPS, op0=ALU.mult, op1=ALU.add)
        fw.op(fw.act, "sqrt", reads=[], writes=[ss_r], out=ss[0:nparts, 0:1], in_=ss[0:nparts, 0:1])
        fw.op(fw.dve, "reciprocal", reads=[], writes=[ss_r], out=ss[0:nparts, 0:1], in_=ss[0:nparts, 0:1])

    tog = [0]

    def evac_copy(out_ap, in_ap, reads, writes):
        tog[0] ^= 1
        if tog[0]:
            fw.op(fw.act, "copy", reads=reads, writes=writes, out=out_ap, in_=in_ap)
        else:
            fw.op(fw.dve, "tensor_copy", reads=reads, writes=writes, out=out_ap, in_=in_ap)

    def transpose_to(dn, src_bf, src_r, dstT, dst_r, col0):
        for half in range(2):
            bt, br = fw.bank()
            pt = bt[:].bitcast(BF16)
            mms = []
            for j in range(8):
                kc = half * 8 + j
                mms.append((pt[:, j * 128:(j + 1) * 128], src_bf[:, kc * 128:(kc + 1) * 128], C("ident")))
            fw.pe_group(mms, reads=[src_r, cs_res], writes=[br], transpose=True)
            evac_copy(dstT[:, half * 8:half * 8 + 8, col0:col0 + 128],
                      pt[:, 0:1024].rearrange("p (j c) -> p j c", j=8), [br], [dst_r])

    def norm_tile(dn, src, src_r, dstT, dst_r, col0):
        ss, ss_r = dn.ss.next()
        junk, junk_r = dn.junk.next()
        xn, xn_r = dn.xn.next()
        rms_rstd(dn, src[:], src_r, 128, D, ss, ss_r, junk[:], junk_r)
        fw.op(fw.dve, "scalar_tensor_tensor", reads=[src_r, ss_r, dn.gain_r], writes=[xn_r], out=xn[:], in0=src[:],
              scalar=ss[:, 0:1], in1=dn.gain[:], op0=ALU.mult, op1=ALU.mult)
        transpose_to(dn, xn, xn_r, dstT, dst_r, col0)

    def load_w(dn, key, r0, nkc, c0, cw):
        wt, wr = dn.wbuf.next()
        fw.dma("sp", out=wt[:, 0:nkc, 0:cw],
               in_=wb[key][r0:r0 + nkc * 128, c0:c0 + cw].rearrange("(kc p) c -> p kc c", p=128),
               reads=[wres[key]], writes=[wr])
        return wt, wr

    def proj_tm(dn, aT, aT_r, KC, KT, key, N, ntt, epilogue, c_start=0, r_off=0):
        nkt = KC // KT
        for c0 in range(c_start, N, 512):
            cw = min(512, N - c0)
            if nkt == 1:
                wt, wr = load_w(dn, key, r_off, KT, c0, cw)
                for tt in range(ntt):
                    bt, br = fw.bank()
                    mms = [(bt[:, 0:cw], aT[:, kc, tt * 128:(tt + 1) * 128], wt[:, kc, 0:cw], kc == 0, kc == KT - 1) for kc in range(KT)]
                    fw.pe_group(mms, reads=[aT_r, wr], writes=[br])
                    epilogue(c0, cw, tt, bt, br)
                continue
            bks = [fw.bank() for _ in range(ntt)]
            for kt in range(nkt):
                wt, wr = load_w(dn, key, r_off + kt * KT * 128, KT, c0, cw)
                for tt in range(ntt):
                    bt, br = bks[tt]
                    mms = [(bt[:, 0:cw], aT[:, kt * KT + kc, tt * 128:(tt + 1) * 128], wt[:, kc, 0:cw],
                            kt == 0 and kc == 0, kt == nkt - 1 and kc == KT - 1) for kc in range(KT)]
                    fw.pe_group(mms, reads=[aT_r, wr], writes=[br])
            for tt in range(ntt):
                epilogue(c0, cw, tt, bks[tt][0], bks[tt][1])

    def proj_fm(dn, aT, aT_r, KC, key, nchunks, T, epilogue, chunk0=0):
        for cb in range(chunk0, nchunks, 4):
            ncb = min(4, nchunks - cb)
            wt, wr = load_w(dn, key, 0, KC, cb * 128, ncb * 128)
            for cc in range(ncb):
                bt, br = fw.bank()
                mms = [(bt[:, 0:T], wt[:, kc, cc * 128:(cc + 1) * 128], aT[:, kc, 0:T], kc == 0, kc == KC - 1)
                       for kc in range(KC)]
                fw.pe_group(mms, reads=[aT_r, wr], writes=[br])
                epilogue(cb + cc, bt, br)

    def phase_inproj0(sc, b):
        if True:
            dn = Dense(sc, small=(len(fw.cur_banks) < 8))
            load_gain(dn.gain, dn.gain_r, 0)
            for g in range(NG):
                for tt in range(4):
                    fw.dma("sp", out=dn.hres[tt][:], in_=x[b, g * G + tt * 128:g * G + (tt + 1) * 128, :],
                           writes=[dn.hres_r[tt]])
                    norm_tile(dn, dn.hres[tt], dn.hres_r[tt], dn.xnT, dn.xnT_r, tt * 128)

                def ep_fm(chunk, bt, br):
                    st, sr = dn.ev16.next()
                    evac_copy(st[:, 0:G], bt[:, 0:G], [br], [sr])
                    fw.dma("pool", out=fm0[b, chunk, :, g * G:(g + 1) * G], in_=st[:, 0:G], reads=[sr], writes=[R["fm0"][b]])
                proj_fm(dn, dn.xnT, dn.xnT_r, 16, "fm0", NFM0, G, ep_fm)

                def ep_tm(c0, cw, tt, bt, br):
                    st, sr = dn.ev32.next()
                    evac_copy(st[:, 0:cw], bt[:, 0:cw], [br], [sr])
                    fw.dma("pool", out=tm0[b, g * G + tt * 128:g * G + (tt + 1) * 128, c0:c0 + cw], in_=st[:, 0:cw],
                           reads=[sr], writes=[R["tm0"][b]])
                proj_tm(dn, dn.xnT, dn.xnT_r, 16, 16, "tm0", TMW0, 4, ep_tm)

    def phase_dsa(sc, b):
        if True:
            qiT, qiT_r = sc.sb([128, 8, S], BF16, "qiT")
            kiT, kiT_r = sc.sb([128, S], BF16, "kiT")
            qaT, qaT_r = sc.sb([128, 8, S], BF16, "qaT")
            cT, cT_r = sc.sb([128, S], BF16, "cT")
            ctm, ctm_r = sc.sb([128, NT, 130], BF16, "ctm")
            wi, wi_r = sc.sb([128, NT, 16], F32, "wi")
            id32, id32_r = sc.sb([128, 128], F32, "id32")
            scores = Ring([sc.sb([128, S], F32, "score") for _ in range(4)])
            NIT = 26
            bis = Ring([(sc.sb([128, NIT + 2], F32, "thr"), sc.sb([128, NIT + 2], F32, "Dst"), sc.sb([128, NIT + 2], F32, "sums"),
                         sc.sb([128, 4], F32, "bmisc")) for _ in range(2)])
            sjunk = Ring([sc.sb([128, S], BF16, "sjunk") for _ in range(2)])
            dgs = Ring([sc.sb([128, 16, 128], BF16, "dg") for _ in range(2)])
            mx = Ring([sc.sb([128, 8], F32, "mx") for _ in range(2)])
            relu = Ring([sc.sb([128, 512], BF16, "relu") for _ in range(4)])
            masks = Ring([sc.sb([128, S], BF16, "mask") for _ in range(4)])
            pending_att = []

            def pump(k):
                for _ in range(k):
                    while pending_att:
                        try:
                            next(pending_att[0])
                            break
                        except StopIteration:
                            pending_att.pop(0)
            maskT, maskT_r = sc.sb([128, NT, 128], BF16, "maskT")
            Eb = Ring([sc.sb([128, 4, 128], BF16, "E") for _ in range(4)])
            Pb = Ring([sc.sb([128, 4, 128], BF16, "P") for _ in range(4)])
            att_i = [0]
            rden, rden_r = sc.sb([128, 8], F32, "rden")
            oa = Ring([sc.sb([128, 1024], BF16, "oa") for _ in range(3)])
            rd = [R["fm0"][b]]
            fw.dma("sp", out=qiT[:], in_=fm0[b, 9:17].rearrange("c p s -> p c s"), reads=rd, writes=[qiT_r])
            fw.dma("sp", out=kiT[:], in_=fm0[b, 17], reads=rd, writes=[kiT_r])
            fw.dma("sp", out=wi[:], in_=tm0[b, :, 128:144].rearrange("(j p) c -> p j c", p=128),
                   reads=[R["tm0"][b]], writes=[wi_r])
            o_id, n_id = CCOL["ident"]
            fw.dma("sp", out=id32[:], in_=cst[:, o_id:o_id + n_id], writes=[id32_r])
            fw.dma("sp", out=qaT[:], in_=fm0[b, 0:8].rearrange("c p s -> p c s"), reads=rd, writes=[qaT_r])
            fw.dma("sp", out=cT[:], in_=fm0[b, 8], reads=rd, writes=[cT_r])
            fw.op(fw.dve, "memset", writes=[ctm_r], ap=ctm[:, :, 128:130], constant=1.0)
            fw.dma("pool", out=ctm[:, :, 0:128], in_=tm0[b, :, 0:128].rearrange("(j p) c -> p j c", p=128),
                   reads=[R["tm0"][b]], writes=[ctm_r])
            pieces = conv_pieces(["out0", "g0", "u0", "d0", "pg0", "pe0", "in1", "out1", "g1", "u1", "d1", "pg1", "pe1"])
            per_blk = -(-len(pieces) // max(1, NT - 1))
            fw.bank_ids = [6, 7]

            def indexer(n):
                t0 = n * 128
                L = t0 + 128
                score, score_r = scores.next()
                dg, dg_r = dgs.next()
                fw.op(fw.dve, "tensor_tensor", reads=[id32_r, wi_r], writes=[dg_r], out=dg[:],
                      in0=id32[:, :].unsqueeze(1).to_broadcast([128, 16, 128]),
                      in1=wi[:, n, :].unsqueeze(2).to_broadcast([128, 16, 128]), op=ALU.mult)
                sb_, sb_r = fw.banks[5]
                for s0 in range(0, L, 512):
                    wn = min(512, L - s0)
                    pend = None
                    for hh in range(17):
                        cur = None
                        if hh < 16:
                            m, e = hh // 2, hh % 2
                            bt, br = fw.bank()
                            fw.pe_group([(bt[:, 0:wn], qiT[64 * e:64 * e + 64, m, t0:t0 + 128], kiT[64 * e:64 * e + 64, s0:s0 + wn],
                                          True, True)], reads=[qiT_r, kiT_r], writes=[br])
                            rl, rl_r = relu.next()
                            if hh % 3 != 2:
                                fw.op(fw.act, "activation", reads=[br], writes=[rl_r], out=rl[:, 0:wn], in_=bt[:, 0:wn], func=AF.Relu)
                            else:
                                fw.op(fw.dve, "tensor_scalar", reads=[br], writes=[rl_r], out=rl[:, 0:wn], in0=bt[:, 0:wn],
                                      scalar1=0.0, scalar2=None, op0=ALU.max)
                            cur = (hh, rl, rl_r)
                        if pend is not None:
                            ph, prl, prl_r = pend
                            fw.pe_group([(sb_[:, 0:wn], dg[:, ph, :], prl[:, 0:wn], ph == 0, ph == 15)], reads=[dg_r, prl_r], writes=[sb_r])
                        pend = cur
                    fw.op(fw.act, "copy", reads=[sb_r], writes=[score_r], out=score[:, s0:s0 + wn], in_=sb_[:, 0:wn])
                return score, score_r

            def select_group(blks):
                outm = {}
                todo = []
                for (n, score, score_r) in blks:
                    L = n * 128 + 128
                    mask, mask_r = masks.next()
                    outm[n] = (mask, mask_r)
                    fw.op(fw.dve, "memset", writes=[score_r], ap=score[0:64, L - 64:L], constant=-BIG)
                    if L > TOPK:
                        (thr, thr_r), (Dt, D_r), (sums, sums_r), (bm, bm_r) = bis.next()
                        m8, m8_r = mx.next()
                        fw.op(fw.dve, "max", reads=[score_r], writes=[m8_r], out=m8[:], in_=score[:, 0:L])
                        fw.op(fw.dve, "tensor_reduce", reads=[score_r], writes=[bm_r], out=bm[:, 0:1], in_=score[:, 0:L - 64],
                              axis=mybir.AxisListType.X, op=ALU.min)
                        fw.op(fw.dve, "tensor_tensor", reads=[m8_r], writes=[bm_r], out=bm[:, 1:2], in0=m8[:, 0:1], in1=bm[:, 0:1], op=ALU.subtract)
                        fw.op(fw.dve, "tensor_scalar", reads=[bm_r, cs_res], writes=[D_r], out=Dt[:, 0:NIT + 2], in0=C("pow2")[:, 0:NIT + 2],
                              scalar1=bm[:, 1:2], scalar2=None, op0=ALU.mult)
                        fw.op(fw.dve, "memset", writes=[sums_r], ap=sums[:], constant=0.0)
                        fw.op(fw.dve, "tensor_tensor", reads=[bm_r, D_r], writes=[thr_r], out=thr[:, 0:1], in0=bm[:, 0:1], in1=Dt[:, 0:1], op=ALU.add)
                        todo.append((n, L, score, score_r, mask, mask_r, thr, thr_r, Dt, D_r, sums, sums_r, bm, bm_r))
                    else:
                        fw.op(fw.dve, "memset", writes=[mask_r], ap=mask[:, 0:L], constant=1.0)
                        fw.op(fw.dve, "memset", writes=[mask_r], ap=mask[0:64, L - 64:L], constant=0.0)
                for i in range(NIT):
                    for (n, L, score, score_r, mask, mask_r, thr, thr_r, Dt, D_r, sums, sums_r, bm, bm_r) in todo:
                        jk, jk_r = sjunk.next()
                        fw.op(fw.act, "activation", reads=[score_r, thr_r], writes=[jk_r, sums_r], out=jk[:, 0:L], in_=score[:, 0:L],
                              func=AF.Sign, scale=-1.0, bias=thr[:, i:i + 1], accum_out=sums[:, i:i + 1])
                    for (n, L, score, score_r, mask, mask_r, thr, thr_r, Dt, D_r, sums, sums_r, bm, bm_r) in todo:
                        fw.op(fw.dve, "tensor_scalar", reads=[sums_r], writes=[bm_r], out=bm[:, 2:3], in0=sums[:, i:i + 1],
                              scalar1=float(L - 2 * TOPK), scalar2=0.5, op0=ALU.is_le, op1=ALU.subtract)
                        fw.op(fw.dve, "scalar_tensor_tensor", reads=[bm_r, D_r], writes=[thr_r], out=thr[:, i + 1:i + 2], in0=Dt[:, i:i + 1],
                              scalar=bm[:, 2:3], in1=thr[:, i:i + 1], op0=ALU.mult, op1=ALU.add)
                    pump(3)
                for (n, L, score, score_r, mask, mask_r, thr, thr_r, Dt, D_r, sums, sums_r, bm, bm_r) in todo:
                    fw.op(fw.dve, "tensor_scalar", reads=[score_r, thr_r, D_r], writes=[mask_r], out=mask[:, 0:L], in0=score[:, 0:L],
                          scalar1=Dt[:, NIT:NIT + 1], scalar2=thr[:, NIT:NIT + 1], op0=ALU.add, op1=ALU.is_ge)
                if debug:
                    for (n, score, score_r) in blks:
                        mask, mask_r = outm[n]
                        fw.dma("pool", out=dbg_score[n, :, :], in_=score[:, :], reads=[score_r], writes=[dbg_r])
                        fw.dma("pool", out=dbg_mask[n, :, :], in_=mask[:, :], reads=[mask_r], writes=[dbg_r])
                return outm

            def attend(n, mask, mask_r):
                t0 = n * 128
                for j0 in range(0, n + 1, 8):
                    nj = min(8, n + 1 - j0)
                    bt, br = fw.bank()
                    pt = bt[:].bitcast(BF16)
                    fw.pe_group([(pt[:, jj * 128:(jj + 1) * 128], mask[:, (j0 + jj) * 128:(j0 + jj + 1) * 128], C("ident"))
                                 for jj in range(nj)], reads=[mask_r, cs_res], writes=[br], transpose=True)
                    evac_copy(maskT[:, j0:j0 + nj, :], pt[:, 0:nj * 128].rearrange("p (j c) -> p j c", j=nj), [br], [maskT_r])
                    yield
                oat, oat_r = oa.next()
                for half in range(2):
                    stt = {}

                    def s1_(j):
                        E, E_r = Eb.next()
                        P, P_r = Pb.next()
                        bt, br = fw.banks[4 + (att_i[0] % 2)]
                        att_i[0] += 1
                        fw.pe_group([(bt[:, :].rearrange("p (h t) -> p h t", h=4), cT[:, j * 128:(j + 1) * 128], qaT[:, half * 4:half * 4 + 4, t0:t0 + 128], True, True)],
                                    reads=[cT_r, qaT_r], writes=[br])
                        fw.op(fw.act, "activation", reads=[br], writes=[E_r], out=E[:],
                              in_=bt[:, :].rearrange("p (h t) -> p h t", h=4), func=AF.Exp, scale=float(128 ** -0.5))
                        fw.op(fw.dve, "tensor_tensor", reads=[E_r, maskT_r], writes=[P_r],
                              out=P[:], in0=E[:], in1=maskT[:, j:j + 1, :].to_broadcast([128, 4, 128]), op=ALU.mult)
                        stt[j] = (P, P_r)

                    def s2_(j):
                        P, P_r = stt.pop(j)
                        for h4 in range(4):
                            ob, ob_r = fw.banks[h4]
                            fw.pe_group([(ob[:, 0:130], P[:, h4, :], ctm[:, j, :], j == 0, j == n)],
                                        reads=[P_r, ctm_r], writes=[ob_r])

                    for j in range(n + 1 + 2):
                        if j <= n:
                            s1_(j)
                        if 0 <= j - 2 <= n:
                            s2_(j - 2)
                        yield
                    for h4 in range(4):
                        hd = half * 4 + h4
                        ob, ob_r = fw.banks[h4]
                        fw.op(fw.dve, "reciprocal", reads=[ob_r], writes=[rden_r], out=rden[:, hd:hd + 1], in_=ob[:, 128:129])
                        fw.op(fw.act, "mul", reads=[ob_r, rden_r], writes=[oat_r], out=oat[:, hd * 128:(hd + 1) * 128],
                              in_=ob[:, 0:128], mul=rden[:, hd:hd + 1])
                fw.dma("pool", out=o0[b, t0:t0 + 128, 0:1024], in_=oat[:], reads=[oat_r], writes=[R["o0"][b]])

            GS = 2
            groups = [list(range(g0, min(NT, g0 + GS))) for g0 in range(0, NT, GS)][::-1]
            pend = [(n,) + tuple(indexer(n)) for n in groups[0]]
            for gi, grp in enumerate(groups):
                cur = pend
                if gi + 1 < len(groups):
                    pend = [(n,) + tuple(indexer(n)) for n in groups[gi + 1]]
                for _ in range(per_blk * len(grp)):
                    if pieces:
                        conv_issue(pieces.pop(0))
                ms = select_group(cur)
                for n in grp:
                    pending_att.append(attend(n, *ms[n]))
            pump(100000)
            while pieces:
                conv_issue(pieces.pop(0))

    def phase_gla(sc, b):
        NCH = S // 64
        if True:
            Sf, Sf_r = sc.sb([128, 4, 256], F32, "Sf")
            Sb, Sb_r = sc.sb([128, 4, 256], BF16, "Sb")
            wg, wg_r = sc.sb([32, 512], BF16, "wgk")
            gn, gn_r = sc.sb([64, 256], F32, "gn")
            gkT, gkT_r = sc.sb([32, S], BF16, "gkT")
            qk = Ring([sc.sb([64, 1024], F32, "qk") for _ in range(2)])
            vb = Ring([sc.sb([64, 1024], BF16, "vb") for _ in range(2)])
            gb = Ring([sc.sb([64, 1024], F32, "gb") for _ in range(1)])
            e1 = Ring([sc.sb([64, 512], F32, "e1") for _ in range(1)])
            sp_ = Ring([sc.sb([64, 512], BF16, "sp") for _ in range(2)])
            eG = Ring([sc.sb([64, 3, 512], F32, "eG") for _ in range(1)])
            dec = Ring([sc.sb([128, 4], F32, "dec") for _ in range(2)])
            qkd = Ring([sc.sb([64, 3, 512], BF16, "qkd") for _ in range(2)])
            qkT = Ring([sc.sb([128, 8, 64], BF16, "qkT") for _ in range(2)])
            attm = Ring([sc.sb([64, 4, 64], BF16, "attm") for _ in range(2)])
            ssr = Ring([sc.sb([64, 4], F32, "ssr") for _ in range(2)])
            junk = Ring([sc.sb([64, 256], BF16, "gjunk") for _ in range(2)])
            on = Ring([sc.sb([64, 1024], F32, "on") for _ in range(1)])
            sg = Ring([sc.sb([64, 1024], F32, "sg") for _ in range(1)])
            ob = Ring([sc.sb([64, 1024], BF16, "ob") for _ in range(1)])
            fw.op(fw.dve, "memset", writes=[Sf_r], ap=Sf[:], constant=0.0)
            fw.op(fw.dve, "memset", writes=[Sb_r], ap=Sb[:], constant=0.0)
            fw.op(fw.dve, "memset", writes=[gkT_r], ap=gkT[:], constant=1.0)
            fw.dma("pool", out=wg[0:17, :], in_=wgk[:, :], writes=[wg_r])
            fw.dma("sp", out=gn[:], in_=smalls[:, :], writes=[gn_r])
            fw.dma("sp", out=gkT[0:16, :], in_=fm0[b, 18, 0:16, :], reads=[R["fm0"][b]], writes=[gkT_r])
            sc_q = float(128 ** -0.5)
            for ci in range(NCH):
                t0 = ci * 64
                qkt, qkt_r = qk.next()
                vbt, vbt_r = vb.next()
                gbt, gbt_r = gb.next()
                fw.dma("sp", out=qkt[:], in_=tm0[b, t0:t0 + 64, 144:1168], reads=[R["tm0"][b]], writes=[qkt_r])
                fw.dma("pool", out=vbt[:], in_=tm0[b, t0:t0 + 64, 1168:2192], reads=[R["tm0"][b]], writes=[vbt_r])
                fw.dma("sp", out=gbt[:], in_=tm0[b, t0:t0 + 64, 2192:3216], reads=[R["tm0"][b]], writes=[gbt_r])
                bz, bz_r = fw.bank()
                fw.pe_group([(bz[0:64, :], gkT[0:17, t0:t0 + 64], wg[0:17, :], True, True)], reads=[gkT_r, wg_r], writes=[bz_r])
                e1t, e1_r = e1.next()
                spt, sp_r = sp_.next()
                fw.op(fw.act, "activation", reads=[bz_r], writes=[e1_r], out=e1t[:], in_=bz[0:64, :], func=AF.Exp, scale=-1.0)
                fw.op(fw.act, "activation", reads=[e1_r], writes=[sp_r], out=spt[:], in_=e1t[:], func=AF.Ln, bias=1.0)
                bG, bG_r = fw.bank()
                bD, bD_r = fw.bank()
                bL, bL_r = fw.bank()
                fw.pe_group([(bG[0:64, :], C("triM", 64), spt[:], True, True)], reads=[sp_r, cs_res], writes=[bG_r])
                fw.pe_group([(bD[0:64, :], C("triM2", 64), spt[:], True, True)], reads=[sp_r, cs_res], writes=[bD_r])
                fw.pe_group([(bL[:, hh * 2:hh * 2 + 2], spt[:, hh * 128:(hh + 1) * 128], C("chunkind", 64), True, True) for hh in range(4)],
                            reads=[sp_r, cs_res], writes=[bL_r])
                eGt, eG_r = eG.next()
                dct, dc_r = dec.next()
                fw.op(fw.act, "activation", reads=[bG_r], writes=[eG_r], out=eGt[:, 0, :], in_=bG[0:64, :], func=AF.Exp)
                fw.op(fw.act, "activation", reads=[bG_r], writes=[eG_r], out=eGt[:, 1, :], in_=bG[0:64, :], func=AF.Exp, scale=-1.0)
                fw.op(fw.act, "activation", reads=[bD_r], writes=[eG_r], out=eGt[:, 2, :], in_=bD[0:64, :], func=AF.Exp)
                fw.op(fw.act, "activation", reads=[bL_r], writes=[dc_r], out=dct[:, :], in_=bL[:, 0:8].rearrange("p (h t) -> p h t", t=2)[:, :, 0],
                      func=AF.Exp)
                qd, qd_r = qkd.next()
                fw.op(fw.dve, "scalar_tensor_tensor", reads=[qkt_r, eG_r], writes=[qd_r], out=qd[:, 0, :], in0=qkt[:, 0:512], scalar=sc_q,
                      in1=eGt[:, 0, :], op0=ALU.mult, op1=ALU.mult)
                fw.op(fw.dve, "tensor_tensor", reads=[qkt_r, eG_r], writes=[qd_r], out=qd[:, 1, :], in0=qkt[:, 512:1024], in1=eGt[:, 1, :], op=ALU.mult)
                fw.op(fw.pool, "tensor_tensor", reads=[qkt_r, eG_r], writes=[qd_r], out=qd[:, 2, :], in0=qkt[:, 512:1024], in1=eGt[:, 2, :], op=ALU.mult)
                bt, br = fw.bank()
                pt = bt[:].bitcast(BF16)
                fw.pe_group([(pt[:, (a * 4 + hh) * 64:(a * 4 + hh + 1) * 64], qd[:, a, hh * 128:(hh + 1) * 128], C("ident", 64)[:, 0:64])
                             for a in range(2) for hh in range(4)], reads=[qd_r, cs_res], writes=[br], transpose=True)
                qT, qT_r = qkT.next()
                evac_copy(qT[:], pt[:, 0:512].rearrange("p (j c) -> p j c", j=8), [br], [qT_r])
                ba, ba_r = fw.bank()
                fw.pe_group([(ba[0:64, hh * 64:(hh + 1) * 64], qT[:, 4 + hh, :], qT[:, hh, :], True, True) for hh in range(4)],
                            reads=[qT_r], writes=[ba_r])
                am, am_r = attm.next()
                for hh in range(4):
                    fw.op(fw.dve, "tensor_tensor", reads=[ba_r, cs_res], writes=[am_r], out=am[:, hh, :], in0=ba[0:64, hh * 64:(hh + 1) * 64],
                          in1=C("maskG", 64), op=ALU.mult)
                bo = [fw.bank(), fw.bank()]
                for hh in range(4):
                    bt2, br2 = bo[hh // 2]
                    cs0 = (hh % 2) * 256
                    fw.pe_group([(bt2[0:64, cs0:cs0 + 256], am[:, hh, :], vbt[:, hh * 256:(hh + 1) * 256], True, False),
                                 (bt2[0:64, cs0:cs0 + 256], qT[:, hh, :], Sb[:, hh, :], False, True)],
                                reads=[am_r, vbt_r, qT_r, Sb_r], writes=[br2])
                bkv = [fw.bank(), fw.bank()]
                for hh in range(4):
                    bt3, br3 = bkv[hh // 2]
                    cs0 = (hh % 2) * 256
                    fw.pe_group([(bt3[:, cs0:cs0 + 256], qd[:, 2, hh * 128:(hh + 1) * 128], vbt[:, hh * 256:(hh + 1) * 256], True, True)],
                                reads=[qd_r, vbt_r], writes=[br3])
                for hh in range(4):
                    bt3, br3 = bkv[hh // 2]
                    cs0 = (hh % 2) * 256
                    fw.op(fw.dve, "scalar_tensor_tensor", reads=[br3, dc_r], writes=[Sf_r], out=Sf[:, hh, :], in0=Sf[:, hh, :],
                          scalar=dct[:, hh:hh + 1], in1=bt3[:, cs0:cs0 + 256], op0=ALU.mult, op1=ALU.add)
                fw.op(fw.act, "copy", reads=[Sf_r], writes=[Sb_r], out=Sb[:], in_=Sf[:])
                sst, ss_r = ssr.next()
                fw.op(fw.dve, "memset", writes=[ss_r], ap=sst[:], constant=0.0)
                for hh in range(4):
                    bt2, br2 = bo[hh // 2]
                    cs0 = (hh % 2) * 256
                    jk, jk_r = junk.next()
                    fw.op(fw.act, "activation", reads=[br2], writes=[jk_r, ss_r], out=jk[:], in_=bt2[0:64, cs0:cs0 + 256], func=AF.Square,
                          accum_out=sst[:, hh:hh + 1])
                fw.op(fw.dve, "tensor_scalar", writes=[ss_r], out=sst[:], in0=sst[:], scalar1=1.0 / 256, scalar2=EPS, op0=ALU.mult, op1=ALU.add)
                fw.op(fw.act, "sqrt", writes=[ss_r], out=sst[:], in_=sst[:])
                fw.op(fw.dve, "reciprocal", writes=[ss_r], out=sst[:], in_=sst[:])
                ont, on_r = on.next()
                for hh in range(4):
                    bt2, br2 = bo[hh // 2]
                    cs0 = (hh % 2) * 256
                    fw.op(fw.dve, "scalar_tensor_tensor", reads=[br2, ss_r, gn_r], writes=[on_r], out=ont[:, hh * 256:(hh + 1) * 256],
                          in0=bt2[0:64, cs0:cs0 + 256], scalar=sst[:, hh:hh + 1], in1=gn[:], op0=ALU.mult, op1=ALU.mult)
                sgt, sg_r = sg.next()
                fw.op(fw.act, "activation", reads=[gbt_r], writes=[sg_r], out=sgt[:], in_=gbt[:], func=AF.Silu)
                obt, ob_r = ob.next()
                fw.op(fw.pool, "tensor_tensor", reads=[on_r, sg_r], writes=[ob_r], out=obt[:], in0=ont[:], in1=sgt[:], op=ALU.mult)
                fw.dma("pool", out=o0[b, t0:t0 + 64, 1024:2048], in_=obt[:], reads=[ob_r], writes=[R["o0"][b]])

    def phase_inproj1(sc, b):
        if True:
            convert(["in1", "out1", "g1", "u1", "d1", "pg1", "pe1"])
            dn = Dense(sc)
            load_gain(dn.gain, dn.gain_r, 1)
            sc_q = float(128 ** -0.5)
            for g in range(NG):
                for tt in range(4):
                    fw.dma("sp", out=dn.hres[tt][:], in_=h[b, g * G + tt * 128:g * G + (tt + 1) * 128, :],
                           reads=[R["h"][b]], writes=[dn.hres_r[tt]])
                    norm_tile(dn, dn.hres[tt], dn.hres_r[tt], dn.xnT, dn.xnT_r, tt * 128)

                def ep_fm(chunk, bt, br):
                    st, sr = dn.ev16.next()
                    if chunk < 16:
                        fw.op(fw.act, "mul", reads=[br], writes=[sr], out=st[:, 0:G], in_=bt[:, 0:G], mul=sc_q)
                    else:
                        evac_copy(st[:, 0:G], bt[:, 0:G], [br], [sr])
                    fw.dma("pool", out=fm1[b, chunk, :, g * G:(g + 1) * G], in_=st[:, 0:G], reads=[sr], writes=[R["fm1"][b]])
                proj_fm(dn, dn.xnT, dn.xnT_r, 16, "in1", 32, G, ep_fm)

                def ep_tm(c0, cw, tt, bt, br):
                    st, sr = dn.ev16.next()
                    evac_copy(st[:, 0:cw], bt[:, 0:cw], [br], [sr])
                    fw.dma("pool", out=tm1[b, g * G + tt * 128:g * G + (tt + 1) * 128, c0 - 2 * D:c0 - 2 * D + cw], in_=st[:, 0:cw],
                           reads=[sr], writes=[R["tm1"][b]])
                proj_tm(dn, dn.xnT, dn.xnT_r, 16, 16, "in1", 3 * D, 4, ep_tm, c_start=2 * D)

    def phase_sb(sc, b):
        QB = 512
        if True:
            qT = Ring([sc.sb([128, S], BF16, "qT") for _ in range(2)])
            kT = Ring([sc.sb([128, S], BF16, "kT") for _ in range(2)])
            vv = Ring([sc.sb([128, NT, 128], BF16, "vv") for _ in range(2)])
            Ebuf = Ring([sc.sb([128, 512], F32, "sbE") for _ in range(4)])
            Lp = Ring([sc.sb([128, 512], BF16, "Lp") for _ in range(3)])
            acc, acc_r = sc.sb([128, 512], BF16, "acc")
            Wt = Ring([sc.sb([128, 512], BF16, "Wt") for _ in range(3)])
            Xt = Ring([sc.sb([128, 512], BF16, "Xt") for _ in range(3)])
            ost = Ring([sc.sb([128, 512], BF16, "ost") for _ in range(2)])
            cb = list(fw.cur_banks)
            if len(cb) >= 8:
                b1_banks, b2_bank, bo_bank = [cb[0], cb[1], cb[2]], cb[3], cb[4]
            else:
                b1_banks, b2_bank, bo_bank = [cb[0], cb[1]], cb[2], cb[3]
            heads = {}

            def load_head(hd):
                if hd >= 16 or hd in heads:
                    return
                q_, q_r = qT.next()
                k_, k_r = kT.next()
                v_, v_r = vv.next()
                fw.dma("sp", out=q_[:], in_=fm1[b, hd], reads=[R["fm1"][b]], writes=[q_r])
                fw.dma("sp", out=k_[:], in_=fm1[b, 16 + hd], reads=[R["fm1"][b]], writes=[k_r])
                fw.dma("sp", out=v_[:], in_=tm1[b, :, hd * 128:(hd + 1) * 128].rearrange("(j p) c -> p j c", p=128),
                       reads=[R["tm1"][b]], writes=[v_r])
                heads[hd] = (q_, q_r, k_, k_r, v_, v_r)

            items = []
            for hd in range(16):
                for qb in range(S // QB):
                    for c in range(4 * qb + 3, -1, -1):
                        items.append((hd, qb, c))
            NI = len(items)
            st = {}

            def geo(it):
                hd, qb, c = it
                k_d = c - 4 * qb
                diag = k_d >= 0
                lo = 128 * k_d if diag else 0
                return hd, qb, c, qb * QB, 4 * qb + 3, k_d, diag, lo

            def stage_a(i):
                hd, qb, c, t0, cmax, k_d, diag, lo = geo(items[i])
                load_head(hd)
                q_, q_r, k_, k_r, v_, v_r = heads[hd]
                b1, b1_r = fw.banks[b1_banks[i % len(b1_banks)]]
                fw.pe_group([(b1[:, lo:QB], k_[:, c * 128:(c + 1) * 128], q_[:, t0 + lo:t0 + QB], True, True)], reads=[k_r, q_r], writes=[b1_r])
                Et, E_r = Ebuf.next()
                fw.op(fw.act, "activation", reads=[b1_r], writes=[E_r], out=Et[:, lo:QB], in_=b1[:, lo:QB], func=AF.Exp)
                Lt, L_r = Lp.next()
                fw.op(fw.act, "activation", reads=[E_r], writes=[L_r], out=Lt[:, lo:QB], in_=Et[:, lo:QB], func=AF.Ln, bias=1.0)
                if diag:
                    fw.op(fw.pool, "tensor_tensor", reads=[L_r, cs_res], writes=[L_r], out=Lt[:, lo:QB], in0=Lt[:, lo:QB],
                          in1=C("m01_%d" % k_d)[:, lo:QB], op=ALU.mult)
                st[i] = {"E": (Et, E_r), "L": (Lt, L_r)}

            def stage_b(i):
                hd, qb, c, t0, cmax, k_d, diag, lo = geo(items[i])
                Lt, L_r = st[i]["L"]
                b2, b2_r = fw.banks[b2_bank]
                mms = [(b2[:, lo:QB], C("triNeg"), Lt[:, lo:QB], True, (c == cmax) and not diag)]
                rds = [L_r, cs_res]
                if c != cmax:
                    mms.append((b2[:, lo:QB], C("onesNeg"), acc[:, lo:QB], False, not diag))
                    rds.append(acc_r)
                if diag:
                    mms.append((b2[:, lo:QB], C("ident"), C("mneg_%d" % k_d)[:, lo:QB], False, True))
                fw.pe_group(mms, reads=rds, writes=[b2_r])
                if c == cmax:
                    fw.op(fw.dve, "memset", writes=[acc_r], ap=acc[:], constant=0.0)
                if c > 0:
                    fw.op(fw.dve, "tensor_tensor", reads=[L_r], writes=[acc_r], out=acc[:, lo:QB], in0=acc[:, lo:QB], in1=Lt[:, lo:QB], op=ALU.add)
                Xtt, X_r = Xt.next()
                fw.op(fw.act, "activation", reads=[b2_r], writes=[X_r], out=Xtt[:, lo:QB], in_=b2[:, lo:QB], func=AF.Exp)
                st[i]["X"] = (Xtt, X_r)

            def stage_c(i):
                hd, qb, c, t0, cmax, k_d, diag, lo = geo(items[i])
                Et, E_r = st[i]["E"]
                Xtt, X_r = st[i]["X"]
                Wtt, W_r = Wt.next()
                fw.op(fw.dve, "tensor_tensor", reads=[X_r, E_r], writes=[W_r], out=Wtt[:, lo:QB], in0=Xtt[:, lo:QB], in1=Et[:, lo:QB], op=ALU.mult)
                st[i]["W"] = (Wtt, W_r)

            def stage_d(i):
                hd, qb, c, t0, cmax, k_d, diag, lo = geo(items[i])
                q_, q_r, k_, k_r, v_, v_r = heads[hd]
                Wtt, W_r = st[i]["W"]
                bo, bo_r = fw.banks[bo_bank]
                fw.pe_group([(bo[:, lo:QB], v_[:, c, :], Wtt[:, lo:QB], c == cmax, c == 0)], reads=[v_r, W_r], writes=[bo_r])
                if c == 0:
                    st_, st_r = ost.next()
                    evac_copy(st_[:], bo[:, :], [bo_r], [st_r])
                    fw.dma("pool", out=oT1[b, hd, :, t0:t0 + QB], in_=st_[:], reads=[st_r], writes=[R["oT1"][b]])
                    load_head(hd + 1)
                del st[i]

            for i in range(NI + 3):
                if i < NI:
                    stage_a(i)
                if 0 <= i - 3 < NI:
                    stage_d(i - 3)
                if 0 <= i - 1 < NI:
                    stage_b(i - 1)
                if 0 <= i - 2 < NI:
                    stage_c(i - 2)

    def phase_post(sc, b, layer):
        if True:
            dn = Dense(sc, evs=False, small=(len(fw.cur_banks) < 8))
            h1T, h1T_r = sc.sb([128, 22, G], BF16, "h1T")
            otile = dn.xn
            sgl = Ring([sc.sb([128, 512], F32, "sgl") for _ in range(2)])
            pt32 = Ring([sc.sb([128, PLE], F32, "pt32") for _ in range(2)])
            pt16 = Ring([sc.sb([128, PLE], BF16, "pt16") for _ in range(2)])
            pT, pT_r = sc.sb([128, 2, G], BF16, "pT")
            wpeb = Ring([sc.sb([128, 2, 512], BF16, "wpe") for _ in range(2)])
            cur_wpe = [None]
            pe_sb = Ring([sc.sb([128, 512], F32, "pesb") for _ in range(1)])
            sig = Ring([sc.sb([128, 512], F32, "sig") for _ in range(1)])
            hsrc = x if layer == 0 else h
            for g in range(NG):
                tok0 = g * G
                if layer == 0:
                    for tt in range(4):
                        ot, ot_r = otile.next()
                        fw.dma("sp", out=ot[:], in_=o0[b, tok0 + tt * 128:tok0 + (tt + 1) * 128, :], reads=[R["o0"][b]], writes=[ot_r])
                        transpose_to(dn, ot, ot_r, dn.xnT, dn.xnT_r, tt * 128)
                else:
                    fw.dma("sp", out=dn.xnT[:], in_=oT1[b, :, :, tok0:tok0 + G].rearrange("c p s -> p c s"),
                           reads=[R["oT1"][b]], writes=[dn.xnT_r])
                for tt in range(4):
                    rds = [R["h"][b]] if layer == 1 else []
                    fw.dma("sp", out=dn.hres[tt][:], in_=hsrc[b, tok0 + tt * 128:tok0 + (tt + 1) * 128, :], reads=rds, writes=[dn.hres_r[tt]])

                def ep_res(c0, cw, tt, bt, br):
                    fw.op(fw.dve, "tensor_tensor", reads=[br], writes=[dn.hres_r[tt]], out=dn.hres[tt][:, c0:c0 + cw],
                          in0=dn.hres[tt][:, c0:c0 + cw], in1=bt[:, 0:cw], op=ALU.add)
                proj_tm(dn, dn.xnT, dn.xnT_r, 16, 16, "out%d" % layer, D, 4, ep_res)
                load_gain(dn.gain, dn.gain_r, 2 + layer)
                for tt in range(4):
                    norm_tile(dn, dn.hres[tt], dn.hres_r[tt], dn.xnT, dn.xnT_r, tt * 128)
                for half in range(2):
                    h0 = half * 22
                    for hb in range(h0, h0 + 22, 4):
                        nb = min(4, h0 + 22 - hb)
                        wgt, wg_r = load_w(dn, "g%d" % layer, 0, 16, hb * 128, nb * 128)
                        wut, wu_r = load_w(dn, "u%d" % layer, 0, 16, hb * 128, nb * 128)
                        for cc in range(nb):
                            bg, bg_r = fw.bank()
                            bu, bu_r = fw.bank()
                            fw.pe_group([(bg[:, 0:G], wgt[:, kc, cc * 128:(cc + 1) * 128], dn.xnT[:, kc, 0:G], kc == 0, kc == 15) for kc in range(16)],
                                        reads=[dn.xnT_r, wg_r], writes=[bg_r])
                            fw.pe_group([(bu[:, 0:G], wut[:, kc, cc * 128:(cc + 1) * 128], dn.xnT[:, kc, 0:G], kc == 0, kc == 15) for kc in range(16)],
                                        reads=[dn.xnT_r, wu_r], writes=[bu_r])
                            s_, s_r = sgl.next()
                            fw.op(fw.act, "activation", reads=[bg_r], writes=[s_r], out=s_[:, 0:G], in_=bg[:, 0:G], func=AF.Silu)
                            fw.op(fw.dve, "tensor_tensor", reads=[s_r, bu_r], writes=[h1T_r], out=h1T[:, hb - h0 + cc, :], in0=s_[:, 0:G], in1=bu[:, 0:G], op=ALU.mult)
                    proj_tm(dn, h1T, h1T_r, 22, 11, "d%d" % layer, D, 4, ep_res, r_off=h0 * 128)
                load_gain(dn.gain, dn.gain_r, 4 + layer)
                for tt in range(4):
                    norm_tile(dn, dn.hres[tt], dn.hres_r[tt], dn.xnT, dn.xnT_r, tt * 128)
                    p32, p32_r = pt32.next()
                    p16, p16_r = pt16.next()
                    fw.dma("sp", out=p32[:], in_=p[layer, b, tok0 + tt * 128:tok0 + (tt + 1) * 128, :], writes=[p32_r])
                    fw.op(fw.pool, "tensor_copy", reads=[p32_r], writes=[p16_r], out=p16[:], in_=p32[:])
                    bt, br = fw.bank()
                    ptb = bt[:].bitcast(BF16)
                    fw.pe_group([(ptb[:, j * 128:(j + 1) * 128], p16[:, j * 128:(j + 1) * 128], C("ident")) for j in range(2)],
                                reads=[p16_r, cs_res], writes=[br], transpose=True)
                    evac_copy(pT[:, :, tt * 128:(tt + 1) * 128], ptb[:, 0:256].rearrange("p (j c) -> p j c", j=2), [br], [pT_r])

                def ep_ple(c0, cw, tt, bt, br):
                    if tt == 0:
                        wpe_t, wpe_r = wpeb.next()
                        fw.dma("sp", out=wpe_t[:, :, 0:cw], in_=wb["pe%d" % layer][:, c0:c0 + cw].rearrange("(kc p) c -> p kc c", p=128),
                               reads=[wres["pe%d" % layer]], writes=[wpe_r])
                        cur_wpe[0] = (wpe_t, wpe_r)
                    wpe_t, wpe_r = cur_wpe[0]
                    b2, b2_r = fw.bank()
                    fw.pe_group([(b2[:, 0:cw], pT[:, kc, tt * 128:(tt + 1) * 128], wpe_t[:, kc, 0:cw], kc == 0, kc == 1) for kc in range(2)],
                                reads=[pT_r, wpe_r], writes=[b2_r])
                    sg_, sg_r = sig.next()
                    fw.op(fw.act, "activation", reads=[br], writes=[sg_r], out=sg_[:, 0:cw], in_=bt[:, 0:cw], func=AF.Sigmoid)
                    pe_, pe_r = pe_sb.next()
                    fw.op(fw.dve, "tensor_tensor", reads=[sg_r, b2_r], writes=[pe_r], out=pe_[:, 0:cw], in0=sg_[:, 0:cw], in1=b2[:, 0:cw], op=ALU.mult)
                    fw.op(fw.pool, "tensor_tensor", reads=[pe_r], writes=[dn.hres_r[tt]], out=dn.hres[tt][:, c0:c0 + cw],
                          in0=dn.hres[tt][:, c0:c0 + cw], in1=pe_[:, 0:cw], op=ALU.add)
                proj_tm(dn, dn.xnT, dn.xnT_r, 16, 16, "pg%d" % layer, D, 4, ep_ple)
                if layer == 0:
                    for tt in range(4):
                        fw.dma("pool", out=h[b, tok0 + tt * 128:tok0 + (tt + 1) * 128, :], in_=dn.hres[tt][:], reads=[dn.hres_r[tt]], writes=[R["h"][b]])
                else:
                    load_gain(dn.gain, dn.gain_r, 6)
                    for tt in range(4):
                        ss, ss_r = dn.ss.next()
                        junk, junk_r = dn.junk.next()
                        rms_rstd(dn, dn.hres[tt][:], dn.hres_r[tt], 128, D, ss, ss_r, junk[:], junk_r)
                        fw.op(fw.dve, "scalar_tensor_tensor", reads=[ss_r, dn.gain_r], writes=[dn.hres_r[tt]], out=dn.hres[tt][:], in0=dn.hres[tt][:],
                              scalar=ss[:, 0:1], in1=dn.gain[:], op0=ALU.mult, op1=ALU.mult)
                        fw.dma("pool", out=out[b, tok0 + tt * 128:tok0 + (tt + 1) * 128, :], in_=dn.hres[tt][:], reads=[dn.hres_r[tt]], writes=[R["out"][b]])

    ALL8 = list(range(8))

    def run_group(streams):
        with Scope(fw) as sc:
            recs = []
            for fn, args, banks in streams:
                fw.rec = []
                fw.bank_ids = list(banks)
                fw.cur_banks = list(banks)
                fw.bank_i = 0
                fn(sc, *args)
                recs.append(fw.rec)
                fw.rec = None
            pos = [0] * len(recs)
            while True:
                best, bf = None, None
                for i, r in enumerate(recs):
                    if pos[i] < len(r):
                        f = pos[i] / float(len(r))
                        if bf is None or f < bf:
                            best, bf = i, f
                if best is None:
                    break
                fnc, a, kw = recs[best][pos[best]]
                fnc(*a, **kw)
                pos[best] += 1
        fw.bank_ids = list(ALL8)

    sched = []
    if NSEQ == 1:
        sched = [("inproj0", [(phase_inproj0, (0,), ALL8)]), ("dsa", [(phase_dsa, (0,), ALL8)]), ("gla", [(phase_gla, (0,), ALL8)]),
                 ("post0", [(phase_post, (0, 0), ALL8)]), ("inproj1", [(phase_inproj1, (0,), ALL8)]), ("sb", [(phase_sb, (0,), ALL8)]),
                 ("post1", [(phase_post, (0, 1), ALL8)])]
    else:
        assert NSEQ == 2
        LO, HI = [0, 1, 2, 3], [4, 5, 6, 7]
        sched = [
            ("a", [(phase_inproj0, (0,), ALL8)]),
            ("b", [(phase_dsa, (0,), ALL8)]),
            ("c", [(phase_inproj0, (1,), LO), (phase_gla, (0,), HI)]),
            ("e", [(phase_post, (0, 0), LO), (phase_gla, (1,), HI)]),
            ("f", [(phase_dsa, (1,), ALL8)]),
            ("g", [(phase_inproj1, (0,), ALL8)]),
            ("h", [(phase_post, (1, 0), LO), (phase_sb, (0,), HI)]),
            ("i", [(phase_inproj1, (1,), ALL8)]),
            ("j", [(phase_post, (0, 1), LO), (phase_sb, (1,), HI)]),
            ("k", [(phase_post, (1, 1), ALL8)]),
        ]
    for name, streams in sched:
        run_group(streams)
        if stop is not None and name == stop:
            break
    fw.barrier()
    es.close()
    return nc


def prep_weights(inp):
    f = lambda a: np.ascontiguousarray(np.asarray(a, dtype=np.float32))
    w0 = f(inp["even_w_in"])[0]
    wfm0 = np.zeros((D, NFM0 * 128), np.float32)
    wfm0[:, 0:1024] = w0[:, OFF_QA:OFF_QA + 1024]
    wfm0[:, 1024:1152] = w0[:, OFF_CA:OFF_CA + 128]
    wfm0[:, 1152:2176] = w0[:, OFF_QI:OFF_QI + 1024]
    wfm0[:, 2176:2240] = w0[:, OFF_KI:OFF_KI + 64]
    wfm0[:, 2240:2304] = w0[:, OFF_KI:OFF_KI + 64]
    wfm0[:, 2304:2320] = w0[:, OFF_GK:OFF_GK + 16]
    wtm0 = np.concatenate([w0[:, OFF_CA:OFF_CA + 128], w0[:, OFF_WI:OFF_WI + 16], w0[:, OFF_QB:OFF_GK]], axis=1)
    assert wtm0.shape[1] == TMW0
    gains = np.stack([f(inp["even_norm"])[0], f(inp["odd_norm"])[0], f(inp["ffn_norm"])[0], f(inp["ffn_norm"])[1],
                      f(inp["ple_norm"])[0], f(inp["ple_norm"])[1], f(inp["final_norm"])], axis=0)
    gains = np.ascontiguousarray(np.broadcast_to(gains[:, None, :], (7, 128, D)))
    smalls = np.ascontiguousarray(np.broadcast_to(f(inp["even_gla_norm"])[0][None, :], (64, 256)))
    wgk = np.concatenate([f(inp["even_w_gk"])[0], f(inp["even_b_gk"])[0][None, :]], axis=0)
    m = {"gains": gains, "smalls": smalls, "cst": CONSTS, "wgk": np.ascontiguousarray(wgk),
         "wfm0": wfm0, "wtm0": np.ascontiguousarray(wtm0), "wout0": f(inp["even_w_out"])[0],
         "win1": f(inp["odd_w_in"])[0], "wout1": f(inp["odd_w_out"])[0]}
    for i in range(2):
        m["wg%d" % i] = f(inp["ffn_w_gate"])[i]
        m["wu%d" % i] = f(inp["ffn_w_up"])[i]
        m["wd%d" % i] = f(inp["ffn_w_down"])[i]
        m["wpg%d" % i] = f(inp["ple_w_gate"])[i]
        m["wpe%d" % i] = f(inp["ple_w_proj"])[i]
    return m


_CACHE = {}


def kernel(**inputs):
    x = np.asarray(inputs["x"], dtype=np.float32)
    p = np.asarray(inputs["p"], dtype=np.float32)
    B, S, _ = x.shape
    ncores = 8
    nseq = B // ncores
    wm = prep_weights(inputs)
    key = (S, nseq)
    if key not in _CACHE:
        _CACHE[key] = build(S, nseq)
    nc = _CACHE[key]
    in_maps = []
    for c in range(ncores):
        m = dict(wm)
        m["x"] = np.ascontiguousarray(x[c * nseq:(c + 1) * nseq])
        m["p"] = np.ascontiguousarray(p[:, c * nseq:(c + 1) * nseq])
        in_maps.append(m)
    res = run_bass_kernel_spmd(nc, in_maps, core_ids=list(range(ncores)))
    return np.concatenate([r["out"] for r in res.results], axis=0).astype(np.float32)
```
